# Optimizing a Trainium2 kernel written in Bass

```python
import math
import jax, jax.numpy as jnp
from jax import lax
import numpy as np

D_MODEL = 1024
BATCH = 4
SEQ = 8192
DEPTH = 2

HEAD_DIM = 64
NSA_HEADS = 8
NSA_KV_HEADS = 2
NSA_GROUP = NSA_HEADS // NSA_KV_HEADS
RET_HEADS = 8
RET_DK = 32
RET_DV = 64
NSA_WIDTH = NSA_HEADS * HEAD_DIM
RET_WIDTH = RET_HEADS * RET_DV
MIX_WIDTH = NSA_WIDTH + RET_WIDTH
KV_WIDTH = NSA_KV_HEADS * HEAD_DIM
CMP_LEN = 32
CMP_STRIDE = 16
SLC_LEN = 64
SLC_TOPK = 16
WIN = 512
Q_BLOCK = 128
RET_CHUNK = 128
D_FF = 2816
N_EXPERTS = 8
TOP_K_EXPERTS = 2
N_DENSE = (DEPTH + 1) // 2
N_MOE = DEPTH // 2
EPS = 1e-6
NEG = -1e30
BIG = 1e9
IN_WIDTHS = (NSA_WIDTH, KV_WIDTH, KV_WIDTH, KV_WIDTH, KV_WIDTH, KV_WIDTH, KV_WIDTH,
             3 * NSA_HEADS, RET_HEADS * RET_DK, RET_HEADS * RET_DK, RET_WIDTH, RET_WIDTH)
IN_COLS = sum(IN_WIDTHS)

kernel_name = "hymba_nsa_retention_moe_block"


def rms_norm(x, g):
    xf = x.astype(jnp.float32)
    y = xf * lax.rsqrt(jnp.mean(xf * xf, axis=-1, keepdims=True) + EPS)
    return (y * g).astype(x.dtype)


def alibi_slopes(n):
    return jnp.exp2(-8.0 * jnp.arange(1, n + 1, dtype=jnp.float32) / n)


def _cmp_to_slc(nc, ns):
    cs = jnp.arange(nc) * CMP_STRIDE
    ce = cs + CMP_LEN
    ss = jnp.arange(ns) * SLC_LEN
    se = ss + SLC_LEN
    ov = jnp.clip(jnp.minimum(ce[:, None], se[None]) - jnp.maximum(cs[:, None], ss[None]), 0, None)
    return ov.astype(jnp.float32) / CMP_LEN


def _nsa(q, k_cmp, v_cmp, k_slc, v_slc, k_win, v_win, gates):
    B, G, R, T, Dh = q.shape
    nc = k_cmp.shape[2]
    ns = T // SLC_LEN
    n_sel = min(SLC_TOPK, ns)
    scale = Dh ** -0.5
    slopes = alibi_slopes(NSA_HEADS).reshape(G, R)[:, :, None, None]
    cmp_end = jnp.arange(nc) * CMP_STRIDE + CMP_LEN - 1
    overlap = _cmp_to_slc(nc, ns)
    k_blk = k_slc.reshape(B, G, ns, SLC_LEN, Dh)
    v_blk = v_slc.reshape(B, G, ns, SLC_LEN, Dh)
    pad = ((0, 0), (0, 0), (WIN, 0), (0, 0))
    k_win_p = jnp.pad(k_win, pad)
    v_win_p = jnp.pad(v_win, pad)
    bi = jnp.arange(B)[:, None, None]
    gi = jnp.arange(G)[None, :, None]
    blk = jnp.arange(ns)

    def block(i):
        q0 = i * Q_BLOCK
        qb = lax.dynamic_slice_in_dim(q, q0, Q_BLOCK, axis=3)
        gb = lax.dynamic_slice_in_dim(gates, q0, Q_BLOCK, axis=3)
        t = q0 + jnp.arange(Q_BLOCK)

        s = jnp.einsum('bgrqd,bgcd->bgrqc', qb, k_cmp).astype(jnp.float32) * scale
        dist = (t[:, None] - cmp_end[None, :]).astype(jnp.float32)
        s = jnp.where(dist >= 0, s - slopes * dist, NEG)
        p_cmp = jax.nn.softmax(s, axis=-1) * (t >= CMP_LEN - 1)[:, None]
        o_cmp = jnp.einsum('bgrqc,bgcd->bgrqd', p_cmp.astype(v_cmp.dtype), v_cmp)

        imp = jnp.einsum('bgrqc,cs->bgqs', p_cmp, overlap)
        cur = t // SLC_LEN
        forced = (blk[None] == 0) | (blk[None] == cur[:, None]) | (blk[None] == cur[:, None] - 1)
        valid = blk[None] * SLC_LEN <= t[:, None]
        imp = jnp.where(forced, BIG, jnp.where(valid, imp, -BIG))
        _, sel = lax.top_k(imp, n_sel)
        sel_flat = sel.reshape(B, G, Q_BLOCK * n_sel)
        k_sel = k_blk[bi, gi, sel_flat].reshape(B, G, Q_BLOCK, n_sel * SLC_LEN, Dh)
        v_sel = v_blk[bi, gi, sel_flat].reshape(B, G, Q_BLOCK, n_sel * SLC_LEN, Dh)
        pos = (sel[..., None] * SLC_LEN + jnp.arange(SLC_LEN)).reshape(B, G, Q_BLOCK, n_sel * SLC_LEN)
        s = jnp.einsum('bgrqd,bgqkd->bgrqk', qb, k_sel).astype(jnp.float32) * scale
        dist = (t[:, None] - pos).astype(jnp.float32)[:, :, None]
        s = jnp.where(dist >= 0, s - slopes * dist, NEG)
        p = jax.nn.softmax(s, axis=-1)
        o_slc = jnp.einsum('bgrqk,bgqkd->bgrqd', p.astype(v_sel.dtype), v_sel)

        kw = lax.dynamic_slice_in_dim(k_win_p, q0, WIN + Q_BLOCK, axis=2)
        vw = lax.dynamic_slice_in_dim(v_win_p, q0, WIN + Q_BLOCK, axis=2)
        posw = q0 - WIN + jnp.arange(WIN + Q_BLOCK)
        dist = (t[:, None] - posw[None, :])
        ok = (dist >= 0) & (dist < WIN) & (posw[None, :] >= 0)
        s = jnp.einsum('bgrqd,bgkd->bgrqk', qb, kw).astype(jnp.float32) * scale
        s = jnp.where(ok, s - slopes * dist.astype(jnp.float32), NEG)
        p = jax.nn.softmax(s, axis=-1)
        o_win = jnp.einsum('bgrqk,bgkd->bgrqd', p.astype(vw.dtype), vw)

        return gb[..., 0:1] * o_cmp + gb[..., 1:2] * o_slc + gb[..., 2:3] * o_win

    out = lax.map(block, jnp.arange(T // Q_BLOCK))
    return out.transpose(1, 0, 4, 2, 3, 5).reshape(B, T, G * R * Dh)


def _retention(q, k, v):
    B, T, H, dk = q.shape
    dv = v.shape[-1]
    nch = T // RET_CHUNK
    log_g = jnp.log1p(-jnp.exp2(-5.0 - jnp.arange(H, dtype=jnp.float32)))
    idx = jnp.arange(RET_CHUNK, dtype=jnp.float32)
    diff = idx[:, None] - idx[None, :]
    decay = jnp.where(diff >= 0, jnp.exp(jnp.maximum(diff, 0.0) * log_g[:, None, None]), 0.0)
    zeta = jnp.exp((RET_CHUNK - 1 - idx) * log_g[:, None])
    xi = jnp.exp((idx + 1) * log_g[:, None])
    g_chunk = jnp.exp(RET_CHUNK * log_g)

    def chunks(a):
        return a.reshape(B, nch, RET_CHUNK, H, a.shape[-1]).transpose(1, 0, 3, 2, 4).astype(jnp.float32)

    def step(state, qkv):
        qc, kc, vc = qkv
        inner = jnp.einsum('bhid,bhjd->bhij', qc, kc) * decay
        o = jnp.einsum('bhij,bhje->bhie', inner, vc) + jnp.einsum('bhid,bhde->bhie', qc, state) * xi[..., None]
        state = state * g_chunk[:, None, None] + jnp.einsum('bhjd,bhje->bhde', kc * zeta[..., None], vc)
        return state, o

    _, o = lax.scan(step, jnp.zeros((B, H, dk, dv), jnp.float32), (chunks(q), chunks(k), chunks(v)))
    return o.transpose(1, 0, 3, 2, 4).reshape(B, T, H, dv)


def _mixer(h, w_in, q_norm_g, k_norm_g, cmp_pos, w_cmp, ret_norm_g, w_out):
    B, T, _ = h.shape
    G, R, Dh = NSA_KV_HEADS, NSA_GROUP, HEAD_DIM
    points = [int(p) for p in np.cumsum(IN_WIDTHS)[:-1]]
    q, kc, vc, ks, vs, kw, vw, gts, rq, rk, rv, rg = jnp.split(h @ w_in, points, axis=-1)

    q = rms_norm(q.reshape(B, T, G, R, Dh).transpose(0, 2, 3, 1, 4), q_norm_g)

    def kv(a):
        return a.reshape(B, T, G, Dh).transpose(0, 2, 1, 3)

    k_slc = rms_norm(kv(ks), k_norm_g[1])
    k_win = rms_norm(kv(kw), k_norm_g[2])
    nc = (T - CMP_LEN) // CMP_STRIDE + 1
    cidx = jnp.arange(nc)[:, None] * CMP_STRIDE + jnp.arange(CMP_LEN)[None, :]

    def compress(a, pos_emb, w):
        blocks = kv(a)[:, :, cidx] + pos_emb
        return blocks.reshape(B, G, nc, CMP_LEN * Dh) @ w

    k_cmp = rms_norm(compress(kc, cmp_pos[0], w_cmp[0]), k_norm_g[0])
    v_cmp = compress(vc, cmp_pos[1], w_cmp[1])
    gates = jax.nn.sigmoid(gts.reshape(B, T, G, R, 3).transpose(0, 2, 3, 1, 4))
    nsa = _nsa(q, k_cmp, v_cmp, k_slc, kv(vs), k_win, kv(vw), gates)

    rq = rq.reshape(B, T, RET_HEADS, RET_DK) * (RET_DK ** -0.5)
    rk = rk.reshape(B, T, RET_HEADS, RET_DK)
    rv = rv.reshape(B, T, RET_HEADS, RET_DV)
    ret = rms_norm(_retention(rq, rk, rv), ret_norm_g.reshape(RET_HEADS, RET_DV))
    ret = ret.reshape(B, T, RET_WIDTH).astype(h.dtype) * jax.nn.silu(rg)

    return jnp.concatenate([nsa, ret], axis=-1) @ w_out


def _swiglu(h, wg, wu, wd):
    return (jax.nn.silu(h @ wg) * (h @ wu)) @ wd


def _moe(h, router, router_b, wg, wu, wd):
    logits = (h @ router).astype(jnp.float32) + router_b
    top_vals, top_idx = lax.top_k(logits, TOP_K_EXPERTS)
    w = jax.nn.softmax(top_vals, axis=-1)
    combine = jnp.sum(jax.nn.one_hot(top_idx, N_EXPERTS, dtype=jnp.float32) * w[..., None], axis=-2)
    y = jnp.zeros_like(h)
    for e in range(N_EXPERTS):
        y = y + combine[..., e:e + 1].astype(h.dtype) * _swiglu(h, wg[e], wu[e], wd[e])
    return y


def setup_inputs(seed: int = 0) -> dict:
    key = jax.random.key(seed)
    ks = jax.random.split(key, 18)

    def nrm(k, shape, scale):
        return jax.random.normal(k, shape, jnp.float32) * scale

    def gain(k, shape):
        return 1.0 + 0.01 * jax.random.normal(k, shape, jnp.float32)

    return {
        "x": nrm(ks[0], (BATCH, SEQ, D_MODEL), 1.0),
        "norm_mix_g": gain(ks[1], (DEPTH, D_MODEL)),
        "w_in": nrm(ks[2], (DEPTH, D_MODEL, IN_COLS), D_MODEL ** -0.5),
        "q_norm_g": gain(ks[3], (DEPTH, HEAD_DIM)),
        "k_norm_g": gain(ks[4], (DEPTH, 3, HEAD_DIM)),
        "cmp_pos": nrm(ks[5], (DEPTH, 2, CMP_LEN, HEAD_DIM), 0.02),
        "w_cmp": nrm(ks[6], (DEPTH, 2, CMP_LEN * HEAD_DIM, HEAD_DIM), (CMP_LEN * HEAD_DIM) ** -0.5),
        "ret_norm_g": gain(ks[7], (DEPTH, RET_WIDTH)),
        "w_out": nrm(ks[8], (DEPTH, MIX_WIDTH, D_MODEL), MIX_WIDTH ** -0.5),
        "norm_ffn_g": gain(ks[9], (DEPTH, D_MODEL)),
        "ffn_w_gate": nrm(ks[10], (N_DENSE, D_MODEL, D_FF), D_MODEL ** -0.5),
        "ffn_w_up": nrm(ks[11], (N_DENSE, D_MODEL, D_FF), D_MODEL ** -0.5),
        "ffn_w_down": nrm(ks[12], (N_DENSE, D_FF, D_MODEL), D_FF ** -0.5),
        "moe_router": nrm(ks[13], (N_MOE, D_MODEL, N_EXPERTS), D_MODEL ** -0.5),
        "moe_router_b": nrm(ks[14], (N_MOE, N_EXPERTS), 0.01),
        "moe_w_gate": nrm(ks[15], (N_MOE, N_EXPERTS, D_MODEL, D_FF), D_MODEL ** -0.5),
        "moe_w_up": nrm(ks[16], (N_MOE, N_EXPERTS, D_MODEL, D_FF), D_MODEL ** -0.5),
        "moe_w_down": nrm(ks[17], (N_MOE, N_EXPERTS, D_FF, D_MODEL), D_FF ** -0.5),
    }


def reference(x, norm_mix_g, w_in, q_norm_g, k_norm_g, cmp_pos, w_cmp, ret_norm_g, w_out,
              norm_ffn_g, ffn_w_gate, ffn_w_up, ffn_w_down,
              moe_router, moe_router_b, moe_w_gate, moe_w_up, moe_w_down):
    for l in range(DEPTH):
        h = rms_norm(x, norm_mix_g[l])
        x = x + _mixer(h, w_in[l], q_norm_g[l], k_norm_g[l], cmp_pos[l], w_cmp[l], ret_norm_g[l], w_out[l])
        h = rms_norm(x, norm_ffn_g[l])
        j = l // 2
        if l % 2 == 0:
            x = x + _swiglu(h, ffn_w_gate[j], ffn_w_up[j], ffn_w_down[j])
        else:
            x = x + _moe(h, moe_router[j], moe_router_b[j], moe_w_gate[j], moe_w_up[j], moe_w_down[j])
    return x
```

```python
import numpy as np
import ml_dtypes
from contextlib import ExitStack
import concourse.bass as bass
import concourse.mybir as mybir
from concourse.bass_utils import run_bass_kernel_spmd

F32 = mybir.dt.float32
BF16 = mybir.dt.bfloat16
AF = mybir.ActivationFunctionType
ALU = mybir.AluOpType
AX = mybir.AxisListType
NPBF = ml_dtypes.bfloat16

PE, ACT, DVE, POOL, SP = "tensor", "scalar", "vector", "gpsimd", "sync"
ENGS = [PE, ACT, DVE, POOL, SP]
SEM_LIMIT = 30000


class Buf:
    __slots__ = ("ap", "w", "r", "rd", "excl", "name")

    def __init__(self, ap, excl=False, name=""):
        self.ap = ap
        self.w = None
        self.r = {}
        self.rd = []
        self.excl = excl
        self.name = name

    def __getitem__(self, k):
        return self.ap[k]


class Op:
    __slots__ = ("eng", "fn", "deps", "inc", "tok", "dma", "slotdep")

    def __init__(self, eng, fn, dma):
        self.eng = eng
        self.fn = fn
        self.deps = []
        self.inc = False
        self.tok = None
        self.dma = dma
        self.slotdep = None


class Sched:
    def __init__(self, nc, es, tag=""):
        self.tag = tag
        self.nc = nc
        self.es = es
        self.ops = {e: [] for e in ENGS}
        self.n_dma_sems = 6

    def sb(self, name, shape, dt, excl=False):
        t = self.es.enter_context(self.nc.sbuf_tensor(self.tag + name, list(shape), dt))
        return Buf(t, excl, name)

    def ps(self, name, shape=(128, 512), dt=F32):
        t = self.es.enter_context(self.nc.psum_tensor(self.tag + name, list(shape), dt))
        return Buf(t, True, name)

    def op(self, eng, fn, r=(), w=(), dma=False, **kw):
        if isinstance(fn, str):
            name = fn
            fn = lambda e, name=name, kw=kw: getattr(e, name)(**kw)
        o = Op(eng, fn, dma)
        deps = o.deps

        def add(d):
            if d is None:
                return
            if d.eng == eng and not d.dma and eng == PE:
                return
            if d not in deps:
                deps.append(d)

        for b in r:
            add(b.w)
            if b.excl:
                for e2, d in b.r.items():
                    if e2 != eng:
                        add(d)
        for b in w:
            add(b.w)
            for e2, d in b.r.items():
                if e2 != eng or dma or d.dma:
                    add(d)
            for d in b.rd:
                add(d)
        for b in r:
            if dma:
                b.rd.append(o)
            else:
                b.r[eng] = o
        for b in w:
            b.w = o
            b.r = {}
            b.rd = []
        self.ops[eng].append(o)
        return o

    def dma(self, eng, out, in_, r=(), w=(), **kw):
        return self.op(eng, lambda e, out=out, in_=in_, kw=kw: e.dma_start(out=out, in_=in_, **kw), r=r, w=w, dma=True)

    def emit(self):
        nc, es = self.nc, self.es
        for e in ENGS:
            for o in self.ops[e]:
                for d in o.deps:
                    d.inc = True
                if o.dma:
                    o.inc = True
        sems = {}
        nsem = [0]

        def newsem(tag):
            nsem[0] += 1
            return es.enter_context(nc.semaphore(f"{self.tag}s_{tag}_{nsem[0]}"))

        for e in ENGS:
            cur = None
            cnt = 0
            dma_sems = []
            dma_cnt = []
            dma_last = []
            k = 0
            for o in self.ops[e]:
                if not o.inc:
                    continue
                if o.dma:
                    i = k % self.n_dma_sems
                    k += 1
                    if len(dma_sems) <= i:
                        dma_sems.append(newsem(e + "d"))
                        dma_cnt.append(0)
                        dma_last.append(None)
                    if dma_cnt[i] + 16 > SEM_LIMIT:
                        dma_sems[i] = newsem(e + "d")
                        dma_cnt[i] = 0
                    o.slotdep = dma_last[i]
                    dma_cnt[i] += 16
                    o.tok = (dma_sems[i], dma_cnt[i], 16)
                    dma_last[i] = o
                else:
                    if cur is None or cnt + 1 > SEM_LIMIT:
                        cur = newsem(e)
                        cnt = 0
                    cnt += 1
                    o.tok = (cur, cnt, 1)
        self.nsem = nsem[0]
        blk = es.enter_context(nc.Block())

        def make(ename):
            def body(eng):
                waited = {}
                for o in self.ops[ename]:
                    dl = list(o.deps)
                    if o.slotdep is not None:
                        dl.append(o.slotdep)
                    for d in dl:
                        s, v, _ = d.tok
                        key = id(s)
                        if waited.get(key, 0) >= v:
                            continue
                        waited[key] = v
                        eng.wait_ge(s, v)
                    inst = o.fn(eng)
                    if o.inc:
                        s, v, step = o.tok
                        inst.then_inc(s, step)
                for o in self.ops[ename]:
                    if o.dma:
                        s, v, _ = o.tok
                        if waited.get(id(s), 0) < v:
                            waited[id(s)] = v
                            eng.wait_ge(s, v)
            return body

        for ename in ENGS:
            if self.ops[ename]:
                getattr(blk, ename)(make(ename))


EPS = 1e-6
NEGM = -30000.0


def mix_tables(T, g):
    NB = T // 128
    tb = {}
    slopes = np.array([2.0 ** -(4 * g + r + 1) for r in range(4)], np.float64)
    pos = np.arange(T)
    kx = np.stack([np.ones(T), np.ones(T), 128.0 * (pos // 128), (pos % 128).astype(np.float64)])
    tb["kx"] = kx.astype(NPBF)
    NS = 512
    s = np.arange(NS)
    cpos = 16 * s + 15
    kxc = np.stack([np.ones(NS), np.ones(NS), 128.0 * (cpos // 128), (cpos % 128).astype(np.float64)])
    tb["kxc"] = kxc.astype(NPBF)
    ql = np.arange(128)
    qx = np.zeros((4, 4, 128))
    for h in range(4):
        qx[0, h] = -slopes[h] * 128.0
        qx[1, h] = -slopes[h] * ql
        qx[2, h] = slopes[h]
        qx[3, h] = slopes[h]
    tb["qx"] = qx.reshape(4, 512).astype(NPBF)
    k = np.arange(128)[:, None]
    q = np.arange(128)[None, :]
    mca = np.where(k <= q, 0.0, NEGM)
    mwi = np.where(k > q, 0.0, NEGM)
    tb["mcaus"] = np.tile(mca, (1, 4)).astype(NPBF)
    tb["mwin"] = np.tile(mwi, (1, 4)).astype(NPBF)
    u = np.arange(2048 + 128)[None, :]
    sl = np.arange(128)[:, None]
    mc = np.where(16 * sl + 15 <= u, 0.0, NEGM)
    mc0 = mc.copy()
    mc0[0, :] = NEGM
    tb["mc"] = np.stack([mc0, mc], 1).astype(NPBF)
    blk = np.arange(128)[:, None]
    key = np.arange(T)[None, :]
    tb["eall"] = (key // 64 == blk).astype(np.float32).astype(NPBF)
    ov = np.zeros((128, 4, 129), np.float32)
    for ct in range(4):
        for s_l in range(128):
            c = 128 * ct + s_l - 1
            if c < 0:
                continue
            cs, ce = 16 * c, 16 * c + 32
            for b_ in range(cs // 64, min(127, (ce - 1) // 64) + 1):
                o = min(ce, 64 * b_ + 64) - max(cs, 64 * b_)
                if o > 0:
                    ov[s_l, ct, b_] = o / 32.0
        ov[:, ct, 128] = 1.0
    tb["ov"] = np.concatenate([ov[:, :, 128:129], ov[:, :, 0:128]], 2).astype(NPBF)
    am = np.zeros((NB, 128, 128), np.float32)
    for i in range(NB):
        t = 128 * i + np.arange(128)
        cur = t // 64
        b_ = np.arange(128)[None, :]
        forced = (b_ == 0) | (b_ == cur[:, None]) | (b_ == cur[:, None] - 1)
        valid = b_ * 64 <= t[:, None]
        am[i] = np.where(forced, 1e9, np.where(valid, 0.0, -1e9))
    tb["am"] = am
    hh = 4 * g + np.arange(4)
    log_g = np.log1p(-np.exp2(-5.0 - hh.astype(np.float64)))
    idx = np.arange(128, dtype=np.float64)
    sc = 32 ** -0.5
    dec = np.zeros((128, 4, 128))
    for h in range(4):
        d = idx[None, :] - idx[:, None]
        dec[:, h, :] = np.where(d >= 0, np.exp(np.maximum(d, 0) * log_g[h]), 0.0) * sc
    tb["decT"] = dec.astype(np.float32)
    xi = np.zeros((128, 128))
    zeta = np.zeros((128, 128))
    gc = np.zeros((128, 1))
    bd = np.zeros((128, 4, 128))
    for h in range(4):
        xi[32 * h:32 * h + 32, :] = np.exp((idx + 1) * log_g[h])[None, :] * sc
        zeta[:, 32 * h:32 * h + 32] = np.exp((127 - idx) * log_g[h])[:, None]
        gc[32 * h:32 * h + 32] = np.exp(128 * log_g[h])
        bd[32 * h:32 * h + 32, h, :] = 1.0
    tb["xi"] = xi.astype(np.float32)
    tb["zeta"] = zeta.astype(np.float32)
    tb["gc"] = gc.astype(np.float32)
    tb["bd"] = bd.astype(np.float32).astype(NPBF)
    tb["ident"] = np.eye(128, dtype=np.float32).astype(NPBF)
    return tb


def mix_weights(l, g, p):
    w_in = p["w_in"][l]
    offs = np.cumsum([0, 512, 128, 128, 128, 128, 128, 128, 24, 256, 256, 512, 512])
    q0, kc0, vc0, ks0, vs0, kw0, vw0, gt0, rq0, rk0, rv0, rg0 = offs[:12]
    cq = w_in[:, q0 + 256 * g: q0 + 256 * g + 256]
    sl = lambda o, w: w_in[:, o + w * g: o + w * g + w]
    wtm = np.concatenate([cq, sl(ks0, 64), sl(kw0, 64), sl(vs0, 64), sl(vw0, 64), sl(rv0, 256), sl(rg0, 256),
                          sl(rk0, 128), sl(gt0, 12)], 1)
    wfm = np.concatenate([sl(kc0, 64), sl(vc0, 64), sl(rq0, 128), sl(rk0, 128)], 1)
    wc = p["w_cmp"][l]
    wbd = np.zeros((128, 32, 128), np.float32)
    wbd[0:64, :, 0:64] = wc[0].reshape(32, 64, 64).transpose(1, 0, 2)
    wbd[64:128, :, 64:128] = wc[1].reshape(32, 64, 64).transpose(1, 0, 2)
    wcf = np.concatenate([wc[0], wc[1]], 1).reshape(16, 128, 128).transpose(1, 0, 2)
    cp = p["cmp_pos"][l].reshape(2, 2048)
    posf = cp.reshape(2, 16, 128).transpose(2, 0, 1)
    rep = lambda v: np.ascontiguousarray(np.broadcast_to(v[None, :], (128, v.shape[0])))
    gqk = np.concatenate([np.tile(p["q_norm_g"][l], 4), p["k_norm_g"][l][1], p["k_norm_g"][l][2]])
    return {
        "gam": rep(p["norm_mix_g"][l]), "wtm": np.ascontiguousarray(wtm), "wfm": np.ascontiguousarray(wfm),
        "wbd": wbd, "wcf": np.ascontiguousarray(wcf), "posf": np.ascontiguousarray(posf),
        "gqk": rep(gqk), "gkc": rep(p["k_norm_g"][l][0]),
        "gret": rep(p["ret_norm_g"][l][256 * g:256 * g + 256]),
    }


def mix_decl(nc, D, T, pw, pt, shared):
    NB = T // 128

    def din(name, shape, dt=F32):
        if name not in D:
            D[name] = nc.dram_tensor(name, list(shape), dt, kind="ExternalInput").ap()

    for nm_, shp in [("gam", [128, 1024]), ("wtm", [1024, 1164]), ("wfm", [1024, 384]), ("wbd", [128, 32, 128]),
                     ("wcf", [128, 16, 128]), ("posf", [128, 2, 16]), ("gqk", [128, 384]), ("gkc", [128, 64]),
                     ("gret", [128, 256])]:
        din(pw + nm_, shp)
    din(pt + "qx", [4, 512], BF16); din(pt + "decT", [128, 4, 128]); din(pt + "xi", [128, 128])
    din(pt + "zeta", [128, 128]); din(pt + "gc", [128, 1])
    if shared:
        din("kx", [4, T], BF16); din("kxc", [4, 512], BF16)
        din("mcaus", [128, 512], BF16); din("mwin", [128, 512], BF16); din("mc", [128, 2, 2176], BF16)
        din("eall", [128, T], BF16); din("ov", [128, 4, 129], BF16); din("am", [NB, 128, 128])
        din("bd", [128, 4, 128], BF16); din("ident", [128, 128], BF16)


WNAMES = ["gam", "wtm", "wfm", "wbd", "wcf", "posf", "gqk", "gkc", "gret"]
TG_NAMES = ["qx", "decT", "xi", "zeta", "gc"]


def emit_mix(nc, D0, T, x, mix_out, pw, pt, tag=""):
    NT = T // 512
    NB = T // 128
    D = dict(D0)
    for k_ in WNAMES:
        D[k_] = D0[pw + k_]
    for k_ in TG_NAMES:
        D[k_] = D0[pt + k_]
    mix_nsa, mix_ret = mix_out

    with ExitStack() as es:
        S = Sched(nc, es, tag)
        sb = S.sb
        gam = sb("gam_s", [128, 1024], F32)
        wtm = sb("wtm_s", [128, 8, 1164], BF16)
        wfm = sb("wfm_s", [128, 8, 384], BF16)
        wbd = sb("wbd_s", [128, 32, 128], BF16)
        wcf = sb("wcf_s", [128, 16, 128], BF16)
        posf = sb("posf_s", [128, 2, 16], BF16)
        gqk = sb("gqk_s", [128, 384], F32)
        gkc = sb("gkc_s", [128, 64], F32)
        gret = sb("gret_s", [128, 256], F32)
        KS = sb("KS", [68, T], BF16)
        KW = sb("KW", [68, T], BF16)
        KC = sb("KC", [68, 512], BF16)
        VSW = sb("VSW", [128, NB, 2, 65], BF16)
        CV = sb("CV", [128, 4, 193], BF16)
        KCVC = sb("KCVC", [128, 16 + 2048], BF16)
        EALL = sb("EALL", [128, T], BF16)
        QXB = sb("QXB", [68, 512], BF16)
        MCA = sb("MCA", [128, 512], BF16); MWI = sb("MWI", [128, 512], BF16)
        MC = sb("MC", [128, 2, 2176], BF16)
        DEC = sb("DEC", [128, 4, 128], F32); XI = sb("XI", [128, 128], F32); ZETA = sb("ZETA", [128, 128], F32)
        GC = sb("GC", [128, 1], F32); BD = sb("BD", [128, 4, 128], BF16)
        IDN = sb("IDN", [128, 128], BF16)
        ONES1 = sb("ONES1", [1, 128], BF16)
        BIASR = sb("BIASR", [1, 128], BF16)
        CM05 = sb("CM05", [128, 8], F32)
        STATE = sb("STATE", [128, 64], F32)
        SBD = sb("SBD", [128, 4, 64], BF16)
        banks = [S.ps(f"pb{i}") for i in range(8)]
        SB_, OB, MB = banks[0:2], banks[2:6], banks[6:8]
        cnt = {"s": 0, "m": 0}

        def mbank():
            cnt["m"] += 1
            return MB[cnt["m"] % 2]

        def sbank():
            cnt["s"] += 1
            return SB_[cnt["s"] % 2]

        class Rot:
            def __init__(self, name, shape, dt, n):
                self.b = [sb(f"{name}{i}", shape, dt) for i in range(n)]
                self.i = 0

            def get(self):
                self.i += 1
                return self.b[self.i % len(self.b)]

        xbuf = Rot("xb", [128, 1024], F32, 2)
        junk = Rot("junk", [128, 1024], F32, 1)
        hb = Rot("hb", [128, 1024], BF16, 2)
        hT = Rot("hT", [128, 8, 512], BF16, 1)
        sm = Rot("sm", [128, 16], F32, 6)
        sqb = Rot("sqb", [128, 384], F32, 1)
        qkn = Rot("qkn", [128, 384], BF16, 2)
        t384 = Rot("t384", [128, 384], F32, 1)
        QT = [sb(f"QT{i}", [68, 512], BF16) for i in range(4)]
        RQT = Rot("RQT", [128, 512], BF16, 1); RKT = Rot("RKT", [128, 512], BF16, 1)
        RV = Rot("RV", [128, 4, 256], BF16, 1)
        G2 = Rot("G2", [128, 4, 256], F32, 1)
        KZ = Rot("KZ", [128, 4, 128], BF16, 1)
        GT = Rot("GT", [128, 4, 12], F32, 1)
        PT = Rot("PT", [128, 512], BF16, 3)
        NMT = Rot("NMT", [128, 512], BF16, 2)
        AMB = Rot("AMB", [128, 128], F32, 2)
        impb = Rot("impb", [128, 128], F32, 2)
        wk = Rot("wk", [128, 128], F32, 1)
        nmq = Rot("nmq", [128, 128], BF16, 1)
        m8 = Rot("m8", [128, 16], F32, 2)
        acc = Rot("acc", [128, 256], F32, 2)
        tmp256 = Rot("tmp256", [128, 256], F32, 2)
        fb = Rot("fb", [128, 16], F32, 4)
        nsab = Rot("nsab", [128, 256], BF16, 2)
        RQBD = Rot("RQBD", [128, 4, 128], BF16, 2)
        RQX = Rot("RQX", [128, 128], BF16, 2)
        IND = Rot("IND", [128, 4, 128], BF16, 2)
        sq256 = Rot("sq256", [128, 256], F32, 1)
        retb = Rot("retb", [128, 256], BF16, 2)
        red64 = Rot("red64", [128, 64], F32, 1)
        t4 = Rot("t4", [128, 4, 64], F32, 1)
        stg = Rot("stg", [64, 8, 512], BF16, 1)
        kcn = Rot("kcn", [128, 64], BF16, 1)
        kct = Rot("kct", [128, 64], F32, 1)

        def ld(eng, dst, src, **kw):
            S.dma(eng, dst.ap[:] if isinstance(dst, Buf) else dst, src, w=[dst] if isinstance(dst, Buf) else (), **kw)

        ld(SP, gam, D["gam"][:, :])
        S.dma(POOL, wtm.ap[:], D["wtm"].rearrange("(kc p) n -> p kc n", p=128), w=[wtm])
        S.dma(POOL, wfm.ap[:], D["wfm"].rearrange("(kc p) n -> p kc n", p=128), w=[wfm])
        S.dma(POOL, wbd.ap[:], D["wbd"][:, :, :], w=[wbd])
        S.dma(POOL, wcf.ap[:], D["wcf"][:, :, :], w=[wcf])
        S.dma(POOL, posf.ap[:], D["posf"][:, :, :], w=[posf])
        ld(SP, gqk, D["gqk"][:, :]); ld(SP, gkc, D["gkc"][:, :]); ld(SP, gret, D["gret"][:, :])
        S.dma(SP, KS.ap[64:68, :], D["kx"][:, :], w=[KS])
        S.dma(SP, KW.ap[64:68, :], D["kx"][:, :], w=[KW])
        S.dma(SP, KC.ap[64:68, :], D["kxc"][:, :], w=[KC])
        S.dma(SP, QXB.ap[64:68, :], D["qx"][:, :], w=[QXB])
        for q_ in QT:
            S.dma(SP, q_.ap[64:68, :], D["qx"][:, :], w=[q_])
        ld(SP, MCA, D["mcaus"][:, :]); ld(SP, MWI, D["mwin"][:, :]); ld(SP, MC, D["mc"][:, :, :])
        ld(SP, EALL, D["eall"][:, :])
        ld(SP, DEC, D["decT"][:, :, :]); ld(SP, XI, D["xi"][:, :]); ld(SP, ZETA, D["zeta"][:, :])
        ld(SP, GC, D["gc"][:, :]); ld(SP, BD, D["bd"][:, :, :]); ld(SP, IDN, D["ident"][:, :])
        S.op(POOL, "memset", ap=VSW.ap[:], constant=1.0, w=[VSW])
        S.op(POOL, "memset", ap=CV.ap[:], constant=0.0, w=[CV])
        S.dma(SP, CV.ap[:, :, 64:193], D["ov"][:, :, :], w=[CV])
        S.op(POOL, "memset", ap=KCVC.ap[:], constant=0.0, w=[KCVC])
        S.op(POOL, "memset", ap=KC.ap[0:64, :], constant=0.0, w=[KC])
        S.op(POOL, "memset", ap=ONES1.ap[:], constant=1.0, w=[ONES1])
        S.op(POOL, "memset", ap=CM05.ap[:], constant=-0.5, w=[CM05])
        S.op(POOL, "memset", ap=STATE.ap[:], constant=0.0, w=[STATE])
        S.op(POOL, "memset", ap=SBD.ap[:], constant=0.0, w=[SBD])
        S.op(DVE, "tensor_scalar", out=gqk.ap[:, 0:256], in0=gqk.ap[:, 0:256], scalar1=0.125, scalar2=None,
                                            op0=ALU.mult, r=[gqk], w=[gqk])
        b_ = mbank()
        for kv in range(2):
            for c in range(16):
                S.op(PE, "matmul", out=b_.ap[0:1, 64 * kv:64 * kv + 64], lhsT=posf.ap[:, kv, c:c + 1],
                                                         rhs=wcf.ap[:, c, 64 * kv:64 * kv + 64],
                                                         start=(c == 0 and kv == 0), stop=(c == 15), skip_group_check=True,
                     r=[posf, wcf], w=[b_])
        S.op(ACT, "activation", out=BIASR.ap[:], in_=b_.ap[0:1, 0:128], func=AF.Copy, r=[b_], w=[BIASR])

        def rstd_pow(dst, src, n, scale):
            pass

        for n in range(NT):
            hTn = hT.get()
            for j in range(4):
                xb_ = xbuf.get()
                r0 = 512 * n + 128 * j
                S.dma(SP, xb_.ap[:], x[r0:r0 + 128, :], w=[xb_])
                jk = junk.get(); st = sm.get()
                S.op(ACT, "activation", out=jk.ap[:], in_=xb_.ap[:], func=AF.Square,
                                                                        accum_out=st.ap[:, 0:1], r=[xb_], w=[jk, st])
                S.op(DVE, "tensor_scalar", out=st.ap[:, 1:2], in0=st.ap[:, 0:1], scalar1=1.0 / 1024,
                                                           scalar2=EPS, op0=ALU.mult, op1=ALU.add, r=[st], w=[st])
                S.op(POOL, "tensor_tensor", out=st.ap[:, 2:3], in0=st.ap[:, 1:2], in1=CM05.ap[:, 0:1],
                                                            op=ALU.pow, r=[st, CM05], w=[st])
                h_ = hb.get()
                S.op(DVE, "scalar_tensor_tensor",
                    out=h_.ap[:], in0=xb_.ap[:], scalar=st.ap[:, 2:3], in1=gam.ap[:], op0=ALU.mult, op1=ALU.mult,
                    r=[xb_, st, gam], w=[h_])
                pb = mbank()
                pbb = pb.ap[:].bitcast(BF16)
                for kc in range(8):
                    S.op(PE, "transpose", out=pbb[:, 128 * kc:128 * kc + 128],
                                                                          in_=h_.ap[:, 128 * kc:128 * kc + 128],
                                                                          identity=IDN.ap[:], r=[h_, IDN], w=[pb])
                S.op(ACT, "activation",
                    out=hTn.ap[:, :, 128 * j:128 * j + 128], in_=pbb.rearrange("p (k t) -> p k t", k=8), func=AF.Copy,
                    r=[pb], w=[hTn])
            rqt = RQT.get(); rkt = RKT.get()
            m = n % 4
            if m == 0 and n > 0:
                S.op(POOL, "tensor_copy", out=KCVC.ap[:, 0:16], in_=KCVC.ap[:, 2048:2064], r=[KCVC], w=[KCVC])
            for ci, dst in enumerate([None, rqt, rkt]):
                pb = mbank()
                for kc in range(8):
                    S.op(PE, "matmul", out=pb.ap[:, :], lhsT=wfm.ap[:, kc, 128 * ci:128 * ci + 128],
                                                                     rhs=hTn.ap[:, kc, :], start=(kc == 0), stop=(kc == 7),
                         r=[wfm, hTn], w=[pb])
                if ci == 0:
                    S.op(ACT, "activation", out=KCVC.ap[:, 16 + 512 * m:16 + 512 * m + 512],
                                                                 in_=pb.ap[:, :], func=AF.Copy, r=[pb], w=[KCVC])
                else:
                    S.op(DVE, "tensor_copy", out=dst.ap[:], in_=pb.ap[:, :], r=[pb], w=[dst])
            rv = RV.get(); g2 = G2.get(); kz = KZ.get(); gt = GT.get()
            for j in range(4):
                blk = 4 * n + j
                tok = slice(128 * j, 128 * j + 128)
                pa = mbank()
                for kc in range(8):
                    S.op(PE, "matmul", out=pa.ap[:, :], lhsT=hTn.ap[:, kc, tok],
                                                                       rhs=wtm.ap[:, kc, 0:512], start=(kc == 0),
                                                                       stop=(kc == 7), r=[hTn, wtm], w=[pa])
                sq = sqb.get(); st = sm.get()
                S.op(ACT, "activation", out=sq.ap[:], in_=pa.ap[:, 0:384], func=AF.Square,
                     r=[pa], w=[sq])
                S.op(ACT, "activation", out=VSW.ap[:, blk, :, 0:64],
                                                                 in_=pa.ap[:, 384:512].rearrange("p (a d) -> p a d", a=2),
                                                                 func=AF.Copy, r=[pa], w=[VSW])
                S.op(DVE, "tensor_reduce", out=st.ap[:, 0:6],
                                                                  in_=sq.ap[:].rearrange("p (a d) -> p a d", a=6),
                                                                  axis=AX.X, op=ALU.add, r=[sq], w=[st])
                S.op(DVE, "tensor_scalar", out=st.ap[:, 6:12], in0=st.ap[:, 0:6], scalar1=1.0 / 64,
                                                           scalar2=EPS, op0=ALU.mult, op1=ALU.add, r=[st], w=[st])
                st2 = sm.get()
                S.op(POOL, "tensor_tensor", out=st2.ap[:, 0:6], in0=st.ap[:, 6:12],
                                                                     in1=CM05.ap[:, 0:6], op=ALU.pow,
                     r=[st, CM05], w=[st2])
                tt = t384.get(); qk = qkn.get()
                S.op(DVE, "tensor_tensor",
                    out=tt.ap[:].rearrange("p (a d) -> p a d", a=6), in0=pa.ap[:, 0:384].rearrange("p (a d) -> p a d", a=6),
                    in1=st2.ap[:, 0:6].unsqueeze(2).broadcast_to([128, 6, 64]), op=ALU.mult, r=[pa, st2], w=[tt])
                S.op(POOL, "tensor_tensor", out=qk.ap[:], in0=tt.ap[:], in1=gqk.ap[:], op=ALU.mult,
                     r=[tt, gqk], w=[qk])
                pt_ = mbank()
                ptb = pt_.ap[:].bitcast(BF16)
                for a in range(6):
                    S.op(PE, "transpose", out=ptb[0:64, 128 * a:128 * a + 128],
                                                                        in_=qk.ap[:, 64 * a:64 * a + 64],
                                                                        identity=IDN.ap[:], r=[qk, IDN], w=[pt_])
                S.op(ACT, "activation", out=QT[j].ap[0:64, :], in_=ptb[0:64, 0:512], func=AF.Copy,
                     r=[pt_], w=[QT[j]])
                S.op(DVE, "tensor_copy", out=KS.ap[0:64, 128 * blk:128 * blk + 128],
                                                                    in_=ptb[0:64, 512:640], r=[pt_], w=[KS])
                S.op(DVE, "tensor_copy", out=KW.ap[0:64, 128 * blk:128 * blk + 128],
                                                                    in_=ptb[0:64, 640:768], r=[pt_], w=[KW])
                S.op(DVE, "tensor_scalar", out=QT[j].ap[64:65, :], in0=QXB.ap[64:65, :],
                                                                  scalar1=float(blk), scalar2=None, op0=ALU.mult,
                     r=[QXB], w=[QT[j]])
                pbk = mbank()
                for kc in range(8):
                    S.op(PE, "matmul", out=pbk.ap[:, :], lhsT=hTn.ap[:, kc, tok],
                                                                         rhs=wtm.ap[:, kc, 512:1024], start=(kc == 0),
                                                                         stop=(kc == 7), r=[hTn, wtm], w=[pbk])
                S.op(DVE, "tensor_copy", out=rv.ap[:, j, :], in_=pbk.ap[:, 0:256], r=[pbk], w=[rv])
                S.op(ACT, "activation", out=g2.ap[:, j, :], in_=pbk.ap[:, 256:512], func=AF.Silu,
                     r=[pbk], w=[g2])
                S.op(POOL, "tensor_tensor", out=g2.ap[:, j, :], in0=g2.ap[:, j, :], in1=gret.ap[:],
                                                          op=ALU.mult, r=[g2, gret], w=[g2])
                pc = mbank()
                for kc in range(8):
                    S.op(PE, "matmul", out=pc.ap[:, 0:140], lhsT=hTn.ap[:, kc, tok],
                                                                       rhs=wtm.ap[:, kc, 1024:1164], start=(kc == 0),
                                                                       stop=(kc == 7), r=[hTn, wtm], w=[pc])
                S.op(DVE, "tensor_tensor", out=kz.ap[:, j, :], in0=pc.ap[:, 0:128], in1=ZETA.ap[:],
                                                                op=ALU.mult, r=[pc, ZETA], w=[kz])
                S.op(ACT, "activation", out=gt.ap[:, j, :], in_=pc.ap[:, 128:140], func=AF.Sigmoid,
                     r=[pc], w=[gt])
            ct = n // 4
            pcm = mbank()
            S.op(PE, "matmul", out=pcm.ap[:, 0:128], lhsT=ONES1.ap[0:1, :], rhs=BIASR.ap[0:1, :],
                                                 start=True, stop=False, r=[ONES1, BIASR], w=[pcm])
            for l in range(32):
                S.op(PE, "matmul", out=pcm.ap[:, 0:128], lhsT=KCVC.ap[:, l:l + 2033:16],
                                                          rhs=wbd.ap[:, l, :], start=False, stop=(l == 31),
                     r=[KCVC, wbd], w=[pcm])
            S.op(ACT, "activation", out=CV.ap[:, ct, 0:64], in_=pcm.ap[:, 64:128], func=AF.Copy,
                 r=[pcm], w=[CV])
            kt_ = kct.get(); st = sm.get()
            S.op(ACT, "activation", out=kt_.ap[:], in_=pcm.ap[:, 0:64], func=AF.Square,
                                                                      accum_out=st.ap[:, 0:1], r=[pcm], w=[kt_, st])
            S.op(DVE, "tensor_scalar", out=st.ap[:, 1:2], in0=st.ap[:, 0:1], scalar1=1.0 / 64, scalar2=EPS,
                                                       op0=ALU.mult, op1=ALU.add, r=[st], w=[st])
            S.op(POOL, "tensor_tensor", out=st.ap[:, 2:3], in0=st.ap[:, 1:2], in1=CM05.ap[:, 0:1],
                                                        op=ALU.pow, r=[st, CM05], w=[st])
            kn = kcn.get()
            S.op(DVE, "scalar_tensor_tensor", out=kn.ap[:], in0=pcm.ap[:, 0:64],
                                                                              scalar=st.ap[:, 2:3], in1=gkc.ap[:],
                                                                              op0=ALU.mult, op1=ALU.mult,
                 r=[pcm, st, gkc], w=[kn])
            pk = mbank()
            pkb = pk.ap[:].bitcast(BF16)
            S.op(PE, "transpose", out=pkb[0:64, 0:128], in_=kn.ap[:, 0:64], identity=IDN.ap[:],
                 r=[kn, IDN], w=[pk])
            S.op(DVE, "tensor_copy", out=KC.ap[0:64, 128 * ct:128 * ct + 128], in_=pkb[0:64, 0:128],
                 r=[pk], w=[KC])

            sg = stg.get()
            for j in range(4):
                i = 4 * n + j
                q0 = 128 * i
                qt = QT[j]
                jobs = []
                nct = (128 * i + 112) // 2048 + 1
                O_c = [OB[0], OB[1]]
                O_w = OB[2]
                O_s = OB[3]
                for c in range(nct):
                    jobs.append(("c", c, c == nct - 1))
                for kt in range(max(0, i - 4), i + 1):
                    jobs.append(("w", kt, kt == i or kt == i - 4))
                for kt in range(0, i + 1):
                    jobs.append(("s", kt, kt == i))
                first = {"c": True, "w": True, "s": True}
                nm_holder = {}

                def emit_qk(job):
                    br, kt, special = job
                    sbk = sbank()
                    if br == "c":
                        S.op(PE, "matmul", out=sbk.ap[:, :], lhsT=KC.ap[0:68, 128 * kt:128 * kt + 128], rhs=qt.ap[0:68, :],
                                                    start=True, stop=not special, r=[KC, qt], w=[sbk])
                        if special:
                            o = q0 - 2048 * kt
                            var = 0 if kt == 0 else 1
                            for h in range(4):
                                S.op(PE, "matmul", out=sbk.ap[:, 128 * h:128 * h + 128], lhsT=IDN.ap[:],
                                                                 rhs=MC.ap[:, var, o:o + 128], start=False, stop=(h == 3),
                                     r=[IDN, MC], w=[sbk])
                    elif br == "w":
                        S.op(PE, "matmul", out=sbk.ap[:, :], lhsT=KW.ap[0:68, 128 * kt:128 * kt + 128], rhs=qt.ap[0:68, :],
                                                    start=True, stop=not special, r=[KW, qt], w=[sbk])
                        if special:
                            msk = MCA if kt == i else MWI
                            S.op(PE, "matmul", out=sbk.ap[:, :], lhsT=IDN.ap[:], rhs=msk.ap[:], start=False, stop=True,
                                 r=[IDN, msk], w=[sbk])
                    else:
                        nm = nm_holder["nm"]
                        S.op(PE, "matmul", out=sbk.ap[:, :], lhsT=KS.ap[0:68, 128 * kt:128 * kt + 128], rhs=qt.ap[0:68, :],
                                                    start=True, stop=False, r=[KS, qt], w=[sbk])
                        S.op(PE, "matmul", out=sbk.ap[:, :], lhsT=EALL.ap[:, 128 * kt:128 * kt + 128], rhs=nm.ap[:],
                                                    start=False, stop=not special, r=[EALL, nm], w=[sbk])
                        if special:
                            S.op(PE, "matmul", out=sbk.ap[:, :], lhsT=IDN.ap[:], rhs=MCA.ap[:], start=False, stop=True,
                                 r=[IDN, MCA], w=[sbk])
                    return sbk

                def emit_exp_pv(job, sbk):
                    br, kt, special = job
                    p = PT.get()
                    S.op(ACT, "activation", out=p.ap[:], in_=sbk.ap[:, :], func=AF.Exp, r=[sbk], w=[p])
                    fst = first[br]
                    first[br] = False
                    if br == "c":
                        for h in range(4):
                            ob = O_c[h // 2]
                            c0 = 193 * (h % 2)
                            S.op(PE, "matmul", out=
                                ob.ap[:, c0:c0 + 193], lhsT=p.ap[:, 128 * h:128 * h + 128], rhs=CV.ap[:, kt, :],
                                start=(fst and h % 2 == 0), stop=False, skip_group_check=True, r=[p, CV], w=[ob])
                    else:
                        ob = O_w if br == "w" else O_s
                        a = 1 if br == "w" else 0
                        for h in range(4):
                            S.op(PE, "matmul", out=ob.ap[:, 65 * h:65 * h + 65], lhsT=p.ap[:, 128 * h:128 * h + 128],
                                                             rhs=VSW.ap[:, kt, a, :], start=(fst and h == 0), stop=False,
                                                             skip_group_check=True, r=[p, VSW], w=[ob])

                ac = acc.get()
                gtj = gt.ap[:, j, :].rearrange("p (r b) -> p r b", b=3)

                def epilogue(br):
                    f = fb.get()
                    bi = {"c": 0, "s": 1, "w": 2}[br]
                    if br == "c":
                        srcs = [(O_c[0], 0), (O_c[0], 193), (O_c[1], 0), (O_c[1], 193)]
                        for h, (ob, c0) in enumerate(srcs):
                            S.op(DVE, "tensor_scalar",
                                out=f.ap[:, h:h + 1], in0=ob.ap[:, c0 + 64:c0 + 65], scalar1=1e-30, scalar2=None, op0=ALU.add,
                                r=[ob], w=[f])
                    else:
                        ob = O_w if br == "w" else O_s
                        S.op(DVE, "tensor_scalar",
                            out=f.ap[:, 0:4], in0=ob.ap[:, 0:260].rearrange("p (h c) -> p h c", c=65)[:, :, 64],
                            scalar1=1e-30, scalar2=None, op0=ALU.add, r=[ob], w=[f])
                    S.op(DVE, "reciprocal", out=f.ap[:, 4:8], in_=f.ap[:, 0:4], r=[f], w=[f])
                    S.op(DVE, "tensor_tensor", out=f.ap[:, 8:12], in0=f.ap[:, 4:8], in1=gtj[:, :, bi], op=ALU.mult,
                         r=[f, gt], w=[f])
                    dst = ac if br == "c" else tmp256.get()
                    if br == "c":
                        for h, (ob, c0) in enumerate(srcs):
                            S.op(DVE, "tensor_scalar",
                                out=dst.ap[:, 64 * h:64 * h + 64], in0=ob.ap[:, c0:c0 + 64], scalar1=f.ap[:, 8 + h:9 + h],
                                scalar2=None, op0=ALU.mult, r=[ob, f], w=[dst])
                    else:
                        S.op(DVE, "tensor_tensor",
                            out=dst.ap[:].rearrange("p (h d) -> p h d", h=4),
                            in0=ob.ap[:, 0:260].rearrange("p (h c) -> p h c", c=65)[:, :, 0:64],
                            in1=f.ap[:, 8:12].unsqueeze(2).broadcast_to([128, 4, 64]), op=ALU.mult, r=[ob, f], w=[dst])
                        S.op(POOL, "tensor_tensor", out=ac.ap[:], in0=ac.ap[:], in1=dst.ap[:], op=ALU.add,
                             r=[ac, dst], w=[ac])
                    return f

                def selection(f):
                    am_ = AMB.get()
                    S.dma(SP, am_.ap[:], D["am"][i, :, :], w=[am_])
                    im = impb.get()
                    srcs = [(O_c[0], 0), (O_c[0], 193), (O_c[1], 0), (O_c[1], 193)]
                    for h, (ob, c0) in enumerate(srcs):
                        prev = am_ if h == 0 else im
                        S.op(DVE, "scalar_tensor_tensor",
                            out=im.ap[:], in0=ob.ap[:, c0 + 65:c0 + 193], scalar=f.ap[:, 4 + h:5 + h], in1=prev.ap[:],
                            op0=ALU.mult, op1=ALU.add, r=[ob, f, prev], w=[im])
                    mm = m8.get(); w_ = wk.get()
                    S.op(DVE, "max", out=mm.ap[:, 0:8], in_=im.ap[:], r=[im], w=[mm])
                    S.op(DVE, "match_replace", out=w_.ap[:], in_to_replace=mm.ap[:, 0:8], in_values=im.ap[:],
                                                        imm_value=-3e38, r=[im, mm], w=[w_])
                    S.op(DVE, "max", out=mm.ap[:, 8:16], in_=w_.ap[:], r=[w_], w=[mm])
                    nq = nmq.get()
                    S.op(DVE, "tensor_scalar", out=nq.ap[:], in0=im.ap[:], scalar1=mm.ap[:, 15:16], scalar2=NEGM,
                                                        op0=ALU.is_lt, op1=ALU.mult, r=[im, mm], w=[nq])
                    nm_holder["nq"] = nq

                def selection2():
                    nq = nm_holder["nq"]
                    pb = mbank()
                    pbb = pb.ap[:].bitcast(BF16)
                    S.op(PE, "transpose", out=pbb[:, 0:128], in_=nq.ap[:], identity=IDN.ap[:], r=[nq, IDN], w=[pb])
                    nm = NMT.get()
                    S.op(ACT, "activation", out=nm.ap[:].rearrange("p (h q) -> p h q", h=4),
                         in_=pbb[:, 0:128].unsqueeze(1).broadcast_to([128, 4, 128]), func=AF.Copy, r=[pb], w=[nm])
                    nm_holder["nm"] = nm

                pend = None
                last_br = None
                k = 0
                cur_s = emit_qk(jobs[0])
                while k < len(jobs):
                    job = jobs[k]
                    nxt = None
                    if k + 1 < len(jobs) and not (jobs[k + 1][0] == "s" and "nm" not in nm_holder):
                        nxt = emit_qk(jobs[k + 1])
                    emit_exp_pv(job, cur_s)
                    br = job[0]
                    if k + 1 == len(jobs) or jobs[k + 1][0] != br:
                        f = epilogue(br)
                        if br == "c":
                            selection(f)
                    if k + 1 < len(jobs) and nxt is None:
                        if "nm" not in nm_holder:
                            selection2()
                        nxt = emit_qk(jobs[k + 1])
                    cur_s = nxt
                    k += 1
                nb_ = nsab.get()
                S.op(ACT, "activation", out=nb_.ap[:], in_=ac.ap[:], func=AF.Copy, r=[ac], w=[nb_])
                pb = mbank()
                pbb = pb.ap[:].bitcast(BF16)
                for h in range(4):
                    S.op(PE, "transpose", out=pbb[0:64, 128 * h:128 * h + 128], in_=nb_.ap[:, 64 * h:64 * h + 64],
                                                        identity=IDN.ap[:], r=[nb_, IDN], w=[pb])
                S.op(DVE, "tensor_copy", out=sg.ap[:, 0:4, 128 * j:128 * j + 128],
                                                  in_=pbb[0:64, 0:512].rearrange("p (h q) -> p h q", h=4), r=[pb], w=[sg])

                tok = slice(128 * j, 128 * j + 128)
                rqbd = RQBD.get(); rqx = RQX.get()
                S.op(POOL, "tensor_tensor", out=rqbd.ap[:], in0=rqt.ap[:, tok].unsqueeze(1).broadcast_to([128, 4, 128]),
                                                     in1=BD.ap[:], op=ALU.mult, r=[rqt, BD], w=[rqbd])
                S.op(POOL, "tensor_tensor", out=rqx.ap[:], in0=rqt.ap[:, tok], in1=XI.ap[:], op=ALU.mult,
                     r=[rqt, XI], w=[rqx])
                pin = mbank()
                S.op(PE, "matmul", out=pin.ap[:, :], lhsT=rkt.ap[:, tok], rhs=rqbd.ap[:].rearrange("p h i -> p (h i)"),
                                            start=True, stop=True, r=[rkt, rqbd], w=[pin])
                ind = IND.get()
                S.op(DVE, "tensor_tensor", out=ind.ap[:].rearrange("p h i -> p (h i)"), in0=pin.ap[:, :],
                                                    in1=DEC.ap[:].rearrange("p h i -> p (h i)"), op=ALU.mult,
                     r=[pin, DEC], w=[ind])
                po = mbank()
                S.op(PE, "matmul", out=po.ap[:, 0:256], lhsT=rqx.ap[:], rhs=SBD.ap[:].rearrange("p h e -> p (h e)"),
                                            start=True, stop=False, r=[rqx, SBD], w=[po])
                for h in range(4):
                    S.op(PE, "matmul", out=po.ap[:, 64 * h:64 * h + 64], lhsT=ind.ap[:, h, :],
                                                     rhs=rv.ap[:, j, 64 * h:64 * h + 64], start=False, stop=(h == 3),
                         r=[ind, rv], w=[po])
                pkv = mbank()
                S.op(PE, "matmul", out=pkv.ap[:, 0:256], lhsT=kz.ap[:, j, :], rhs=rv.ap[:, j, :], start=True, stop=True,
                     r=[kz, rv], w=[pkv])
                sq = sq256.get(); st = sm.get()
                S.op(ACT, "activation", out=sq.ap[:], in_=po.ap[:, 0:256], func=AF.Square, r=[po], w=[sq])
                S.op(DVE, "tensor_reduce", out=st.ap[:, 0:4], in_=sq.ap[:].rearrange("p (h d) -> p h d", h=4),
                                                    axis=AX.X, op=ALU.add, r=[sq], w=[st])
                S.op(DVE, "tensor_scalar", out=st.ap[:, 4:8], in0=st.ap[:, 0:4], scalar1=1.0 / 64, scalar2=EPS,
                                                    op0=ALU.mult, op1=ALU.add, r=[st], w=[st])
                S.op(POOL, "tensor_tensor", out=st.ap[:, 8:12], in0=st.ap[:, 4:8], in1=CM05.ap[:, 0:4], op=ALU.pow,
                     r=[st, CM05], w=[st])
                tm_ = tmp256.get()
                S.op(DVE, "tensor_tensor", out=tm_.ap[:].rearrange("p (h d) -> p h d", h=4),
                                                    in0=po.ap[:, 0:256].rearrange("p (h d) -> p h d", h=4),
                                                    in1=st.ap[:, 8:12].unsqueeze(2).broadcast_to([128, 4, 64]), op=ALU.mult,
                     r=[po, st], w=[tm_])
                rb = retb.get()
                S.op(POOL, "tensor_tensor", out=rb.ap[:], in0=tm_.ap[:], in1=g2.ap[:, j, :], op=ALU.mult,
                     r=[tm_, g2], w=[rb])
                pb2 = mbank()
                pbb2 = pb2.ap[:].bitcast(BF16)
                for h in range(4):
                    S.op(PE, "transpose", out=pbb2[0:64, 128 * h:128 * h + 128], in_=rb.ap[:, 64 * h:64 * h + 64],
                                                        identity=IDN.ap[:], r=[rb, IDN], w=[pb2])
                S.op(DVE, "tensor_copy", out=sg.ap[:, 4:8, 128 * j:128 * j + 128],
                                                  in_=pbb2[0:64, 0:512].rearrange("p (h q) -> p h q", h=4), r=[pb2], w=[sg])
                t4_ = t4.get(); r64 = red64.get()
                S.op(DVE, "tensor_tensor", out=t4_.ap[:], in0=pkv.ap[:, 0:256].rearrange("p (h e) -> p h e", h=4),
                                                    in1=BD.ap[:, :, 0:64], op=ALU.mult, r=[pkv, BD], w=[t4_])
                S.op(DVE, "tensor_reduce", out=r64.ap[:], in_=t4_.ap[:].rearrange("p h e -> p e h"), axis=AX.X,
                                                    op=ALU.add, r=[t4_], w=[r64])
                S.op(DVE, "scalar_tensor_tensor", out=STATE.ap[:], in0=STATE.ap[:], scalar=GC.ap[:, 0:1], in1=r64.ap[:],
                                                           op0=ALU.mult, op1=ALU.add, r=[STATE, GC, r64], w=[STATE])
                S.op(POOL, "tensor_tensor", out=SBD.ap[:], in0=STATE.ap[:].unsqueeze(1).broadcast_to([128, 4, 64]),
                                                     in1=BD.ap[:, :, 0:64], op=ALU.mult, r=[STATE, BD], w=[SBD])
            S.dma(POOL, mix_nsa[:, :, 512 * n:512 * n + 512].rearrange("c p t -> p c t"), sg.ap[:, 0:4, :], r=[sg])
            S.dma(POOL, mix_ret[:, :, 512 * n:512 * n + 512].rearrange("c p t -> p c t"), sg.ap[:, 4:8, :], r=[sg])
        S.emit()
        print("mix ops:", {e: len(S.ops[e]) for e in ENGS}, "sems:", S.nsem)


def build_mix(T):
    nc = bass.Bass("TRN2", target_bir_lowering=False)
    D = {}
    mix_decl(nc, D, T, "", "", True)
    x = nc.dram_tensor("x", [T, 1024], F32, kind="ExternalInput").ap()
    mixT = nc.dram_tensor("mixT", [8, 64, T], BF16, kind="ExternalOutput").ap()
    emit_mix(nc, D, T, x, (mixT[0:4], mixT[4:8]), "", "")
    return nc


DFF = 2816
NFC = DFF // 128
FG = 2
NG = NFC // FG


def ffn_decl(nc, D, NE, pf):
    def din(name, shape, dt=F32):
        if name not in D:
            D[name] = nc.dram_tensor(name, list(shape), dt, kind="ExternalInput").ap()
    din(pf + "wout", [1024, 1024]); din(pf + "gam", [128, 1024])
    din(pf + "wg", [NE, 1024, DFF]); din(pf + "wu", [NE, 1024, DFF]); din(pf + "wd", [NE, DFF, 1024])
    din("identf", [128, 128])
    if NE > 1:
        din(pf + "router", [1024, 8]); din(pf + "rb", [128, 8])


def emit_ffn(nc, D0, TOK, NE, x_srcs, mix_srcs, y, pf, sel=None, TS=2048, tag=""):
    moe = NE > 1
    TS = min(TS, TOK)
    NST = TOK // TS
    NSUB = TS // 128
    NTT = TS // 512
    D = dict(D0)
    for k_ in ["wout", "gam", "wg", "wu", "wd", "router", "rb"]:
        if pf + k_ in D0:
            D[k_] = D0[pf + k_]
    blend = len(x_srcs) == 2

    with ExitStack() as es:
        S = Sched(nc, es, tag)
        sb = S.sb
        gam = sb("gam_s", [128, 1024], F32)
        IDF = sb("IDF", [128, 128], F32)
        CM05 = sb("CM05", [128, 8], F32)
        wout = sb("wout_s", [128, 8, 1024], BF16)
        yacc = sb("yacc", [128, NSUB, 1024], F32)
        h2T = sb("h2T", [128, 8, TS], BF16)
        cmb = sb("cmb", [128, NSUB, 8], F32)
        if moe:
            rt = sb("rt_s", [128, 8, 8], F32)
            rb = sb("rb_s", [128, 8], F32)
        banks = [S.ps(f"pb{i}") for i in range(8)]

        class Rot:
            def __init__(self, name, shape, dt, n):
                self.b = [sb(f"{name}{i}", shape, dt) for i in range(n)]
                self.i = 0

            def get(self):
                self.i += 1
                return self.b[self.i % len(self.b)]

        xbuf = Rot("xb", [128, 1024], F32, 2)
        if blend:
            xbuf2 = Rot("xb2", [128, 1024], F32, 1)
            mxb2 = Rot("mxb2", [128, 8, 128], BF16, 1)
            SEL = sb("SEL", [128, 2], F32)
            S.dma(SP, SEL.ap[:], D0[sel][:, :], w=[SEL])
        mxb = Rot("mxb", [128, 8, 128], BF16, 2)
        h2f = Rot("h2f", [128, 1024], F32, 1)
        h2tf = Rot("h2tf", [128, 8, 128], F32, 1)
        sm = Rot("sm", [128, 16], F32, 4)
        lgb = Rot("lgb", [128, 8], F32, 2)
        m8 = Rot("m8", [128, 8], F32, 2)
        c1b = Rot("c1b", [128, 8], F32, 2)
        WG = Rot("WG", [128, 8, FG * 128], BF16, 3)
        WU = Rot("WU", [128, 8, FG * 128], BF16, 3)
        WD = Rot("WD", [128, FG, 1024], BF16, 3)
        AT = Rot("AT", [128, FG, TS], BF16, 2)
        sgb = Rot("sgb", [128, 512], F32, 2)

        S.dma(SP, gam.ap[:], D["gam"][:, :], w=[gam])
        S.dma(SP, IDF.ap[:], D["identf"][:, :], w=[IDF])
        S.dma(POOL, wout.ap[:], D["wout"].rearrange("(c p) n -> p c n", p=128), w=[wout])
        S.op(POOL, "memset", ap=CM05.ap[:], constant=-0.5, w=[CM05])
        S.op(POOL, "memset", ap=cmb.ap[:], constant=1.0, w=[cmb])
        if moe:
            S.dma(SP, rt.ap[:], D["router"].rearrange("(kc p) n -> p kc n", p=128), w=[rt])
            S.dma(SP, rb.ap[:], D["rb"][:, :], w=[rb])

        for stile in range(NST):
            t0 = stile * TS
            for s in range(NSUB):
                r0 = t0 + 128 * s
                xb_ = xbuf.get(); mx = mxb.get()
                S.dma(SP, xb_.ap[:], x_srcs[0][r0:r0 + 128, :], w=[xb_])
                S.dma(SP, mx.ap[:], mix_srcs[0][:, :, r0:r0 + 128].rearrange("c p t -> p c t"), w=[mx])
                if blend:
                    xb2 = xbuf2.get(); mx2 = mxb2.get()
                    S.dma(SP, xb2.ap[:], x_srcs[1][r0:r0 + 128, :], w=[xb2])
                    S.dma(SP, mx2.ap[:], mix_srcs[1][:, :, r0:r0 + 128].rearrange("c p t -> p c t"), w=[mx2])
                    S.op(POOL, "tensor_scalar", out=xb_.ap[:], in0=xb_.ap[:], scalar1=SEL.ap[:, 0:1], scalar2=None,
                         op0=ALU.mult, r=[xb_, SEL], w=[xb_])
                    S.op(DVE, "scalar_tensor_tensor", out=xb_.ap[:], in0=xb2.ap[:], scalar=SEL.ap[:, 1:2], in1=xb_.ap[:],
                         op0=ALU.mult, op1=ALU.add, r=[xb2, SEL, xb_], w=[xb_])
                    S.op(POOL, "tensor_scalar", out=mx.ap[:], in0=mx.ap[:], scalar1=SEL.ap[:, 0:1], scalar2=None,
                         op0=ALU.mult, r=[mx, SEL], w=[mx])
                    S.op(DVE, "scalar_tensor_tensor", out=mx.ap[:], in0=mx2.ap[:], scalar=SEL.ap[:, 1:2], in1=mx.ap[:],
                         op0=ALU.mult, op1=ALU.add, r=[mx2, SEL, mx], w=[mx])
                for half in range(2):
                    pb = banks[half]
                    for c in range(8):
                        S.op(PE, "matmul", out=pb.ap[:, :], lhsT=mx.ap[:, c, :], rhs=wout.ap[:, c, 512 * half:512 * half + 512],
                             start=(c == 0), stop=(c == 7), r=[mx, wout], w=[pb])
                    S.op(DVE, "tensor_tensor", out=yacc.ap[:, s, 512 * half:512 * half + 512], in0=pb.ap[:, :],
                         in1=xb_.ap[:, 512 * half:512 * half + 512], op=ALU.add, r=[pb, xb_], w=[yacc])
                hf = h2f.get(); st = sm.get()
                S.op(ACT, "activation", out=hf.ap[:], in_=yacc.ap[:, s, :], func=AF.Square, accum_out=st.ap[:, 0:1],
                     r=[yacc], w=[hf, st])
                S.op(DVE, "tensor_scalar", out=st.ap[:, 1:2], in0=st.ap[:, 0:1], scalar1=1.0 / 1024, scalar2=EPS,
                     op0=ALU.mult, op1=ALU.add, r=[st], w=[st])
                S.op(POOL, "tensor_tensor", out=st.ap[:, 2:3], in0=st.ap[:, 1:2], in1=CM05.ap[:, 0:1], op=ALU.pow,
                     r=[st, CM05], w=[st])
                S.op(DVE, "scalar_tensor_tensor", out=hf.ap[:], in0=yacc.ap[:, s, :], scalar=st.ap[:, 2:3], in1=gam.ap[:],
                     op0=ALU.mult, op1=ALU.mult, r=[yacc, st, gam], w=[hf])
                for hh in range(2):
                    pb = banks[2 + hh]
                    for k4 in range(4):
                        kc = 4 * hh + k4
                        S.op(PE, "transpose", out=pb.ap[:, 128 * k4:128 * k4 + 128], in_=hf.ap[:, 128 * kc:128 * kc + 128],
                             identity=IDF.ap[:], r=[hf, IDF], w=[pb])
                    S.op(ACT, "activation", out=h2T.ap[:, 4 * hh:4 * hh + 4, 128 * s:128 * s + 128],
                         in_=pb.ap[:, :].rearrange("p (k t) -> p k t", k=4), func=AF.Copy, r=[pb], w=[h2T])
                    if moe:
                        if hh == 0:
                            htf = h2tf.get()
                        S.op(DVE, "tensor_copy", out=htf.ap[:, 4 * hh:4 * hh + 4, :],
                             in_=pb.ap[:, :].rearrange("p (k t) -> p k t", k=4), r=[pb], w=[htf])
                if moe:
                    pr = banks[4]
                    for kc in range(8):
                        S.op(PE, "matmul", out=pr.ap[:, 0:8], lhsT=htf.ap[:, kc, :], rhs=rt.ap[:, kc, :], start=(kc == 0),
                             stop=(kc == 7), r=[htf, rt], w=[pr])
                    lg = lgb.get(); mm = m8.get(); c1 = c1b.get(); st2 = sm.get()
                    S.op(DVE, "tensor_tensor", out=lg.ap[:], in0=pr.ap[:, 0:8], in1=rb.ap[:], op=ALU.add, r=[pr, rb], w=[lg])
                    S.op(DVE, "max", out=mm.ap[:], in_=lg.ap[:], r=[lg], w=[mm])
                    S.op(DVE, "tensor_tensor", out=st2.ap[:, 0:1], in0=mm.ap[:, 0:1], in1=mm.ap[:, 1:2], op=ALU.subtract,
                         r=[mm], w=[st2])
                    S.op(ACT, "activation", out=st2.ap[:, 1:2], in_=st2.ap[:, 0:1], func=AF.Sigmoid, r=[st2], w=[st2])
                    S.op(DVE, "tensor_scalar", out=st2.ap[:, 2:3], in0=st2.ap[:, 1:2], scalar1=-1.0, scalar2=1.0,
                         op0=ALU.mult, op1=ALU.add, r=[st2], w=[st2])
                    S.op(DVE, "tensor_scalar", out=c1.ap[:], in0=lg.ap[:], scalar1=mm.ap[:, 0:1], scalar2=st2.ap[:, 1:2],
                         op0=ALU.is_equal, op1=ALU.mult, r=[lg, mm, st2], w=[c1])
                    S.op(DVE, "tensor_scalar", out=cmb.ap[:, s, :], in0=lg.ap[:], scalar1=mm.ap[:, 1:2], scalar2=st2.ap[:, 2:3],
                         op0=ALU.is_equal, op1=ALU.mult, r=[lg, mm, st2], w=[cmb])
                    S.op(DVE, "tensor_tensor", out=cmb.ap[:, s, :], in0=cmb.ap[:, s, :], in1=c1.ap[:], op=ALU.add,
                         r=[cmb, c1], w=[cmb])
            groups = [(e, fg) for e in range(NE) for fg in range(NG)]

            def load(e, fg):
                wg_ = WG.get(); wu_ = WU.get(); wd_ = WD.get()
                c0 = fg * FG * 128
                S.dma(POOL, wg_.ap[:], D["wg"][e].rearrange("(kc p) n -> p kc n", p=128)[:, :, c0:c0 + FG * 128], w=[wg_])
                S.dma(POOL, wu_.ap[:], D["wu"][e].rearrange("(kc p) n -> p kc n", p=128)[:, :, c0:c0 + FG * 128], w=[wu_])
                S.dma(POOL, wd_.ap[:], D["wd"][e].rearrange("(fc p) n -> p fc n", p=128)[:, fg * FG:fg * FG + FG, :], w=[wd_])
                return wg_, wu_, wd_

            def gu(w3):
                wg_, wu_, wd_ = w3
                at = AT.get()
                for tt in range(NTT):
                    for fc in range(FG):
                        pg = banks[(tt * FG + fc) % 2]
                        pu = banks[2 + (tt * FG + fc) % 2]
                        for kc in range(8):
                            S.op(PE, "matmul", out=pg.ap[:, :], lhsT=wg_.ap[:, kc, 128 * fc:128 * fc + 128],
                                 rhs=h2T.ap[:, kc, 512 * tt:512 * tt + 512], start=(kc == 0), stop=(kc == 7), r=[wg_, h2T], w=[pg])
                        for kc in range(8):
                            S.op(PE, "matmul", out=pu.ap[:, :], lhsT=wu_.ap[:, kc, 128 * fc:128 * fc + 128],
                                 rhs=h2T.ap[:, kc, 512 * tt:512 * tt + 512], start=(kc == 0), stop=(kc == 7), r=[wu_, h2T], w=[pu])
                        sg = sgb.get()
                        S.op(ACT, "activation", out=sg.ap[:], in_=pg.ap[:, :], func=AF.Silu, r=[pg], w=[sg])
                        S.op(DVE, "tensor_tensor", out=at.ap[:, fc, 512 * tt:512 * tt + 512], in0=pu.ap[:, :], in1=sg.ap[:],
                             op=ALU.mult, r=[pu, sg], w=[at])
                return at

            dcnt = [0]

            def down(e, w3, at):
                wd_ = w3[2]
                for s in range(NSUB):
                    for half in range(2):
                        dcnt[0] += 1
                        pd = banks[4 + dcnt[0] % 4]
                        for fc in range(FG):
                            S.op(PE, "matmul", out=pd.ap[:, :], lhsT=at.ap[:, fc, 128 * s:128 * s + 128],
                                 rhs=wd_.ap[:, fc, 512 * half:512 * half + 512], start=(fc == 0), stop=(fc == FG - 1),
                                 r=[at, wd_], w=[pd])
                        S.op(DVE, "scalar_tensor_tensor", out=yacc.ap[:, s, 512 * half:512 * half + 512], in0=pd.ap[:, :],
                             scalar=cmb.ap[:, s, e:e + 1], in1=yacc.ap[:, s, 512 * half:512 * half + 512], op0=ALU.mult,
                             op1=ALU.add, r=[pd, cmb, yacc], w=[yacc])

            w_cur = load(*groups[0])
            w_nxt = load(*groups[1]) if len(groups) > 1 else None
            at_prev = None
            prev = None
            for gi, (e, fg) in enumerate(groups):
                at = gu(w_cur)
                if prev is not None:
                    down(*prev)
                prev = (e, w_cur, at)
                w_cur = w_nxt
                if gi + 2 < len(groups):
                    w_nxt = load(*groups[gi + 2])
            down(*prev)
            for s in range(NSUB):
                r0 = t0 + 128 * s
                S.dma(SP, y[r0:r0 + 128, :], yacc.ap[:, s, :], r=[yacc])
        S.emit()
        print("ffn ops:", {e: len(S.ops[e]) for e in ENGS}, "sems:", S.nsem)


def build_ffn(TOK, NE, TS=2048):
    nc = bass.Bass("TRN2", target_bir_lowering=False)
    D = {}
    ffn_decl(nc, D, NE, "")
    x = nc.dram_tensor("x", [TOK, 1024], F32, kind="ExternalInput").ap()
    mixf = nc.dram_tensor("mixf", [8, 128, TOK], BF16, kind="ExternalInput").ap()
    y = nc.dram_tensor("y", [TOK, 1024], F32, kind="ExternalOutput").ap()
    emit_ffn(nc, D, TOK, NE, [x], [mixf], y, "", TS=TS)
    return nc


def build_fused(T):
    TOK = T // 2
    nc = bass.Bass("TRN2", target_bir_lowering=False)
    D = {}
    x = nc.dram_tensor("x", [T, 1024], F32, kind="ExternalInput").ap()
    D["sel"] = nc.dram_tensor("sel", [128, 2], F32, kind="ExternalInput").ap()
    first = True
    for l in range(2):
        for g in range(2):
            mix_decl(nc, D, T, f"m{l}{g}_", f"t{g}_", first)
            first = False
    ffn_decl(nc, D, 1, "f0_")
    ffn_decl(nc, D, 8, "f1_")
    y = nc.dram_tensor("y", [TOK, 1024], F32, kind="ExternalOutput").ap()
    mixs = nc.dram_tensor("mixs", [16, 64, T], BF16, kind="Internal").ap()
    x1s = nc.dram_tensor("x1s", [T, 1024], F32, kind="Internal").ap()
    mix8 = mixs.rearrange("(c two) p t -> c (two p) t", two=2)
    for g in range(2):
        emit_mix(nc, D, T, x, (mixs[4 * g:4 * g + 4], mixs[8 + 4 * g:8 + 4 * g + 4]), f"m0{g}_", f"t{g}_", tag=f"a{g}")
    emit_ffn(nc, D, T, 1, [x], [mix8], x1s, "f0_", tag="b")
    for g in range(2):
        emit_mix(nc, D, T, x1s, (mixs[4 * g:4 * g + 4], mixs[8 + 4 * g:8 + 4 * g + 4]), f"m1{g}_", f"t{g}_", tag=f"c{g}")
    emit_ffn(nc, D, TOK, 8, [x1s[0:TOK], x1s[TOK:T]], [mix8[:, :, 0:TOK], mix8[:, :, TOK:T]], y, "f1_", sel="sel", tag="d")
    return nc


def fused_inputs(T, b, hf, xb, p, tabs):
    d = {"x": xb, "sel": np.ascontiguousarray(np.broadcast_to(np.array([1.0 - hf, float(hf)], np.float32)[None, :], (128, 2)))}
    shared = ["kx", "kxc", "mcaus", "mwin", "mc", "eall", "ov", "am", "bd", "ident"]
    for k_ in shared:
        d[k_] = tabs[0][k_]
    for g in range(2):
        for k_ in TG_NAMES:
            d[f"t{g}_{k_}"] = tabs[g][k_]
        for l in range(2):
            for k_, v in mix_weights(l, g, p).items():
                d[f"m{l}{g}_{k_}"] = v
    rep = lambda v, n: np.ascontiguousarray(np.broadcast_to(v[None, :], (128, n)))
    d["identf"] = np.eye(128, dtype=np.float32)
    d["f0_wout"] = p["w_out"][0]; d["f0_gam"] = rep(p["norm_ffn_g"][0], 1024)
    d["f0_wg"] = p["ffn_w_gate"]; d["f0_wu"] = p["ffn_w_up"]; d["f0_wd"] = p["ffn_w_down"]
    d["f1_wout"] = p["w_out"][1]; d["f1_gam"] = rep(p["norm_ffn_g"][1], 1024)
    d["f1_wg"] = p["moe_w_gate"][0]; d["f1_wu"] = p["moe_w_up"][0]; d["f1_wd"] = p["moe_w_down"][0]
    d["f1_router"] = p["moe_router"][0]; d["f1_rb"] = rep(p["moe_router_b"][0], 8)
    return d


_CACHE = {}


def kernel(x, norm_mix_g, w_in, q_norm_g, k_norm_g, cmp_pos, w_cmp, ret_norm_g, w_out,
           norm_ffn_g, ffn_w_gate, ffn_w_up, ffn_w_down,
           moe_router, moe_router_b, moe_w_gate, moe_w_up, moe_w_down):
    f32 = lambda a: np.ascontiguousarray(np.asarray(a, dtype=np.float32))
    p = {"norm_mix_g": f32(norm_mix_g), "w_in": f32(w_in), "q_norm_g": f32(q_norm_g), "k_norm_g": f32(k_norm_g),
         "cmp_pos": f32(cmp_pos), "w_cmp": f32(w_cmp), "ret_norm_g": f32(ret_norm_g), "w_out": f32(w_out),
         "norm_ffn_g": f32(norm_ffn_g), "ffn_w_gate": f32(ffn_w_gate), "ffn_w_up": f32(ffn_w_up),
         "ffn_w_down": f32(ffn_w_down), "moe_router": f32(moe_router), "moe_router_b": f32(moe_router_b),
         "moe_w_gate": f32(moe_w_gate), "moe_w_up": f32(moe_w_up), "moe_w_down": f32(moe_w_down)}
    xc = f32(x)
    B, T, _ = xc.shape
    TOK = T // 2
    if T not in _CACHE:
        _CACHE[T] = build_fused(T)
    nc = _CACHE[T]
    tabs = [mix_tables(T, g) for g in range(2)]
    ims = [fused_inputs(T, c // 2, c % 2, xc[c // 2], p, tabs) for c in range(8)]
    res = run_bass_kernel_spmd(nc, ims, core_ids=list(range(8))).results
    out = np.empty_like(xc)
    for c in range(8):
        b, hf = c // 2, c % 2
        out[b, hf * TOK:(hf + 1) * TOK] = np.asarray(res[c]["y"])
    return out
```

```python
import numpy as np
import ml_dtypes
from contextlib import ExitStack
import concourse.bass as bass
import concourse.mybir as mybir
from concourse.bass_utils import run_bass_kernel_spmd

F32 = mybir.dt.float32
BF16 = mybir.dt.bfloat16
AF = mybir.ActivationFunctionType
ALU = mybir.AluOpType
AX = mybir.AxisListType
NPBF = ml_dtypes.bfloat16

PE, ACT, DVE, POOL, SP = "tensor", "scalar", "vector", "gpsimd", "sync"
ENGS = [PE, ACT, DVE, POOL, SP]
SEM_LIMIT = 30000


class Buf:
    __slots__ = ("ap", "w", "r", "rd", "excl", "name")

    def __init__(self, ap, excl=False, name=""):
        self.ap = ap
        self.w = None
        self.r = {}
        self.rd = []
        self.excl = excl
        self.name = name

    def __getitem__(self, k):
        return self.ap[k]


class Op:
    __slots__ = ("eng", "fn", "deps", "inc", "tok", "dma", "slotdep")

    def __init__(self, eng, fn, dma):
        self.eng = eng
        self.fn = fn
        self.deps = []
        self.inc = False
        self.tok = None
        self.dma = dma
        self.slotdep = None


class Sched:
    def __init__(self, nc, es, tag=""):
        self.tag = tag
        self.nc = nc
        self.es = es
        self.ops = {e: [] for e in ENGS}
        self.n_dma_sems = 6

    def sb(self, name, shape, dt, excl=False):
        t = self.es.enter_context(self.nc.sbuf_tensor(self.tag + name, list(shape), dt))
        return Buf(t, excl, name)

    def ps(self, name, shape=(128, 512), dt=F32):
        t = self.es.enter_context(self.nc.psum_tensor(self.tag + name, list(shape), dt))
        return Buf(t, True, name)

    def op(self, eng, fn, r=(), w=(), dma=False, **kw):
        if isinstance(fn, str):
            name = fn
            fn = lambda e, name=name, kw=kw: getattr(e, name)(**kw)
        o = Op(eng, fn, dma)
        deps = o.deps

        def add(d):
            if d is None:
                return
            if d.eng == eng and not d.dma and eng == PE:
                return
            if d not in deps:
                deps.append(d)

        for b in r:
            add(b.w)
            if b.excl:
                for e2, d in b.r.items():
                    if e2 != eng:
                        add(d)
        for b in w:
            add(b.w)
            for e2, d in b.r.items():
                if e2 != eng or dma or d.dma:
                    add(d)
            for d in b.rd:
                add(d)
        for b in r:
            if dma:
                b.rd.append(o)
            else:
                b.r[eng] = o
        for b in w:
            b.w = o
            b.r = {}
            b.rd = []
        self.ops[eng].append(o)
        return o

    def dma(self, eng, out, in_, r=(), w=(), **kw):
        return self.op(eng, lambda e, out=out, in_=in_, kw=kw: e.dma_start(out=out, in_=in_, **kw), r=r, w=w, dma=True)

    def emit(self):
        nc, es = self.nc, self.es
        for e in ENGS:
            for o in self.ops[e]:
                for d in o.deps:
                    d.inc = True
                if o.dma:
                    o.inc = True
        sems = {}
        nsem = [0]

        def newsem(tag):
            nsem[0] += 1
            return es.enter_context(nc.semaphore(f"{self.tag}s_{tag}_{nsem[0]}"))

        for e in ENGS:
            cur = None
            cnt = 0
            dma_sems = []
            dma_cnt = []
            dma_last = []
            k = 0
            for o in self.ops[e]:
                if not o.inc:
                    continue
                if o.dma:
                    i = k % self.n_dma_sems
                    k += 1
                    if len(dma_sems) <= i:
                        dma_sems.append(newsem(e + "d"))
                        dma_cnt.append(0)
                        dma_last.append(None)
                    if dma_cnt[i] + 16 > SEM_LIMIT:
                        dma_sems[i] = newsem(e + "d")
                        dma_cnt[i] = 0
                    o.slotdep = dma_last[i]
                    dma_cnt[i] += 16
                    o.tok = (dma_sems[i], dma_cnt[i], 16)
                    dma_last[i] = o
                else:
                    if cur is None or cnt + 1 > SEM_LIMIT:
                        cur = newsem(e)
                        cnt = 0
                    cnt += 1
                    o.tok = (cur, cnt, 1)
        self.nsem = nsem[0]
        blk = es.enter_context(nc.Block())

        def make(ename):
            def body(eng):
                waited = {}
                for o in self.ops[ename]:
                    dl = list(o.deps)
                    if o.slotdep is not None:
                        dl.append(o.slotdep)
                    for d in dl:
                        s, v, _ = d.tok
                        key = id(s)
                        if waited.get(key, 0) >= v:
                            continue
                        waited[key] = v
                        eng.wait_ge(s, v)
                    inst = o.fn(eng)
                    if o.inc:
                        s, v, step = o.tok
                        inst.then_inc(s, step)
                for o in self.ops[ename]:
                    if o.dma:
                        s, v, _ = o.tok
                        if waited.get(id(s), 0) < v:
                            waited[id(s)] = v
                            eng.wait_ge(s, v)
            return body

        for ename in ENGS:
            if self.ops[ename]:
                getattr(blk, ename)(make(ename))


EPS = 1e-6
NEGM = -30000.0


def mix_tables(T, g):
    NB = T // 128
    tb = {}
    slopes = np.array([2.0 ** -(4 * g + r + 1) for r in range(4)], np.float64)
    pos = np.arange(T)
    kx = np.stack([np.ones(T), np.ones(T), 128.0 * (pos // 128), (pos % 128).astype(np.float64)])
    tb["kx"] = kx.astype(NPBF)
    NS = 512
    s = np.arange(NS)
    cpos = 16 * s + 15
    kxc = np.stack([np.ones(NS), np.ones(NS), 128.0 * (cpos // 128), (cpos % 128).astype(np.float64)])
    tb["kxc"] = kxc.astype(NPBF)
    ql = np.arange(128)
    qx = np.zeros((4, 4, 128))
    for h in range(4):
        qx[0, h] = -slopes[h] * 128.0
        qx[1, h] = -slopes[h] * ql
        qx[2, h] = slopes[h]
        qx[3, h] = slopes[h]
    tb["qx"] = qx.reshape(4, 512).astype(NPBF)
    k = np.arange(128)[:, None]
    q = np.arange(128)[None, :]
    mca = np.where(k <= q, 0.0, NEGM)
    mwi = np.where(k > q, 0.0, NEGM)
    tb["mcaus"] = np.tile(mca, (1, 4)).astype(NPBF)
    tb["mwin"] = np.tile(mwi, (1, 4)).astype(NPBF)
    u = np.arange(2048 + 128)[None, :]
    sl = np.arange(128)[:, None]
    mc = np.where(16 * sl + 15 <= u, 0.0, NEGM)
    mc0 = mc.copy()
    mc0[0, :] = NEGM
    tb["mc"] = np.stack([mc0, mc], 1).astype(NPBF)
    blk = np.arange(128)[:, None]
    key = np.arange(T)[None, :]
    tb["eall"] = (key // 64 == blk).astype(np.float32).astype(NPBF)
    ov = np.zeros((128, 4, 129), np.float32)
    for ct in range(4):
        for s_l in range(128):
            c = 128 * ct + s_l - 1
            if c < 0:
                continue
            cs, ce = 16 * c, 16 * c + 32
            for b_ in range(cs // 64, min(127, (ce - 1) // 64) + 1):
                o = min(ce, 64 * b_ + 64) - max(cs, 64 * b_)
                if o > 0:
                    ov[s_l, ct, b_] = o / 32.0
        ov[:, ct, 128] = 1.0
    tb["ov"] = np.concatenate([ov[:, :, 128:129], ov[:, :, 0:128]], 2).astype(NPBF)
    am = np.zeros((NB, 128, 128), np.float32)
    for i in range(NB):
        t = 128 * i + np.arange(128)
        cur = t // 64
        b_ = np.arange(128)[None, :]
        forced = (b_ == 0) | (b_ == cur[:, None]) | (b_ == cur[:, None] - 1)
        valid = b_ * 64 <= t[:, None]
        am[i] = np.where(forced, 1e9, np.where(valid, 0.0, -1e9))
    tb["am"] = am
    hh = 4 * g + np.arange(4)
    log_g = np.log1p(-np.exp2(-5.0 - hh.astype(np.float64)))
    idx = np.arange(128, dtype=np.float64)
    sc = 32 ** -0.5
    dec = np.zeros((128, 4, 128))
    for h in range(4):
        d = idx[None, :] - idx[:, None]
        dec[:, h, :] = np.where(d >= 0, np.exp(np.maximum(d, 0) * log_g[h]), 0.0) * sc
    tb["decT"] = dec.astype(np.float32)
    xi = np.zeros((128, 128))
    zeta = np.zeros((128, 128))
    gc = np.zeros((128, 1))
    bd = np.zeros((128, 4, 128))
    for h in range(4):
        xi[32 * h:32 * h + 32, :] = np.exp((idx + 1) * log_g[h])[None, :] * sc
        zeta[:, 32 * h:32 * h + 32] = np.exp((127 - idx) * log_g[h])[:, None]
        gc[32 * h:32 * h + 32] = np.exp(128 * log_g[h])
        bd[32 * h:32 * h + 32, h, :] = 1.0
    tb["xi"] = xi.astype(np.float32)
    tb["zeta"] = zeta.astype(np.float32)
    tb["gc"] = gc.astype(np.float32)
    tb["bd"] = bd.astype(np.float32).astype(NPBF)
    tb["ident"] = np.eye(128, dtype=np.float32).astype(NPBF)
    return tb


def mix_weights(l, g, p):
    w_in = p["w_in"][l]
    offs = np.cumsum([0, 512, 128, 128, 128, 128, 128, 128, 24, 256, 256, 512, 512])
    q0, kc0, vc0, ks0, vs0, kw0, vw0, gt0, rq0, rk0, rv0, rg0 = offs[:12]
    cq = w_in[:, q0 + 256 * g: q0 + 256 * g + 256]
    sl = lambda o, w: w_in[:, o + w * g: o + w * g + w]
    wtm = np.concatenate([cq, sl(ks0, 64), sl(kw0, 64), sl(vs0, 64), sl(vw0, 64), sl(rv0, 256), sl(rg0, 256),
                          sl(rk0, 128), sl(gt0, 12)], 1)
    wfm = np.concatenate([sl(kc0, 64), sl(vc0, 64), sl(rq0, 128), sl(rk0, 128)], 1)
    wc = p["w_cmp"][l]
    wbd = np.zeros((128, 32, 128), np.float32)
    wbd[0:64, :, 0:64] = wc[0].reshape(32, 64, 64).transpose(1, 0, 2)
    wbd[64:128, :, 64:128] = wc[1].reshape(32, 64, 64).transpose(1, 0, 2)
    wcf = np.concatenate([wc[0], wc[1]], 1).reshape(16, 128, 128).transpose(1, 0, 2)
    cp = p["cmp_pos"][l].reshape(2, 2048)
    posf = cp.reshape(2, 16, 128).transpose(2, 0, 1)
    rep = lambda v: np.ascontiguousarray(np.broadcast_to(v[None, :], (128, v.shape[0])))
    gqk = np.concatenate([np.tile(p["q_norm_g"][l], 4), p["k_norm_g"][l][1], p["k_norm_g"][l][2]])
    return {
        "gam": rep(p["norm_mix_g"][l]), "wtm": np.ascontiguousarray(wtm), "wfm": np.ascontiguousarray(wfm),
        "wbd": wbd, "wcf": np.ascontiguousarray(wcf), "posf": np.ascontiguousarray(posf),
        "gqk": rep(gqk), "gkc": rep(p["k_norm_g"][l][0]),
        "gret": rep(p["ret_norm_g"][l][256 * g:256 * g + 256]),
    }


def mix_decl(nc, D, T, pw, pt, shared):
    NB = T // 128

    def din(name, shape, dt=F32):
        if name not in D:
            D[name] = nc.dram_tensor(name, list(shape), dt, kind="ExternalInput").ap()

    for nm_, shp in [("gam", [128, 1024]), ("wtm", [1024, 1164]), ("wfm", [1024, 384]), ("wbd", [128, 32, 128]),
                     ("wcf", [128, 16, 128]), ("posf", [128, 2, 16]), ("gqk", [128, 384]), ("gkc", [128, 64]),
                     ("gret", [128, 256])]:
        din(pw + nm_, shp)
    din(pt + "qx", [4, 512], BF16); din(pt + "decT", [128, 4, 128]); din(pt + "xi", [128, 128])
    din(pt + "zeta", [128, 128]); din(pt + "gc", [128, 1])
    if shared:
        din("kx", [4, T], BF16); din("kxc", [4, 512], BF16)
        din("mcaus", [128, 512], BF16); din("mwin", [128, 512], BF16); din("mc", [128, 2, 2176], BF16)
        din("eall", [128, T], BF16); din("ov", [128, 4, 129], BF16); din("am", [NB, 128, 128])
        din("bd", [128, 4, 128], BF16); din("ident", [128, 128], BF16)


WNAMES = ["gam", "wtm", "wfm", "wbd", "wcf", "posf", "gqk", "gkc", "gret"]
TG_NAMES = ["qx", "decT", "xi", "zeta", "gc"]


def emit_mix(nc, D0, T, x, mix_out, pw, pt, tag=""):
    NT = T // 512
    NB = T // 128
    D = dict(D0)
    for k_ in WNAMES:
        D[k_] = D0[pw + k_]
    for k_ in TG_NAMES:
        D[k_] = D0[pt + k_]
    mix_nsa, mix_ret = mix_out

    with ExitStack() as es:
        S = Sched(nc, es, tag)
        sb = S.sb
        gam = sb("gam_s", [128, 1024], F32)
        wtm = sb("wtm_s", [128, 8, 1164], BF16)
        wfm = sb("wfm_s", [128, 8, 384], BF16)
        wbd = sb("wbd_s", [128, 32, 128], BF16)
        wcf = sb("wcf_s", [128, 16, 128], BF16)
        posf = sb("posf_s", [128, 2, 16], BF16)
        gqk = sb("gqk_s", [128, 384], F32)
        gkc = sb("gkc_s", [128, 64], F32)
        gret = sb("gret_s", [128, 256], F32)
        KS = sb("KS", [68, T], BF16)
        KW = sb("KW", [68, T], BF16)
        KC = sb("KC", [68, 512], BF16)
        VSW = sb("VSW", [128, NB, 2, 65], BF16)
        CV = sb("CV", [128, 4, 193], BF16)
        KCVC = sb("KCVC", [128, 16 + 2048], BF16)
        EALL = sb("EALL", [128, T], BF16)
        QXB = sb("QXB", [68, 512], BF16)
        MCA = sb("MCA", [128, 512], BF16); MWI = sb("MWI", [128, 512], BF16)
        MC = sb("MC", [128, 2, 2176], BF16)
        DEC = sb("DEC", [128, 4, 128], F32); XI = sb("XI", [128, 128], F32); ZETA = sb("ZETA", [128, 128], F32)
        GC = sb("GC", [128, 1], F32); BD = sb("BD", [128, 4, 128], BF16)
        IDN = sb("IDN", [128, 128], BF16)
        ONES1 = sb("ONES1", [1, 128], BF16)
        BIASR = sb("BIASR", [1, 128], BF16)
        CM05 = sb("CM05", [128, 8], F32)
        STATE = sb("STATE", [128, 64], F32)
        SBD = sb("SBD", [128, 4, 64], BF16)
        banks = [S.ps(f"pb{i}") for i in range(8)]
        OB, PB4, RB = banks[4:8], banks[0:3], banks[3]
        cnt = {"s": 0, "m": 0}

        def mbank():
            cnt["m"] += 1
            return PB4[cnt["m"] % 3]

        def sbank():
            return mbank()

        class Rot:
            def __init__(self, name, shape, dt, n):
                self.b = [sb(f"{name}{i}", shape, dt) for i in range(n)]
                self.i = 0

            def get(self):
                self.i += 1
                return self.b[self.i % len(self.b)]

        xbuf = Rot("xb", [128, 1024], F32, 2)
        junk = Rot("junk", [128, 1024], F32, 1)
        hb = Rot("hb", [128, 1024], BF16, 2)
        hT = Rot("hT", [128, 8, 512], BF16, 1)
        sm = Rot("sm", [128, 16], F32, 6)
        sqb = Rot("sqb", [128, 384], F32, 1)
        qkn = Rot("qkn", [128, 384], BF16, 2)
        t384 = Rot("t384", [128, 384], F32, 1)
        QT = [sb(f"QT{i}", [68, 512], BF16) for i in range(4)]
        RQT = Rot("RQT", [128, 512], BF16, 1); RKT = Rot("RKT", [128, 512], BF16, 1)
        RV = Rot("RV", [128, 4, 256], BF16, 1)
        G2 = Rot("G2", [128, 4, 256], F32, 1)
        KZ = Rot("KZ", [128, 4, 128], BF16, 1)
        GT = Rot("GT", [128, 4, 12], F32, 1)
        PT = Rot("PT", [128, 512], BF16, 3)
        NMT = Rot("NMT", [128, 512], BF16, 2)
        AMB = Rot("AMB", [128, 128], F32, 2)
        impb = Rot("impb", [128, 128], F32, 2)
        wk = Rot("wk", [128, 128], F32, 1)
        nmq = Rot("nmq", [128, 128], BF16, 1)
        m8 = Rot("m8", [128, 16], F32, 2)
        acc = Rot("acc", [128, 256], F32, 2)
        tmp256 = Rot("tmp256", [128, 256], F32, 2)
        fb = Rot("fb", [128, 16], F32, 4)
        nsab = Rot("nsab", [128, 256], BF16, 2)
        RQBD = Rot("RQBD", [128, 4, 128], BF16, 2)
        RQX = Rot("RQX", [128, 128], BF16, 2)
        IND = Rot("IND", [128, 4, 128], BF16, 2)
        sq256 = Rot("sq256", [128, 256], F32, 1)
        retb = Rot("retb", [128, 256], BF16, 2)
        red64 = Rot("red64", [128, 64], F32, 1)
        t4 = Rot("t4", [128, 4, 64], F32, 1)
        stg = Rot("stg", [64, 8, 512], BF16, 1)
        kcn = Rot("kcn", [128, 64], BF16, 1)
        kct = Rot("kct", [128, 64], F32, 1)

        def ld(eng, dst, src, **kw):
            S.dma(eng, dst.ap[:] if isinstance(dst, Buf) else dst, src, w=[dst] if isinstance(dst, Buf) else (), **kw)

        ld(SP, gam, D["gam"][:, :])
        S.dma(POOL, wtm.ap[:], D["wtm"].rearrange("(kc p) n -> p kc n", p=128), w=[wtm])
        S.dma(POOL, wfm.ap[:], D["wfm"].rearrange("(kc p) n -> p kc n", p=128), w=[wfm])
        S.dma(POOL, wbd.ap[:], D["wbd"][:, :, :], w=[wbd])
        S.dma(POOL, wcf.ap[:], D["wcf"][:, :, :], w=[wcf])
        S.dma(POOL, posf.ap[:], D["posf"][:, :, :], w=[posf])
        ld(SP, gqk, D["gqk"][:, :]); ld(SP, gkc, D["gkc"][:, :]); ld(SP, gret, D["gret"][:, :])
        S.dma(SP, KS.ap[64:68, :], D["kx"][:, :], w=[KS])
        S.dma(SP, KW.ap[64:68, :], D["kx"][:, :], w=[KW])
        S.dma(SP, KC.ap[64:68, :], D["kxc"][:, :], w=[KC])
        S.dma(SP, QXB.ap[64:68, :], D["qx"][:, :], w=[QXB])
        for q_ in QT:
            S.dma(SP, q_.ap[64:68, :], D["qx"][:, :], w=[q_])
        ld(SP, MCA, D["mcaus"][:, :]); ld(SP, MWI, D["mwin"][:, :]); ld(SP, MC, D["mc"][:, :, :])
        ld(SP, EALL, D["eall"][:, :])
        ld(SP, DEC, D["decT"][:, :, :]); ld(SP, XI, D["xi"][:, :]); ld(SP, ZETA, D["zeta"][:, :])
        ld(SP, GC, D["gc"][:, :]); ld(SP, BD, D["bd"][:, :, :]); ld(SP, IDN, D["ident"][:, :])
        S.op(POOL, "memset", ap=VSW.ap[:], constant=1.0, w=[VSW])
        S.op(POOL, "memset", ap=CV.ap[:], constant=0.0, w=[CV])
        S.dma(SP, CV.ap[:, :, 64:193], D["ov"][:, :, :], w=[CV])
        S.op(POOL, "memset", ap=KCVC.ap[:], constant=0.0, w=[KCVC])
        S.op(POOL, "memset", ap=KC.ap[0:64, :], constant=0.0, w=[KC])
        S.op(POOL, "memset", ap=ONES1.ap[:], constant=1.0, w=[ONES1])
        S.op(POOL, "memset", ap=CM05.ap[:], constant=-0.5, w=[CM05])
        S.op(POOL, "memset", ap=STATE.ap[:], constant=0.0, w=[STATE])
        S.op(POOL, "memset", ap=SBD.ap[:], constant=0.0, w=[SBD])
        S.op(DVE, "tensor_scalar", out=gqk.ap[:, 0:256], in0=gqk.ap[:, 0:256], scalar1=0.125, scalar2=None,
                                            op0=ALU.mult, r=[gqk], w=[gqk])
        b_ = mbank()
        for kv in range(2):
            for c in range(16):
                S.op(PE, "matmul", out=b_.ap[0:1, 64 * kv:64 * kv + 64], lhsT=posf.ap[:, kv, c:c + 1],
                                                         rhs=wcf.ap[:, c, 64 * kv:64 * kv + 64],
                                                         start=(c == 0 and kv == 0), stop=(c == 15), skip_group_check=True,
                     r=[posf, wcf], w=[b_])
        S.op(ACT, "activation", out=BIASR.ap[:], in_=b_.ap[0:1, 0:128], func=AF.Copy, r=[b_], w=[BIASR])

        def rstd_pow(dst, src, n, scale):
            pass

        def ret_gen(j, rqt, rkt, rv, g2, kz, sg):
            tok = slice(128 * j, 128 * j + 128)
            rqbd = RQBD.get(); rqx = RQX.get()
            S.op(POOL, "tensor_tensor", out=rqbd.ap[:], in0=rqt.ap[:, tok].unsqueeze(1).broadcast_to([128, 4, 128]),
                 in1=BD.ap[:], op=ALU.mult, r=[rqt, BD], w=[rqbd])
            S.op(POOL, "tensor_tensor", out=rqx.ap[:], in0=rqt.ap[:, tok], in1=XI.ap[:], op=ALU.mult, r=[rqt, XI], w=[rqx])
            yield
            pin = RB
            S.op(PE, "matmul", out=pin.ap[:, :], lhsT=rkt.ap[:, tok], rhs=rqbd.ap[:].rearrange("p h i -> p (h i)"),
                 start=True, stop=True, r=[rkt, rqbd], w=[pin])
            yield
            ind = IND.get()
            S.op(DVE, "tensor_tensor", out=ind.ap[:].rearrange("p h i -> p (h i)"), in0=pin.ap[:, :],
                 in1=DEC.ap[:].rearrange("p h i -> p (h i)"), op=ALU.mult, r=[pin, DEC], w=[ind])
            yield
            po = RB
            S.op(PE, "matmul", out=po.ap[:, 0:256], lhsT=rqx.ap[:], rhs=SBD.ap[:].rearrange("p h e -> p (h e)"),
                 start=True, stop=False, r=[rqx, SBD], w=[po])
            for h in range(4):
                S.op(PE, "matmul", out=po.ap[:, 64 * h:64 * h + 64], lhsT=ind.ap[:, h, :], rhs=rv.ap[:, j, 64 * h:64 * h + 64],
                     start=False, stop=(h == 3), r=[ind, rv], w=[po])
            pkv = RB
            S.op(PE, "matmul", out=pkv.ap[:, 256:512], lhsT=kz.ap[:, j, :], rhs=rv.ap[:, j, :], start=True, stop=True,
                 r=[kz, rv], w=[pkv])
            yield
            sq = sq256.get(); st = sm.get()
            S.op(ACT, "activation", out=sq.ap[:], in_=po.ap[:, 0:256], func=AF.Square, r=[po], w=[sq])
            t4_ = t4.get(); r64 = red64.get()
            S.op(DVE, "tensor_tensor", out=t4_.ap[:], in0=pkv.ap[:, 256:512].rearrange("p (h e) -> p h e", h=4),
                 in1=BD.ap[:, :, 0:64], op=ALU.mult, r=[pkv, BD], w=[t4_])
            S.op(DVE, "tensor_reduce", out=r64.ap[:], in_=t4_.ap[:].rearrange("p h e -> p e h"), axis=AX.X, op=ALU.add,
                 r=[t4_], w=[r64])
            S.op(DVE, "scalar_tensor_tensor", out=STATE.ap[:], in0=STATE.ap[:], scalar=GC.ap[:, 0:1], in1=r64.ap[:],
                 op0=ALU.mult, op1=ALU.add, r=[STATE, GC, r64], w=[STATE])
            S.op(POOL, "tensor_tensor", out=SBD.ap[:], in0=STATE.ap[:].unsqueeze(1).broadcast_to([128, 4, 64]),
                 in1=BD.ap[:, :, 0:64], op=ALU.mult, r=[STATE, BD], w=[SBD])
            S.op(DVE, "tensor_reduce", out=st.ap[:, 0:4], in_=sq.ap[:].rearrange("p (h d) -> p h d", h=4), axis=AX.X,
                 op=ALU.add, r=[sq], w=[st])
            S.op(DVE, "tensor_scalar", out=st.ap[:, 4:8], in0=st.ap[:, 0:4], scalar1=1.0 / 64, scalar2=EPS, op0=ALU.mult,
                 op1=ALU.add, r=[st], w=[st])
            S.op(POOL, "tensor_tensor", out=st.ap[:, 8:12], in0=st.ap[:, 4:8], in1=CM05.ap[:, 0:4], op=ALU.pow,
                 r=[st, CM05], w=[st])
            yield
            tm_ = tmp256.get()
            S.op(DVE, "tensor_tensor", out=tm_.ap[:].rearrange("p (h d) -> p h d", h=4),
                 in0=po.ap[:, 0:256].rearrange("p (h d) -> p h d", h=4),
                 in1=st.ap[:, 8:12].unsqueeze(2).broadcast_to([128, 4, 64]), op=ALU.mult, r=[po, st], w=[tm_])
            rb = retb.get()
            S.op(POOL, "tensor_tensor", out=rb.ap[:], in0=tm_.ap[:], in1=g2.ap[:, j, :], op=ALU.mult, r=[tm_, g2], w=[rb])
            yield
            pb2 = RB
            pbb2 = pb2.ap[:].bitcast(BF16)
            for h in range(4):
                S.op(PE, "transpose", out=pbb2[0:64, 128 * h:128 * h + 128], in_=rb.ap[:, 64 * h:64 * h + 64],
                     identity=IDN.ap[:], r=[rb, IDN], w=[pb2])
            yield
            S.op(DVE, "tensor_copy", out=sg.ap[:, 4:8, 128 * j:128 * j + 128],
                 in_=pbb2[0:64, 0:512].rearrange("p (h q) -> p h q", h=4), r=[pb2], w=[sg])

        for n in range(NT):
            hTn = hT.get()
            for j in range(4):
                xb_ = xbuf.get()
                r0 = 512 * n + 128 * j
                S.dma(SP, xb_.ap[:], x[r0:r0 + 128, :], w=[xb_])
                jk = junk.get(); st = sm.get()
                S.op(ACT, "activation", out=jk.ap[:], in_=xb_.ap[:], func=AF.Square,
                                                                        accum_out=st.ap[:, 0:1], r=[xb_], w=[jk, st])
                S.op(DVE, "tensor_scalar", out=st.ap[:, 1:2], in0=st.ap[:, 0:1], scalar1=1.0 / 1024,
                                                           scalar2=EPS, op0=ALU.mult, op1=ALU.add, r=[st], w=[st])
                S.op(POOL, "tensor_tensor", out=st.ap[:, 2:3], in0=st.ap[:, 1:2], in1=CM05.ap[:, 0:1],
                                                            op=ALU.pow, r=[st, CM05], w=[st])
                h_ = hb.get()
                S.op(DVE, "scalar_tensor_tensor",
                    out=h_.ap[:], in0=xb_.ap[:], scalar=st.ap[:, 2:3], in1=gam.ap[:], op0=ALU.mult, op1=ALU.mult,
                    r=[xb_, st, gam], w=[h_])
                pb = mbank()
                pbb = pb.ap[:].bitcast(BF16)
                for kc in range(8):
                    S.op(PE, "transpose", out=pbb[:, 128 * kc:128 * kc + 128],
                                                                          in_=h_.ap[:, 128 * kc:128 * kc + 128],
                                                                          identity=IDN.ap[:], r=[h_, IDN], w=[pb])
                S.op(ACT, "activation",
                    out=hTn.ap[:, :, 128 * j:128 * j + 128], in_=pbb.rearrange("p (k t) -> p k t", k=8), func=AF.Copy,
                    r=[pb], w=[hTn])
            rqt = RQT.get(); rkt = RKT.get()
            m = n % 4
            if m == 0 and n > 0:
                S.op(POOL, "tensor_copy", out=KCVC.ap[:, 0:16], in_=KCVC.ap[:, 2048:2064], r=[KCVC], w=[KCVC])
            for ci, dst in enumerate([None, rqt, rkt]):
                pb = mbank()
                for kc in range(8):
                    S.op(PE, "matmul", out=pb.ap[:, :], lhsT=wfm.ap[:, kc, 128 * ci:128 * ci + 128],
                                                                     rhs=hTn.ap[:, kc, :], start=(kc == 0), stop=(kc == 7),
                         r=[wfm, hTn], w=[pb])
                if ci == 0:
                    S.op(ACT, "activation", out=KCVC.ap[:, 16 + 512 * m:16 + 512 * m + 512],
                                                                 in_=pb.ap[:, :], func=AF.Copy, r=[pb], w=[KCVC])
                else:
                    S.op(DVE, "tensor_copy", out=dst.ap[:], in_=pb.ap[:, :], r=[pb], w=[dst])
            rv = RV.get(); g2 = G2.get(); kz = KZ.get(); gt = GT.get()
            for j in range(4):
                blk = 4 * n + j
                tok = slice(128 * j, 128 * j + 128)
                pa = mbank()
                for kc in range(8):
                    S.op(PE, "matmul", out=pa.ap[:, :], lhsT=hTn.ap[:, kc, tok],
                                                                       rhs=wtm.ap[:, kc, 0:512], start=(kc == 0),
                                                                       stop=(kc == 7), r=[hTn, wtm], w=[pa])
                sq = sqb.get(); st = sm.get()
                S.op(ACT, "activation", out=sq.ap[:], in_=pa.ap[:, 0:384], func=AF.Square,
                     r=[pa], w=[sq])
                S.op(ACT, "activation", out=VSW.ap[:, blk, :, 0:64],
                                                                 in_=pa.ap[:, 384:512].rearrange("p (a d) -> p a d", a=2),
                                                                 func=AF.Copy, r=[pa], w=[VSW])
                S.op(DVE, "tensor_reduce", out=st.ap[:, 0:6],
                                                                  in_=sq.ap[:].rearrange("p (a d) -> p a d", a=6),
                                                                  axis=AX.X, op=ALU.add, r=[sq], w=[st])
                S.op(DVE, "tensor_scalar", out=st.ap[:, 6:12], in0=st.ap[:, 0:6], scalar1=1.0 / 64,
                                                           scalar2=EPS, op0=ALU.mult, op1=ALU.add, r=[st], w=[st])
                st2 = sm.get()
                S.op(POOL, "tensor_tensor", out=st2.ap[:, 0:6], in0=st.ap[:, 6:12],
                                                                     in1=CM05.ap[:, 0:6], op=ALU.pow,
                     r=[st, CM05], w=[st2])
                tt = t384.get(); qk = qkn.get()
                S.op(DVE, "tensor_tensor",
                    out=tt.ap[:].rearrange("p (a d) -> p a d", a=6), in0=pa.ap[:, 0:384].rearrange("p (a d) -> p a d", a=6),
                    in1=st2.ap[:, 0:6].unsqueeze(2).broadcast_to([128, 6, 64]), op=ALU.mult, r=[pa, st2], w=[tt])
                S.op(POOL, "tensor_tensor", out=qk.ap[:], in0=tt.ap[:], in1=gqk.ap[:], op=ALU.mult,
                     r=[tt, gqk], w=[qk])
                pt_ = mbank()
                ptb = pt_.ap[:].bitcast(BF16)
                for a in range(6):
                    S.op(PE, "transpose", out=ptb[0:64, 128 * a:128 * a + 128],
                                                                        in_=qk.ap[:, 64 * a:64 * a + 64],
                                                                        identity=IDN.ap[:], r=[qk, IDN], w=[pt_])
                S.op(ACT, "activation", out=QT[j].ap[0:64, :], in_=ptb[0:64, 0:512], func=AF.Copy,
                     r=[pt_], w=[QT[j]])
                S.op(DVE, "tensor_copy", out=KS.ap[0:64, 128 * blk:128 * blk + 128],
                                                                    in_=ptb[0:64, 512:640], r=[pt_], w=[KS])
                S.op(DVE, "tensor_copy", out=KW.ap[0:64, 128 * blk:128 * blk + 128],
                                                                    in_=ptb[0:64, 640:768], r=[pt_], w=[KW])
                S.op(DVE, "tensor_scalar", out=QT[j].ap[64:65, :], in0=QXB.ap[64:65, :],
                                                                  scalar1=float(blk), scalar2=None, op0=ALU.mult,
                     r=[QXB], w=[QT[j]])
                pbk = mbank()
                for kc in range(8):
                    S.op(PE, "matmul", out=pbk.ap[:, :], lhsT=hTn.ap[:, kc, tok],
                                                                         rhs=wtm.ap[:, kc, 512:1024], start=(kc == 0),
                                                                         stop=(kc == 7), r=[hTn, wtm], w=[pbk])
                S.op(DVE, "tensor_copy", out=rv.ap[:, j, :], in_=pbk.ap[:, 0:256], r=[pbk], w=[rv])
                S.op(ACT, "activation", out=g2.ap[:, j, :], in_=pbk.ap[:, 256:512], func=AF.Silu,
                     r=[pbk], w=[g2])
                S.op(POOL, "tensor_tensor", out=g2.ap[:, j, :], in0=g2.ap[:, j, :], in1=gret.ap[:],
                                                          op=ALU.mult, r=[g2, gret], w=[g2])
                pc = mbank()
                for kc in range(8):
                    S.op(PE, "matmul", out=pc.ap[:, 0:140], lhsT=hTn.ap[:, kc, tok],
                                                                       rhs=wtm.ap[:, kc, 1024:1164], start=(kc == 0),
                                                                       stop=(kc == 7), r=[hTn, wtm], w=[pc])
                S.op(DVE, "tensor_tensor", out=kz.ap[:, j, :], in0=pc.ap[:, 0:128], in1=ZETA.ap[:],
                                                                op=ALU.mult, r=[pc, ZETA], w=[kz])
                S.op(ACT, "activation", out=gt.ap[:, j, :], in_=pc.ap[:, 128:140], func=AF.Sigmoid,
                     r=[pc], w=[gt])
            ct = n // 4
            pcm = mbank()
            S.op(PE, "matmul", out=pcm.ap[:, 0:128], lhsT=ONES1.ap[0:1, :], rhs=BIASR.ap[0:1, :],
                                                 start=True, stop=False, r=[ONES1, BIASR], w=[pcm])
            for l in range(32):
                S.op(PE, "matmul", out=pcm.ap[:, 0:128], lhsT=KCVC.ap[:, l:l + 2033:16],
                                                          rhs=wbd.ap[:, l, :], start=False, stop=(l == 31),
                     r=[KCVC, wbd], w=[pcm])
            S.op(ACT, "activation", out=CV.ap[:, ct, 0:64], in_=pcm.ap[:, 64:128], func=AF.Copy,
                 r=[pcm], w=[CV])
            kt_ = kct.get(); st = sm.get()
            S.op(ACT, "activation", out=kt_.ap[:], in_=pcm.ap[:, 0:64], func=AF.Square,
                                                                      accum_out=st.ap[:, 0:1], r=[pcm], w=[kt_, st])
            S.op(DVE, "tensor_scalar", out=st.ap[:, 1:2], in0=st.ap[:, 0:1], scalar1=1.0 / 64, scalar2=EPS,
                                                       op0=ALU.mult, op1=ALU.add, r=[st], w=[st])
            S.op(POOL, "tensor_tensor", out=st.ap[:, 2:3], in0=st.ap[:, 1:2], in1=CM05.ap[:, 0:1],
                                                        op=ALU.pow, r=[st, CM05], w=[st])
            kn = kcn.get()
            S.op(DVE, "scalar_tensor_tensor", out=kn.ap[:], in0=pcm.ap[:, 0:64],
                                                                              scalar=st.ap[:, 2:3], in1=gkc.ap[:],
                                                                              op0=ALU.mult, op1=ALU.mult,
                 r=[pcm, st, gkc], w=[kn])
            pk = mbank()
            pkb = pk.ap[:].bitcast(BF16)
            S.op(PE, "transpose", out=pkb[0:64, 0:128], in_=kn.ap[:, 0:64], identity=IDN.ap[:],
                 r=[kn, IDN], w=[pk])
            S.op(DVE, "tensor_copy", out=KC.ap[0:64, 128 * ct:128 * ct + 128], in_=pkb[0:64, 0:128],
                 r=[pk], w=[KC])

            sg = stg.get()
            for j in range(4):
                i = 4 * n + j
                q0 = 128 * i
                qt = QT[j]
                jobs = []
                nct = (128 * i + 112) // 2048 + 1
                O_c = [OB[0], OB[1]]
                O_w = OB[2]
                O_s = OB[3]
                for c in range(nct):
                    jobs.append(("c", c, c == nct - 1))
                for kt in range(max(0, i - 4), i + 1):
                    jobs.append(("w", kt, kt == i or kt == i - 4))
                for kt in range(0, i + 1):
                    jobs.append(("s", kt, kt == i))
                first = {"c": True, "w": True, "s": True}
                nm_holder = {}

                def emit_qk(job):
                    br, kt, special = job
                    sbk = sbank()
                    if br == "c":
                        S.op(PE, "matmul", out=sbk.ap[:, :], lhsT=KC.ap[0:68, 128 * kt:128 * kt + 128], rhs=qt.ap[0:68, :],
                                                    start=True, stop=not special, r=[KC, qt], w=[sbk])
                        if special:
                            o = q0 - 2048 * kt
                            var = 0 if kt == 0 else 1
                            for h in range(4):
                                S.op(PE, "matmul", out=sbk.ap[:, 128 * h:128 * h + 128], lhsT=IDN.ap[:],
                                                                 rhs=MC.ap[:, var, o:o + 128], start=False, stop=(h == 3),
                                     r=[IDN, MC], w=[sbk])
                    elif br == "w":
                        S.op(PE, "matmul", out=sbk.ap[:, :], lhsT=KW.ap[0:68, 128 * kt:128 * kt + 128], rhs=qt.ap[0:68, :],
                                                    start=True, stop=not special, r=[KW, qt], w=[sbk])
                        if special:
                            msk = MCA if kt == i else MWI
                            S.op(PE, "matmul", out=sbk.ap[:, :], lhsT=IDN.ap[:], rhs=msk.ap[:], start=False, stop=True,
                                 r=[IDN, msk], w=[sbk])
                    else:
                        nm = nm_holder["nm"]
                        S.op(PE, "matmul", out=sbk.ap[:, :], lhsT=KS.ap[0:68, 128 * kt:128 * kt + 128], rhs=qt.ap[0:68, :],
                                                    start=True, stop=False, r=[KS, qt], w=[sbk])
                        S.op(PE, "matmul", out=sbk.ap[:, :], lhsT=EALL.ap[:, 128 * kt:128 * kt + 128], rhs=nm.ap[:],
                                                    start=False, stop=not special, r=[EALL, nm], w=[sbk])
                        if special:
                            S.op(PE, "matmul", out=sbk.ap[:, :], lhsT=IDN.ap[:], rhs=MCA.ap[:], start=False, stop=True,
                                 r=[IDN, MCA], w=[sbk])
                    return sbk

                def emit_exp_pv(job, sbk):
                    br, kt, special = job
                    p = PT.get()
                    S.op(ACT, "activation", out=p.ap[:], in_=sbk.ap[:, :], func=AF.Exp, r=[sbk], w=[p])
                    fst = first[br]
                    first[br] = False
                    if br == "c":
                        for h in range(4):
                            ob = O_c[h // 2]
                            c0 = 193 * (h % 2)
                            S.op(PE, "matmul", out=
                                ob.ap[:, c0:c0 + 193], lhsT=p.ap[:, 128 * h:128 * h + 128], rhs=CV.ap[:, kt, :],
                                start=(fst and h % 2 == 0), stop=False, skip_group_check=True, r=[p, CV], w=[ob])
                    else:
                        ob = O_w if br == "w" else O_s
                        a = 1 if br == "w" else 0
                        for h in range(4):
                            S.op(PE, "matmul", out=ob.ap[:, 65 * h:65 * h + 65], lhsT=p.ap[:, 128 * h:128 * h + 128],
                                                             rhs=VSW.ap[:, kt, a, :], start=(fst and h == 0), stop=False,
                                                             skip_group_check=True, r=[p, VSW], w=[ob])

                ac = acc.get()
                gtj = gt.ap[:, j, :].rearrange("p (r b) -> p r b", b=3)

                def epilogue(br):
                    f = fb.get()
                    bi = {"c": 0, "s": 1, "w": 2}[br]
                    if br == "c":
                        srcs = [(O_c[0], 0), (O_c[0], 193), (O_c[1], 0), (O_c[1], 193)]
                        for h, (ob, c0) in enumerate(srcs):
                            S.op(DVE, "tensor_scalar",
                                out=f.ap[:, h:h + 1], in0=ob.ap[:, c0 + 64:c0 + 65], scalar1=1e-30, scalar2=None, op0=ALU.add,
                                r=[ob], w=[f])
                    else:
                        ob = O_w if br == "w" else O_s
                        S.op(DVE, "tensor_scalar",
                            out=f.ap[:, 0:4], in0=ob.ap[:, 0:260].rearrange("p (h c) -> p h c", c=65)[:, :, 64],
                            scalar1=1e-30, scalar2=None, op0=ALU.add, r=[ob], w=[f])
                    S.op(DVE, "reciprocal", out=f.ap[:, 4:8], in_=f.ap[:, 0:4], r=[f], w=[f])
                    S.op(DVE, "tensor_tensor", out=f.ap[:, 8:12], in0=f.ap[:, 4:8], in1=gtj[:, :, bi], op=ALU.mult,
                         r=[f, gt], w=[f])
                    dst = ac if br == "c" else tmp256.get()
                    if br == "c":
                        for h, (ob, c0) in enumerate(srcs):
                            S.op(DVE, "tensor_scalar",
                                out=dst.ap[:, 64 * h:64 * h + 64], in0=ob.ap[:, c0:c0 + 64], scalar1=f.ap[:, 8 + h:9 + h],
                                scalar2=None, op0=ALU.mult, r=[ob, f], w=[dst])
                    else:
                        S.op(DVE, "tensor_tensor",
                            out=dst.ap[:].rearrange("p (h d) -> p h d", h=4),
                            in0=ob.ap[:, 0:260].rearrange("p (h c) -> p h c", c=65)[:, :, 0:64],
                            in1=f.ap[:, 8:12].unsqueeze(2).broadcast_to([128, 4, 64]), op=ALU.mult, r=[ob, f], w=[dst])
                        S.op(POOL, "tensor_tensor", out=ac.ap[:], in0=ac.ap[:], in1=dst.ap[:], op=ALU.add,
                             r=[ac, dst], w=[ac])
                    return f

                def selection(f):
                    am_ = AMB.get()
                    S.dma(SP, am_.ap[:], D["am"][i, :, :], w=[am_])
                    im = impb.get()
                    srcs = [(O_c[0], 0), (O_c[0], 193), (O_c[1], 0), (O_c[1], 193)]
                    for h, (ob, c0) in enumerate(srcs):
                        prev = am_ if h == 0 else im
                        S.op(DVE, "scalar_tensor_tensor",
                            out=im.ap[:], in0=ob.ap[:, c0 + 65:c0 + 193], scalar=f.ap[:, 4 + h:5 + h], in1=prev.ap[:],
                            op0=ALU.mult, op1=ALU.add, r=[ob, f, prev], w=[im])
                    mm = m8.get(); w_ = wk.get()
                    S.op(DVE, "max", out=mm.ap[:, 0:8], in_=im.ap[:], r=[im], w=[mm])
                    S.op(DVE, "match_replace", out=w_.ap[:], in_to_replace=mm.ap[:, 0:8], in_values=im.ap[:],
                                                        imm_value=-3e38, r=[im, mm], w=[w_])
                    S.op(DVE, "max", out=mm.ap[:, 8:16], in_=w_.ap[:], r=[w_], w=[mm])
                    nq = nmq.get()
                    S.op(DVE, "tensor_scalar", out=nq.ap[:], in0=im.ap[:], scalar1=mm.ap[:, 15:16], scalar2=NEGM,
                                                        op0=ALU.is_lt, op1=ALU.mult, r=[im, mm], w=[nq])
                    nm_holder["nq"] = nq

                def selection2():
                    nq = nm_holder["nq"]
                    pb = mbank()
                    pbb = pb.ap[:].bitcast(BF16)
                    S.op(PE, "transpose", out=pbb[:, 0:128], in_=nq.ap[:], identity=IDN.ap[:], r=[nq, IDN], w=[pb])
                    nm = NMT.get()
                    S.op(ACT, "activation", out=nm.ap[:].rearrange("p (h q) -> p h q", h=4),
                         in_=pbb[:, 0:128].unsqueeze(1).broadcast_to([128, 4, 128]), func=AF.Copy, r=[pb], w=[nm])
                    nm_holder["nm"] = nm

                emitted = {}
                rgen = ret_gen(j, rqt, rkt, rv, g2, kz, sg)

                def ensure(kk):
                    if kk >= len(jobs) or kk in emitted:
                        return True
                    if jobs[kk][0] == "s" and "nm" not in nm_holder:
                        return False
                    emitted[kk] = emit_qk(jobs[kk])
                    return True

                ensure(0)
                for k in range(len(jobs)):
                    job = jobs[k]
                    if ensure(k + 1):
                        ensure(k + 2)
                    emit_exp_pv(job, emitted[k])
                    next(rgen, None)
                    br = job[0]
                    if k + 1 == len(jobs) or jobs[k + 1][0] != br:
                        f = epilogue(br)
                        if br == "c":
                            selection(f)
                    if k + 1 < len(jobs) and (k + 1) not in emitted:
                        if "nm" not in nm_holder:
                            selection2()
                        ensure(k + 1)
                nb_ = nsab.get()
                S.op(ACT, "activation", out=nb_.ap[:], in_=ac.ap[:], func=AF.Copy, r=[ac], w=[nb_])
                pb = mbank()
                pbb = pb.ap[:].bitcast(BF16)
                for h in range(4):
                    S.op(PE, "transpose", out=pbb[0:64, 128 * h:128 * h + 128], in_=nb_.ap[:, 64 * h:64 * h + 64],
                                                        identity=IDN.ap[:], r=[nb_, IDN], w=[pb])
                S.op(DVE, "tensor_copy", out=sg.ap[:, 0:4, 128 * j:128 * j + 128],
                                                  in_=pbb[0:64, 0:512].rearrange("p (h q) -> p h q", h=4), r=[pb], w=[sg])

                for _ in rgen:
                    pass
            S.dma(POOL, mix_nsa[:, :, 512 * n:512 * n + 512].rearrange("c p t -> p c t"), sg.ap[:, 0:4, :], r=[sg])
            S.dma(POOL, mix_ret[:, :, 512 * n:512 * n + 512].rearrange("c p t -> p c t"), sg.ap[:, 4:8, :], r=[sg])
        S.emit()
        print("mix ops:", {e: len(S.ops[e]) for e in ENGS}, "sems:", S.nsem)


def build_mix(T):
    nc = bass.Bass("TRN2", target_bir_lowering=False)
    D = {}
    mix_decl(nc, D, T, "", "", True)
    x = nc.dram_tensor("x", [T, 1024], F32, kind="ExternalInput").ap()
    mixT = nc.dram_tensor("mixT", [8, 64, T], BF16, kind="ExternalOutput").ap()
    emit_mix(nc, D, T, x, (mixT[0:4], mixT[4:8]), "", "")
    return nc


DFF = 2816
NFC = DFF // 128
FG = 2
NG = NFC // FG


def ffn_decl(nc, D, NE, pf):
    def din(name, shape, dt=F32):
        if name not in D:
            D[name] = nc.dram_tensor(name, list(shape), dt, kind="ExternalInput").ap()
    din(pf + "wout", [1024, 1024]); din(pf + "gam", [128, 1024])
    din(pf + "wg", [NE, 1024, DFF]); din(pf + "wu", [NE, 1024, DFF]); din(pf + "wd", [NE, DFF, 1024])
    din("identf", [128, 128])
    if NE > 1:
        din(pf + "router", [1024, 8]); din(pf + "rb", [128, 8])


def emit_ffn(nc, D0, TOK, NE, x_srcs, mix_srcs, y, pf, sel=None, TS=2048, tag=""):
    moe = NE > 1
    TS = min(TS, TOK)
    NST = TOK // TS
    NSUB = TS // 128
    NTT = TS // 512
    D = dict(D0)
    for k_ in ["wout", "gam", "wg", "wu", "wd", "router", "rb"]:
        if pf + k_ in D0:
            D[k_] = D0[pf + k_]
    blend = len(x_srcs) == 2

    with ExitStack() as es:
        S = Sched(nc, es, tag)
        sb = S.sb
        gam = sb("gam_s", [128, 1024], F32)
        IDF = sb("IDF", [128, 128], F32)
        CM05 = sb("CM05", [128, 8], F32)
        wout = sb("wout_s", [128, 8, 1024], BF16)
        yacc = sb("yacc", [128, NSUB, 1024], F32)
        h2T = sb("h2T", [128, 8, TS], BF16)
        cmb = sb("cmb", [128, NSUB, 8], F32)
        if moe:
            rt = sb("rt_s", [128, 8, 8], F32)
            rb = sb("rb_s", [128, 8], F32)
        banks = [S.ps(f"pb{i}") for i in range(8)]

        class Rot:
            def __init__(self, name, shape, dt, n):
                self.b = [sb(f"{name}{i}", shape, dt) for i in range(n)]
                self.i = 0

            def get(self):
                self.i += 1
                return self.b[self.i % len(self.b)]

        xbuf = Rot("xb", [128, 1024], F32, 2)
        if blend:
            xbuf2 = Rot("xb2", [128, 1024], F32, 1)
            mxb2 = Rot("mxb2", [128, 8, 128], BF16, 1)
            SEL = sb("SEL", [128, 2], F32)
            S.dma(SP, SEL.ap[:], D0[sel][:, :], w=[SEL])
        mxb = Rot("mxb", [128, 8, 128], BF16, 2)
        h2f = Rot("h2f", [128, 1024], F32, 1)
        h2tf = Rot("h2tf", [128, 8, 128], F32, 1)
        sm = Rot("sm", [128, 16], F32, 4)
        lgb = Rot("lgb", [128, 8], F32, 2)
        m8 = Rot("m8", [128, 8], F32, 2)
        c1b = Rot("c1b", [128, 8], F32, 2)
        WG = Rot("WG", [128, 8, FG * 128], BF16, 3)
        WU = Rot("WU", [128, 8, FG * 128], BF16, 3)
        WD = Rot("WD", [128, FG, 1024], BF16, 3)
        AT = Rot("AT", [128, FG, TS], BF16, 2)
        sgb = Rot("sgb", [128, 512], F32, 2)

        S.dma(SP, gam.ap[:], D["gam"][:, :], w=[gam])
        S.dma(SP, IDF.ap[:], D["identf"][:, :], w=[IDF])
        S.dma(POOL, wout.ap[:], D["wout"].rearrange("(c p) n -> p c n", p=128), w=[wout])
        S.op(POOL, "memset", ap=CM05.ap[:], constant=-0.5, w=[CM05])
        S.op(POOL, "memset", ap=cmb.ap[:], constant=1.0, w=[cmb])
        if moe:
            S.dma(SP, rt.ap[:], D["router"].rearrange("(kc p) n -> p kc n", p=128), w=[rt])
            S.dma(SP, rb.ap[:], D["rb"][:, :], w=[rb])

        for stile in range(NST):
            t0 = stile * TS
            for s in range(NSUB):
                r0 = t0 + 128 * s
                xb_ = xbuf.get(); mx = mxb.get()
                S.dma(SP, xb_.ap[:], x_srcs[0][r0:r0 + 128, :], w=[xb_])
                S.dma(SP, mx.ap[:], mix_srcs[0][:, :, r0:r0 + 128].rearrange("c p t -> p c t"), w=[mx])
                if blend:
                    xb2 = xbuf2.get(); mx2 = mxb2.get()
                    S.dma(SP, xb2.ap[:], x_srcs[1][r0:r0 + 128, :], w=[xb2])
                    S.dma(SP, mx2.ap[:], mix_srcs[1][:, :, r0:r0 + 128].rearrange("c p t -> p c t"), w=[mx2])
                    S.op(POOL, "tensor_scalar", out=xb_.ap[:], in0=xb_.ap[:], scalar1=SEL.ap[:, 0:1], scalar2=None,
                         op0=ALU.mult, r=[xb_, SEL], w=[xb_])
                    S.op(DVE, "scalar_tensor_tensor", out=xb_.ap[:], in0=xb2.ap[:], scalar=SEL.ap[:, 1:2], in1=xb_.ap[:],
                         op0=ALU.mult, op1=ALU.add, r=[xb2, SEL, xb_], w=[xb_])
                    S.op(POOL, "tensor_scalar", out=mx.ap[:], in0=mx.ap[:], scalar1=SEL.ap[:, 0:1], scalar2=None,
                         op0=ALU.mult, r=[mx, SEL], w=[mx])
                    S.op(DVE, "scalar_tensor_tensor", out=mx.ap[:], in0=mx2.ap[:], scalar=SEL.ap[:, 1:2], in1=mx.ap[:],
                         op0=ALU.mult, op1=ALU.add, r=[mx2, SEL, mx], w=[mx])
                for half in range(2):
                    pb = banks[half]
                    for c in range(8):
                        S.op(PE, "matmul", out=pb.ap[:, :], lhsT=mx.ap[:, c, :], rhs=wout.ap[:, c, 512 * half:512 * half + 512],
                             start=(c == 0), stop=(c == 7), r=[mx, wout], w=[pb])
                    S.op(DVE, "tensor_tensor", out=yacc.ap[:, s, 512 * half:512 * half + 512], in0=pb.ap[:, :],
                         in1=xb_.ap[:, 512 * half:512 * half + 512], op=ALU.add, r=[pb, xb_], w=[yacc])
                hf = h2f.get(); st = sm.get()
                S.op(ACT, "activation", out=hf.ap[:], in_=yacc.ap[:, s, :], func=AF.Square, accum_out=st.ap[:, 0:1],
                     r=[yacc], w=[hf, st])
                S.op(DVE, "tensor_scalar", out=st.ap[:, 1:2], in0=st.ap[:, 0:1], scalar1=1.0 / 1024, scalar2=EPS,
                     op0=ALU.mult, op1=ALU.add, r=[st], w=[st])
                S.op(POOL, "tensor_tensor", out=st.ap[:, 2:3], in0=st.ap[:, 1:2], in1=CM05.ap[:, 0:1], op=ALU.pow,
                     r=[st, CM05], w=[st])
                S.op(DVE, "scalar_tensor_tensor", out=hf.ap[:], in0=yacc.ap[:, s, :], scalar=st.ap[:, 2:3], in1=gam.ap[:],
                     op0=ALU.mult, op1=ALU.mult, r=[yacc, st, gam], w=[hf])
                for hh in range(2):
                    pb = banks[2 + hh]
                    for k4 in range(4):
                        kc = 4 * hh + k4
                        S.op(PE, "transpose", out=pb.ap[:, 128 * k4:128 * k4 + 128], in_=hf.ap[:, 128 * kc:128 * kc + 128],
                             identity=IDF.ap[:], r=[hf, IDF], w=[pb])
                    S.op(ACT, "activation", out=h2T.ap[:, 4 * hh:4 * hh + 4, 128 * s:128 * s + 128],
                         in_=pb.ap[:, :].rearrange("p (k t) -> p k t", k=4), func=AF.Copy, r=[pb], w=[h2T])
                    if moe:
                        if hh == 0:
                            htf = h2tf.get()
                        S.op(DVE, "tensor_copy", out=htf.ap[:, 4 * hh:4 * hh + 4, :],
                             in_=pb.ap[:, :].rearrange("p (k t) -> p k t", k=4), r=[pb], w=[htf])
                if moe:
                    pr = banks[4]
                    for kc in range(8):
                        S.op(PE, "matmul", out=pr.ap[:, 0:8], lhsT=htf.ap[:, kc, :], rhs=rt.ap[:, kc, :], start=(kc == 0),
                             stop=(kc == 7), r=[htf, rt], w=[pr])
                    lg = lgb.get(); mm = m8.get(); c1 = c1b.get(); st2 = sm.get()
                    S.op(DVE, "tensor_tensor", out=lg.ap[:], in0=pr.ap[:, 0:8], in1=rb.ap[:], op=ALU.add, r=[pr, rb], w=[lg])
                    S.op(DVE, "max", out=mm.ap[:], in_=lg.ap[:], r=[lg], w=[mm])
                    S.op(DVE, "tensor_tensor", out=st2.ap[:, 0:1], in0=mm.ap[:, 0:1], in1=mm.ap[:, 1:2], op=ALU.subtract,
                         r=[mm], w=[st2])
                    S.op(ACT, "activation", out=st2.ap[:, 1:2], in_=st2.ap[:, 0:1], func=AF.Sigmoid, r=[st2], w=[st2])
                    S.op(DVE, "tensor_scalar", out=st2.ap[:, 2:3], in0=st2.ap[:, 1:2], scalar1=-1.0, scalar2=1.0,
                         op0=ALU.mult, op1=ALU.add, r=[st2], w=[st2])
                    S.op(DVE, "tensor_scalar", out=c1.ap[:], in0=lg.ap[:], scalar1=mm.ap[:, 0:1], scalar2=st2.ap[:, 1:2],
                         op0=ALU.is_equal, op1=ALU.mult, r=[lg, mm, st2], w=[c1])
                    S.op(DVE, "tensor_scalar", out=cmb.ap[:, s, :], in0=lg.ap[:], scalar1=mm.ap[:, 1:2], scalar2=st2.ap[:, 2:3],
                         op0=ALU.is_equal, op1=ALU.mult, r=[lg, mm, st2], w=[cmb])
                    S.op(DVE, "tensor_tensor", out=cmb.ap[:, s, :], in0=cmb.ap[:, s, :], in1=c1.ap[:], op=ALU.add,
                         r=[cmb, c1], w=[cmb])
            groups = [(e, fg) for e in range(NE) for fg in range(NG)]

            def load(e, fg):
                wg_ = WG.get(); wu_ = WU.get(); wd_ = WD.get()
                c0 = fg * FG * 128
                S.dma(POOL, wg_.ap[:], D["wg"][e].rearrange("(kc p) n -> p kc n", p=128)[:, :, c0:c0 + FG * 128], w=[wg_])
                S.dma(POOL, wu_.ap[:], D["wu"][e].rearrange("(kc p) n -> p kc n", p=128)[:, :, c0:c0 + FG * 128], w=[wu_])
                S.dma(POOL, wd_.ap[:], D["wd"][e].rearrange("(fc p) n -> p fc n", p=128)[:, fg * FG:fg * FG + FG, :], w=[wd_])
                return wg_, wu_, wd_

            def gu(w3):
                wg_, wu_, wd_ = w3
                at = AT.get()
                for tt in range(NTT):
                    for fc in range(FG):
                        pg = banks[(tt * FG + fc) % 2]
                        pu = banks[2 + (tt * FG + fc) % 2]
                        for kc in range(8):
                            S.op(PE, "matmul", out=pg.ap[:, :], lhsT=wg_.ap[:, kc, 128 * fc:128 * fc + 128],
                                 rhs=h2T.ap[:, kc, 512 * tt:512 * tt + 512], start=(kc == 0), stop=(kc == 7), r=[wg_, h2T], w=[pg])
                        for kc in range(8):
                            S.op(PE, "matmul", out=pu.ap[:, :], lhsT=wu_.ap[:, kc, 128 * fc:128 * fc + 128],
                                 rhs=h2T.ap[:, kc, 512 * tt:512 * tt + 512], start=(kc == 0), stop=(kc == 7), r=[wu_, h2T], w=[pu])
                        sg = sgb.get()
                        S.op(ACT, "activation", out=sg.ap[:], in_=pg.ap[:, :], func=AF.Silu, r=[pg], w=[sg])
                        S.op(DVE, "tensor_tensor", out=at.ap[:, fc, 512 * tt:512 * tt + 512], in0=pu.ap[:, :], in1=sg.ap[:],
                             op=ALU.mult, r=[pu, sg], w=[at])
                return at

            dcnt = [0]

            def down(e, w3, at):
                wd_ = w3[2]
                for s in range(NSUB):
                    for half in range(2):
                        dcnt[0] += 1
                        pd = banks[4 + dcnt[0] % 4]
                        for fc in range(FG):
                            S.op(PE, "matmul", out=pd.ap[:, :], lhsT=at.ap[:, fc, 128 * s:128 * s + 128],
                                 rhs=wd_.ap[:, fc, 512 * half:512 * half + 512], start=(fc == 0), stop=(fc == FG - 1),
                                 r=[at, wd_], w=[pd])
                        S.op(DVE, "scalar_tensor_tensor", out=yacc.ap[:, s, 512 * half:512 * half + 512], in0=pd.ap[:, :],
                             scalar=cmb.ap[:, s, e:e + 1], in1=yacc.ap[:, s, 512 * half:512 * half + 512], op0=ALU.mult,
                             op1=ALU.add, r=[pd, cmb, yacc], w=[yacc])

            w_cur = load(*groups[0])
            w_nxt = load(*groups[1]) if len(groups) > 1 else None
            at_prev = None
            prev = None
            for gi, (e, fg) in enumerate(groups):
                at = gu(w_cur)
                if prev is not None:
                    down(*prev)
                prev = (e, w_cur, at)
                w_cur = w_nxt
                if gi + 2 < len(groups):
                    w_nxt = load(*groups[gi + 2])
            down(*prev)
            for s in range(NSUB):
                r0 = t0 + 128 * s
                S.dma(SP, y[r0:r0 + 128, :], yacc.ap[:, s, :], r=[yacc])
        S.emit()
        print("ffn ops:", {e: len(S.ops[e]) for e in ENGS}, "sems:", S.nsem)


def build_ffn(TOK, NE, TS=2048):
    nc = bass.Bass("TRN2", target_bir_lowering=False)
    D = {}
    ffn_decl(nc, D, NE, "")
    x = nc.dram_tensor("x", [TOK, 1024], F32, kind="ExternalInput").ap()
    mixf = nc.dram_tensor("mixf", [8, 128, TOK], BF16, kind="ExternalInput").ap()
    y = nc.dram_tensor("y", [TOK, 1024], F32, kind="ExternalOutput").ap()
    emit_ffn(nc, D, TOK, NE, [x], [mixf], y, "", TS=TS)
    return nc


def build_fused(T):
    TOK = T // 2
    nc = bass.Bass("TRN2", target_bir_lowering=False)
    D = {}
    x = nc.dram_tensor("x", [T, 1024], F32, kind="ExternalInput").ap()
    D["sel"] = nc.dram_tensor("sel", [128, 2], F32, kind="ExternalInput").ap()
    first = True
    for l in range(2):
        for g in range(2):
            mix_decl(nc, D, T, f"m{l}{g}_", f"t{g}_", first)
            first = False
    ffn_decl(nc, D, 1, "f0_")
    ffn_decl(nc, D, 8, "f1_")
    y = nc.dram_tensor("y", [TOK, 1024], F32, kind="ExternalOutput").ap()
    mixs = nc.dram_tensor("mixs", [16, 64, T], BF16, kind="Internal").ap()
    x1s = nc.dram_tensor("x1s", [T, 1024], F32, kind="Internal").ap()
    mix8 = mixs.rearrange("(c two) p t -> c (two p) t", two=2)
    for g in range(2):
        emit_mix(nc, D, T, x, (mixs[4 * g:4 * g + 4], mixs[8 + 4 * g:8 + 4 * g + 4]), f"m0{g}_", f"t{g}_", tag=f"a{g}")
    emit_ffn(nc, D, T, 1, [x], [mix8], x1s, "f0_", tag="b")
    for g in range(2):
        emit_mix(nc, D, T, x1s, (mixs[4 * g:4 * g + 4], mixs[8 + 4 * g:8 + 4 * g + 4]), f"m1{g}_", f"t{g}_", tag=f"c{g}")
    emit_ffn(nc, D, TOK, 8, [x1s[0:TOK], x1s[TOK:T]], [mix8[:, :, 0:TOK], mix8[:, :, TOK:T]], y, "f1_", sel="sel", tag="d")
    return nc


def fused_inputs(T, b, hf, xb, p, tabs):
    d = {"x": xb, "sel": np.ascontiguousarray(np.broadcast_to(np.array([1.0 - hf, float(hf)], np.float32)[None, :], (128, 2)))}
    shared = ["kx", "kxc", "mcaus", "mwin", "mc", "eall", "ov", "am", "bd", "ident"]
    for k_ in shared:
        d[k_] = tabs[0][k_]
    for g in range(2):
        for k_ in TG_NAMES:
            d[f"t{g}_{k_}"] = tabs[g][k_]
        for l in range(2):
            for k_, v in mix_weights(l, g, p).items():
                d[f"m{l}{g}_{k_}"] = v
    rep = lambda v, n: np.ascontiguousarray(np.broadcast_to(v[None, :], (128, n)))
    d["identf"] = np.eye(128, dtype=np.float32)
    d["f0_wout"] = p["w_out"][0]; d["f0_gam"] = rep(p["norm_ffn_g"][0], 1024)
    d["f0_wg"] = p["ffn_w_gate"]; d["f0_wu"] = p["ffn_w_up"]; d["f0_wd"] = p["ffn_w_down"]
    d["f1_wout"] = p["w_out"][1]; d["f1_gam"] = rep(p["norm_ffn_g"][1], 1024)
    d["f1_wg"] = p["moe_w_gate"][0]; d["f1_wu"] = p["moe_w_up"][0]; d["f1_wd"] = p["moe_w_down"][0]
    d["f1_router"] = p["moe_router"][0]; d["f1_rb"] = rep(p["moe_router_b"][0], 8)
    return d


_CACHE = {}


def kernel(x, norm_mix_g, w_in, q_norm_g, k_norm_g, cmp_pos, w_cmp, ret_norm_g, w_out,
           norm_ffn_g, ffn_w_gate, ffn_w_up, ffn_w_down,
           moe_router, moe_router_b, moe_w_gate, moe_w_up, moe_w_down):
    f32 = lambda a: np.ascontiguousarray(np.asarray(a, dtype=np.float32))
    p = {"norm_mix_g": f32(norm_mix_g), "w_in": f32(w_in), "q_norm_g": f32(q_norm_g), "k_norm_g": f32(k_norm_g),
         "cmp_pos": f32(cmp_pos), "w_cmp": f32(w_cmp), "ret_norm_g": f32(ret_norm_g), "w_out": f32(w_out),
         "norm_ffn_g": f32(norm_ffn_g), "ffn_w_gate": f32(ffn_w_gate), "ffn_w_up": f32(ffn_w_up),
         "ffn_w_down": f32(ffn_w_down), "moe_router": f32(moe_router), "moe_router_b": f32(moe_router_b),
         "moe_w_gate": f32(moe_w_gate), "moe_w_up": f32(moe_w_up), "moe_w_down": f32(moe_w_down)}
    xc = f32(x)
    B, T, _ = xc.shape
    TOK = T // 2
    if T not in _CACHE:
        _CACHE[T] = build_fused(T)
    nc = _CACHE[T]
    tabs = [mix_tables(T, g) for g in range(2)]
    ims = [fused_inputs(T, c // 2, c % 2, xc[c // 2], p, tabs) for c in range(8)]
    res = run_bass_kernel_spmd(nc, ims, core_ids=list(range(8))).results
    out = np.empty_like(xc)
    for c in range(8):
        b, hf = c // 2, c % 2
        out[b, hf * TOK:(hf + 1) * TOK] = np.asarray(res[c]["y"])
    return out
```

```python
import numpy as np
import ml_dtypes
from contextlib import ExitStack
import concourse.bass as bass
import concourse.mybir as mybir
from concourse.bass_utils import run_bass_kernel_spmd

F32 = mybir.dt.float32
BF16 = mybir.dt.bfloat16
AF = mybir.ActivationFunctionType
ALU = mybir.AluOpType
AX = mybir.AxisListType
NPBF = ml_dtypes.bfloat16

PE, ACT, DVE, POOL, SP = "tensor", "scalar", "vector", "gpsimd", "sync"
ENGS = [PE, ACT, DVE, POOL, SP]
SEM_LIMIT = 30000


class Buf:
    __slots__ = ("ap", "w", "r", "rd", "excl", "name")

    def __init__(self, ap, excl=False, name=""):
        self.ap = ap
        self.w = None
        self.r = {}
        self.rd = []
        self.excl = excl
        self.name = name

    def __getitem__(self, k):
        return self.ap[k]


class Op:
    __slots__ = ("eng", "fn", "deps", "inc", "tok", "dma", "slotdep")

    def __init__(self, eng, fn, dma):
        self.eng = eng
        self.fn = fn
        self.deps = []
        self.inc = False
        self.tok = None
        self.dma = dma
        self.slotdep = None


class Sched:
    def __init__(self, nc, es, tag=""):
        self.tag = tag
        self.nc = nc
        self.es = es
        self.ops = {e: [] for e in ENGS}
        self.n_dma_sems = 6

    def sb(self, name, shape, dt, excl=False):
        t = self.es.enter_context(self.nc.sbuf_tensor(self.tag + name, list(shape), dt))
        return Buf(t, excl, name)

    def ps(self, name, shape=(128, 512), dt=F32):
        t = self.es.enter_context(self.nc.psum_tensor(self.tag + name, list(shape), dt))
        return Buf(t, True, name)

    def op(self, eng, fn, r=(), w=(), dma=False, **kw):
        if isinstance(fn, str):
            name = fn
            fn = lambda e, name=name, kw=kw: getattr(e, name)(**kw)
        o = Op(eng, fn, dma)
        deps = o.deps

        def add(d):
            if d is None:
                return
            if d.eng == eng and not d.dma and eng == PE:
                return
            if d not in deps:
                deps.append(d)

        for b in r:
            add(b.w)
            if b.excl:
                for e2, d in b.r.items():
                    if e2 != eng:
                        add(d)
        for b in w:
            add(b.w)
            for e2, d in b.r.items():
                if e2 != eng or dma or d.dma:
                    add(d)
            for d in b.rd:
                add(d)
        for b in r:
            if dma:
                b.rd.append(o)
            else:
                b.r[eng] = o
        for b in w:
            b.w = o
            b.r = {}
            b.rd = []
        self.ops[eng].append(o)
        return o

    def dma(self, eng, out, in_, r=(), w=(), **kw):
        return self.op(eng, lambda e, out=out, in_=in_, kw=kw: e.dma_start(out=out, in_=in_, **kw), r=r, w=w, dma=True)

    def emit(self):
        nc, es = self.nc, self.es
        for e in ENGS:
            for o in self.ops[e]:
                for d in o.deps:
                    d.inc = True
                if o.dma:
                    o.inc = True
        sems = {}
        nsem = [0]

        def newsem(tag):
            nsem[0] += 1
            return es.enter_context(nc.semaphore(f"{self.tag}s_{tag}_{nsem[0]}"))

        for e in ENGS:
            cur = None
            cnt = 0
            dma_sems = []
            dma_cnt = []
            dma_last = []
            k = 0
            for o in self.ops[e]:
                if not o.inc:
                    continue
                if o.dma:
                    i = k % self.n_dma_sems
                    k += 1
                    if len(dma_sems) <= i:
                        dma_sems.append(newsem(e + "d"))
                        dma_cnt.append(0)
                        dma_last.append(None)
                    if dma_cnt[i] + 16 > SEM_LIMIT:
                        dma_sems[i] = newsem(e + "d")
                        dma_cnt[i] = 0
                    o.slotdep = dma_last[i]
                    dma_cnt[i] += 16
                    o.tok = (dma_sems[i], dma_cnt[i], 16)
                    dma_last[i] = o
                else:
                    if cur is None or cnt + 1 > SEM_LIMIT:
                        cur = newsem(e)
                        cnt = 0
                    cnt += 1
                    o.tok = (cur, cnt, 1)
        self.nsem = nsem[0]
        blk = es.enter_context(nc.Block())

        def make(ename):
            def body(eng):
                waited = {}
                for o in self.ops[ename]:
                    dl = list(o.deps)
                    if o.slotdep is not None:
                        dl.append(o.slotdep)
                    for d in dl:
                        s, v, _ = d.tok
                        key = id(s)
                        if waited.get(key, 0) >= v:
                            continue
                        waited[key] = v
                        eng.wait_ge(s, v)
                    inst = o.fn(eng)
                    if o.inc:
                        s, v, step = o.tok
                        inst.then_inc(s, step)
                for o in self.ops[ename]:
                    if o.dma:
                        s, v, _ = o.tok
                        if waited.get(id(s), 0) < v:
                            waited[id(s)] = v
                            eng.wait_ge(s, v)
            return body

        for ename in ENGS:
            if self.ops[ename]:
                getattr(blk, ename)(make(ename))


EPS = 1e-6
NEGM = -30000.0


def mix_tables(T, g):
    NB = T // 128
    tb = {}
    slopes = np.array([2.0 ** -(4 * g + r + 1) for r in range(4)], np.float64)
    pos = np.arange(T)
    kx = np.stack([np.ones(T), np.ones(T), 128.0 * (pos // 128), (pos % 128).astype(np.float64)])
    tb["kx"] = kx.astype(NPBF)
    NS = 512
    s = np.arange(NS)
    cpos = 16 * s + 15
    kxc = np.stack([np.ones(NS), np.ones(NS), 128.0 * (cpos // 128), (cpos % 128).astype(np.float64)])
    tb["kxc"] = kxc.astype(NPBF)
    ql = np.arange(128)
    qx = np.zeros((4, 4, 128))
    for h in range(4):
        qx[0, h] = -slopes[h] * 128.0
        qx[1, h] = -slopes[h] * ql
        qx[2, h] = slopes[h]
        qx[3, h] = slopes[h]
    tb["qx"] = qx.reshape(4, 512).astype(NPBF)
    k = np.arange(128)[:, None]
    q = np.arange(128)[None, :]
    mca = np.where(k <= q, 0.0, NEGM)
    mwi = np.where(k > q, 0.0, NEGM)
    tb["mcaus"] = np.tile(mca, (1, 4)).astype(NPBF)
    tb["mwin"] = np.tile(mwi, (1, 4)).astype(NPBF)
    u = np.arange(2048 + 128)[None, :]
    sl = np.arange(128)[:, None]
    mc = np.where(16 * sl + 15 <= u, 0.0, NEGM)
    mc0 = mc.copy()
    mc0[0, :] = NEGM
    tb["mc"] = np.stack([mc0, mc], 1).astype(NPBF)
    blk = np.arange(128)[:, None]
    key = np.arange(T)[None, :]
    tb["eall"] = (key // 64 == blk).astype(np.float32).astype(NPBF)
    ov = np.zeros((128, 4, 129), np.float32)
    for ct in range(4):
        for s_l in range(128):
            c = 128 * ct + s_l - 1
            if c < 0:
                continue
            cs, ce = 16 * c, 16 * c + 32
            for b_ in range(cs // 64, min(127, (ce - 1) // 64) + 1):
                o = min(ce, 64 * b_ + 64) - max(cs, 64 * b_)
                if o > 0:
                    ov[s_l, ct, b_] = o / 32.0
        ov[:, ct, 128] = 1.0
    tb["ov"] = np.concatenate([ov[:, :, 128:129], ov[:, :, 0:128]], 2).astype(NPBF)
    am = np.zeros((NB, 128, 128), np.float32)
    for i in range(NB):
        t = 128 * i + np.arange(128)
        cur = t // 64
        b_ = np.arange(128)[None, :]
        forced = (b_ == 0) | (b_ == cur[:, None]) | (b_ == cur[:, None] - 1)
        valid = b_ * 64 <= t[:, None]
        am[i] = np.where(forced, 1e9, np.where(valid, 0.0, -1e9))
    tb["am"] = am
    hh = 4 * g + np.arange(4)
    log_g = np.log1p(-np.exp2(-5.0 - hh.astype(np.float64)))
    idx = np.arange(128, dtype=np.float64)
    sc = 32 ** -0.5
    dec = np.zeros((128, 4, 128))
    for h in range(4):
        d = idx[None, :] - idx[:, None]
        dec[:, h, :] = np.where(d >= 0, np.exp(np.maximum(d, 0) * log_g[h]), 0.0) * sc
    tb["decT"] = dec.astype(np.float32)
    xi = np.zeros((128, 128))
    zeta = np.zeros((128, 128))
    gc = np.zeros((128, 1))
    bd = np.zeros((128, 4, 128))
    for h in range(4):
        xi[32 * h:32 * h + 32, :] = np.exp((idx + 1) * log_g[h])[None, :] * sc
        zeta[:, 32 * h:32 * h + 32] = np.exp((127 - idx) * log_g[h])[:, None]
        gc[32 * h:32 * h + 32] = np.exp(128 * log_g[h])
        bd[32 * h:32 * h + 32, h, :] = 1.0
    tb["xi"] = xi.astype(np.float32)
    tb["zeta"] = zeta.astype(np.float32)
    tb["gc"] = gc.astype(np.float32)
    tb["bd"] = bd.astype(np.float32).astype(NPBF)
    tb["ident"] = np.eye(128, dtype=np.float32).astype(NPBF)
    return tb


def mix_weights(l, g, p):
    w_in = p["w_in"][l]
    offs = np.cumsum([0, 512, 128, 128, 128, 128, 128, 128, 24, 256, 256, 512, 512])
    q0, kc0, vc0, ks0, vs0, kw0, vw0, gt0, rq0, rk0, rv0, rg0 = offs[:12]
    cq = w_in[:, q0 + 256 * g: q0 + 256 * g + 256]
    sl = lambda o, w: w_in[:, o + w * g: o + w * g + w]
    wtm = np.concatenate([cq, sl(ks0, 64), sl(kw0, 64), sl(vs0, 64), sl(vw0, 64), sl(rv0, 256), sl(rg0, 256),
                          sl(rk0, 128), sl(gt0, 12)], 1)
    wfm = np.concatenate([sl(kc0, 64), sl(vc0, 64), sl(rq0, 128), sl(rk0, 128)], 1)
    wc = p["w_cmp"][l]
    wbd = np.zeros((128, 32, 128), np.float32)
    wbd[0:64, :, 0:64] = wc[0].reshape(32, 64, 64).transpose(1, 0, 2)
    wbd[64:128, :, 64:128] = wc[1].reshape(32, 64, 64).transpose(1, 0, 2)
    wcf = np.concatenate([wc[0], wc[1]], 1).reshape(16, 128, 128).transpose(1, 0, 2)
    cp = p["cmp_pos"][l].reshape(2, 2048)
    posf = cp.reshape(2, 16, 128).transpose(2, 0, 1)
    rep = lambda v: np.ascontiguousarray(np.broadcast_to(v[None, :], (128, v.shape[0])))
    gqk = np.concatenate([np.tile(p["q_norm_g"][l], 4), p["k_norm_g"][l][1], p["k_norm_g"][l][2]])
    return {
        "gam": rep(p["norm_mix_g"][l]), "wtm": np.ascontiguousarray(wtm), "wfm": np.ascontiguousarray(wfm),
        "wbd": wbd, "wcf": np.ascontiguousarray(wcf), "posf": np.ascontiguousarray(posf),
        "gqk": rep(gqk), "gkc": rep(p["k_norm_g"][l][0]),
        "gret": rep(p["ret_norm_g"][l][256 * g:256 * g + 256]),
    }


def mix_decl(nc, D, T, pw, pt, shared):
    NB = T // 128

    def din(name, shape, dt=F32):
        if name not in D:
            D[name] = nc.dram_tensor(name, list(shape), dt, kind="ExternalInput").ap()

    for nm_, shp in [("gam", [128, 1024]), ("wtm", [1024, 1164]), ("wfm", [1024, 384]), ("wbd", [128, 32, 128]),
                     ("wcf", [128, 16, 128]), ("posf", [128, 2, 16]), ("gqk", [128, 384]), ("gkc", [128, 64]),
                     ("gret", [128, 256])]:
        din(pw + nm_, shp)
    din(pt + "qx", [4, 512], BF16); din(pt + "decT", [128, 4, 128]); din(pt + "xi", [128, 128])
    din(pt + "zeta", [128, 128]); din(pt + "gc", [128, 1])
    if shared:
        din("kx", [4, T], BF16); din("kxc", [4, 512], BF16)
        din("mcaus", [128, 512], BF16); din("mwin", [128, 512], BF16); din("mc", [128, 2, 2176], BF16)
        din("eall", [128, T], BF16); din("ov", [128, 4, 129], BF16); din("am", [NB, 128, 128])
        din("bd", [128, 4, 128], BF16); din("ident", [128, 128], BF16)


WNAMES = ["gam", "wtm", "wfm", "wbd", "wcf", "posf", "gqk", "gkc", "gret"]
TG_NAMES = ["qx", "decT", "xi", "zeta", "gc"]


def emit_mix(nc, D0, T, x, mix_out, pw, pt, tag=""):
    NT = T // 512
    NB = T // 128
    D = dict(D0)
    for k_ in WNAMES:
        D[k_] = D0[pw + k_]
    for k_ in TG_NAMES:
        D[k_] = D0[pt + k_]
    mix_nsa, mix_ret = mix_out

    with ExitStack() as es:
        S = Sched(nc, es, tag)
        sb = S.sb
        gam = sb("gam_s", [128, 1024], F32)
        wtm = sb("wtm_s", [128, 8, 1164], BF16)
        wfm = sb("wfm_s", [128, 8, 384], BF16)
        wbd = sb("wbd_s", [128, 32, 128], BF16)
        wcf = sb("wcf_s", [128, 16, 128], BF16)
        posf = sb("posf_s", [128, 2, 16], BF16)
        gqk = sb("gqk_s", [128, 384], F32)
        gkc = sb("gkc_s", [128, 64], F32)
        gret = sb("gret_s", [128, 256], F32)
        KS = sb("KS", [68, T], BF16)
        KW = sb("KW", [68, T], BF16)
        KC = sb("KC", [68, 512], BF16)
        VSW = sb("VSW", [128, NB, 2, 65], BF16)
        CV = sb("CV", [128, 4, 193], BF16)
        KCVC = sb("KCVC", [128, 16 + 2048], BF16)
        EALL = sb("EALL", [128, T], BF16)
        QXB = sb("QXB", [68, 512], BF16)
        MCA = sb("MCA", [128, 512], BF16); MWI = sb("MWI", [128, 512], BF16)
        MC = sb("MC", [128, 2, 2176], BF16)
        DEC = sb("DEC", [128, 4, 128], F32); XI = sb("XI", [128, 128], F32); ZETA = sb("ZETA", [128, 128], F32)
        GC = sb("GC", [128, 1], F32); BD = sb("BD", [128, 4, 128], BF16)
        IDN = sb("IDN", [128, 128], BF16)
        ONES1 = sb("ONES1", [1, 128], BF16)
        BIASR = sb("BIASR", [1, 128], BF16)
        CM05 = sb("CM05", [128, 8], F32)
        STATE = sb("STATE", [128, 64], F32)
        SBD = sb("SBD", [128, 4, 64], BF16)
        banks = [S.ps(f"pb{i}") for i in range(8)]
        OB, PB4, RB = banks[4:8], banks[0:3], banks[3]
        cnt = {"s": 0, "m": 0}

        def mbank():
            cnt["m"] += 1
            return PB4[cnt["m"] % 3]

        def sbank():
            return mbank()

        class Rot:
            def __init__(self, name, shape, dt, n):
                self.b = [sb(f"{name}{i}", shape, dt) for i in range(n)]
                self.i = 0

            def get(self):
                self.i += 1
                return self.b[self.i % len(self.b)]

        xbuf = Rot("xb", [128, 1024], F32, 2)
        hb = Rot("hb", [128, 1024], BF16, 4)
        hT = Rot("hT", [128, 8, 512], BF16, 1)
        sm = Rot("sm", [128, 16], F32, 24)
        sqb = Rot("sqb", [128, 384], F32, 2)
        qkn = Rot("qkn", [128, 384], BF16, 4)
        t384 = Rot("t384", [128, 384], F32, 2)
        QT = [sb(f"QT{i}", [68, 512], BF16) for i in range(4)]
        RQT = Rot("RQT", [128, 512], BF16, 1); RKT = Rot("RKT", [128, 512], BF16, 1)
        RV = Rot("RV", [128, 4, 256], BF16, 1)
        G2 = Rot("G2", [128, 4, 256], F32, 1)
        KZ = Rot("KZ", [128, 4, 128], BF16, 1)
        GT = Rot("GT", [128, 4, 12], F32, 1)
        PT = Rot("PT", [128, 512], BF16, 3)
        NMT = Rot("NMT", [128, 512], BF16, 2)
        AMB = Rot("AMB", [128, 128], F32, 2)
        impb = Rot("impb", [128, 128], F32, 2)
        wk = Rot("wk", [128, 128], F32, 1)
        nmq = Rot("nmq", [128, 128], BF16, 1)
        m8 = Rot("m8", [128, 16], F32, 2)
        acc = Rot("acc", [128, 256], F32, 2)
        tmp256 = Rot("tmp256", [128, 256], F32, 2)
        fb = Rot("fb", [128, 16], F32, 4)
        nsab = Rot("nsab", [128, 256], BF16, 2)
        RQBD = Rot("RQBD", [128, 4, 128], BF16, 2)
        RQX = Rot("RQX", [128, 128], BF16, 2)
        IND = Rot("IND", [128, 4, 128], BF16, 2)
        sq256 = Rot("sq256", [128, 256], F32, 1)
        retb = Rot("retb", [128, 256], BF16, 2)
        red64 = Rot("red64", [128, 64], F32, 1)
        t4 = Rot("t4", [128, 4, 64], F32, 1)
        stg = Rot("stg", [64, 8, 512], BF16, 1)
        kcn = Rot("kcn", [128, 64], BF16, 1)
        kct = Rot("kct", [128, 64], F32, 1)

        def ld(eng, dst, src, **kw):
            S.dma(eng, dst.ap[:] if isinstance(dst, Buf) else dst, src, w=[dst] if isinstance(dst, Buf) else (), **kw)

        ld(SP, gam, D["gam"][:, :])
        S.dma(POOL, wtm.ap[:], D["wtm"].rearrange("(kc p) n -> p kc n", p=128), w=[wtm])
        S.dma(POOL, wfm.ap[:], D["wfm"].rearrange("(kc p) n -> p kc n", p=128), w=[wfm])
        S.dma(POOL, wbd.ap[:], D["wbd"][:, :, :], w=[wbd])
        S.dma(POOL, wcf.ap[:], D["wcf"][:, :, :], w=[wcf])
        S.dma(POOL, posf.ap[:], D["posf"][:, :, :], w=[posf])
        ld(SP, gqk, D["gqk"][:, :]); ld(SP, gkc, D["gkc"][:, :]); ld(SP, gret, D["gret"][:, :])
        S.dma(SP, KS.ap[64:68, :], D["kx"][:, :], w=[KS])
        S.dma(SP, KW.ap[64:68, :], D["kx"][:, :], w=[KW])
        S.dma(SP, KC.ap[64:68, :], D["kxc"][:, :], w=[KC])
        S.dma(SP, QXB.ap[64:68, :], D["qx"][:, :], w=[QXB])
        for q_ in QT:
            S.dma(SP, q_.ap[64:68, :], D["qx"][:, :], w=[q_])
        ld(SP, MCA, D["mcaus"][:, :]); ld(SP, MWI, D["mwin"][:, :]); ld(SP, MC, D["mc"][:, :, :])
        ld(SP, EALL, D["eall"][:, :])
        ld(SP, DEC, D["decT"][:, :, :]); ld(SP, XI, D["xi"][:, :]); ld(SP, ZETA, D["zeta"][:, :])
        ld(SP, GC, D["gc"][:, :]); ld(SP, BD, D["bd"][:, :, :]); ld(SP, IDN, D["ident"][:, :])
        S.op(POOL, "memset", ap=VSW.ap[:], constant=1.0, w=[VSW])
        S.op(POOL, "memset", ap=CV.ap[:], constant=0.0, w=[CV])
        S.dma(SP, CV.ap[:, :, 64:193], D["ov"][:, :, :], w=[CV])
        S.op(POOL, "memset", ap=KCVC.ap[:], constant=0.0, w=[KCVC])
        S.op(POOL, "memset", ap=KC.ap[0:64, :], constant=0.0, w=[KC])
        S.op(POOL, "memset", ap=ONES1.ap[:], constant=1.0, w=[ONES1])
        S.op(POOL, "memset", ap=CM05.ap[:], constant=-0.5, w=[CM05])
        S.op(POOL, "memset", ap=STATE.ap[:], constant=0.0, w=[STATE])
        S.op(POOL, "memset", ap=SBD.ap[:], constant=0.0, w=[SBD])
        S.op(DVE, "tensor_scalar", out=gqk.ap[:, 0:256], in0=gqk.ap[:, 0:256], scalar1=0.125, scalar2=None,
                                            op0=ALU.mult, r=[gqk], w=[gqk])
        b_ = mbank()
        for kv in range(2):
            for c in range(16):
                S.op(PE, "matmul", out=b_.ap[0:1, 64 * kv:64 * kv + 64], lhsT=posf.ap[:, kv, c:c + 1],
                                                         rhs=wcf.ap[:, c, 64 * kv:64 * kv + 64],
                                                         start=(c == 0 and kv == 0), stop=(c == 15), skip_group_check=True,
                     r=[posf, wcf], w=[b_])
        S.op(ACT, "activation", out=BIASR.ap[:], in_=b_.ap[0:1, 0:128], func=AF.Copy, r=[b_], w=[BIASR])

        def rstd_pow(dst, src, n, scale):
            pass

        def ret_gen(j, rqt, rkt, rv, g2, kz, sg):
            tok = slice(128 * j, 128 * j + 128)
            rqbd = RQBD.get(); rqx = RQX.get()
            S.op(POOL, "tensor_tensor", out=rqbd.ap[:], in0=rqt.ap[:, tok].unsqueeze(1).broadcast_to([128, 4, 128]),
                 in1=BD.ap[:], op=ALU.mult, r=[rqt, BD], w=[rqbd])
            S.op(POOL, "tensor_tensor", out=rqx.ap[:], in0=rqt.ap[:, tok], in1=XI.ap[:], op=ALU.mult, r=[rqt, XI], w=[rqx])
            yield
            pin = RB
            S.op(PE, "matmul", out=pin.ap[:, :], lhsT=rkt.ap[:, tok], rhs=rqbd.ap[:].rearrange("p h i -> p (h i)"),
                 start=True, stop=True, r=[rkt, rqbd], w=[pin])
            yield
            ind = IND.get()
            S.op(DVE, "tensor_tensor", out=ind.ap[:].rearrange("p h i -> p (h i)"), in0=pin.ap[:, :],
                 in1=DEC.ap[:].rearrange("p h i -> p (h i)"), op=ALU.mult, r=[pin, DEC], w=[ind])
            yield
            po = RB
            S.op(PE, "matmul", out=po.ap[:, 0:256], lhsT=rqx.ap[:], rhs=SBD.ap[:].rearrange("p h e -> p (h e)"),
                 start=True, stop=False, r=[rqx, SBD], w=[po])
            for h in range(4):
                S.op(PE, "matmul", out=po.ap[:, 64 * h:64 * h + 64], lhsT=ind.ap[:, h, :], rhs=rv.ap[:, j, 64 * h:64 * h + 64],
                     start=False, stop=(h == 3), r=[ind, rv], w=[po])
            pkv = RB
            S.op(PE, "matmul", out=pkv.ap[:, 256:512], lhsT=kz.ap[:, j, :], rhs=rv.ap[:, j, :], start=True, stop=True,
                 r=[kz, rv], w=[pkv])
            yield
            sq = sq256.get(); st = sm.get()
            S.op(ACT, "activation", out=sq.ap[:], in_=po.ap[:, 0:256], func=AF.Square, r=[po], w=[sq])
            t4_ = t4.get(); r64 = red64.get()
            S.op(DVE, "tensor_tensor", out=t4_.ap[:], in0=pkv.ap[:, 256:512].rearrange("p (h e) -> p h e", h=4),
                 in1=BD.ap[:, :, 0:64], op=ALU.mult, r=[pkv, BD], w=[t4_])
            S.op(DVE, "tensor_reduce", out=r64.ap[:], in_=t4_.ap[:].rearrange("p h e -> p e h"), axis=AX.X, op=ALU.add,
                 r=[t4_], w=[r64])
            S.op(DVE, "scalar_tensor_tensor", out=STATE.ap[:], in0=STATE.ap[:], scalar=GC.ap[:, 0:1], in1=r64.ap[:],
                 op0=ALU.mult, op1=ALU.add, r=[STATE, GC, r64], w=[STATE])
            S.op(POOL, "tensor_tensor", out=SBD.ap[:], in0=STATE.ap[:].unsqueeze(1).broadcast_to([128, 4, 64]),
                 in1=BD.ap[:, :, 0:64], op=ALU.mult, r=[STATE, BD], w=[SBD])
            S.op(DVE, "tensor_reduce", out=st.ap[:, 0:4], in_=sq.ap[:].rearrange("p (h d) -> p h d", h=4), axis=AX.X,
                 op=ALU.add, r=[sq], w=[st])
            S.op(DVE, "tensor_scalar", out=st.ap[:, 4:8], in0=st.ap[:, 0:4], scalar1=1.0 / 64, scalar2=EPS, op0=ALU.mult,
                 op1=ALU.add, r=[st], w=[st])
            S.op(POOL, "tensor_tensor", out=st.ap[:, 8:12], in0=st.ap[:, 4:8], in1=CM05.ap[:, 0:4], op=ALU.pow,
                 r=[st, CM05], w=[st])
            yield
            tm_ = tmp256.get()
            S.op(DVE, "tensor_tensor", out=tm_.ap[:].rearrange("p (h d) -> p h d", h=4),
                 in0=po.ap[:, 0:256].rearrange("p (h d) -> p h d", h=4),
                 in1=st.ap[:, 8:12].unsqueeze(2).broadcast_to([128, 4, 64]), op=ALU.mult, r=[po, st], w=[tm_])
            rb = retb.get()
            S.op(POOL, "tensor_tensor", out=rb.ap[:], in0=tm_.ap[:], in1=g2.ap[:, j, :], op=ALU.mult, r=[tm_, g2], w=[rb])
            yield
            pb2 = RB
            pbb2 = pb2.ap[:].bitcast(BF16)
            for h in range(4):
                S.op(PE, "transpose", out=pbb2[0:64, 128 * h:128 * h + 128], in_=rb.ap[:, 64 * h:64 * h + 64],
                     identity=IDN.ap[:], r=[rb, IDN], w=[pb2])
            yield
            S.op(DVE, "tensor_copy", out=sg.ap[:, 4:8, 128 * j:128 * j + 128],
                 in_=pbb2[0:64, 0:512].rearrange("p (h q) -> p h q", h=4), r=[pb2], w=[sg])

        def nsa_out(ac, j, sg):
            nb_ = nsab.get()
            S.op(ACT, "activation", out=nb_.ap[:], in_=ac.ap[:], func=AF.Copy, r=[ac], w=[nb_])
            pb = mbank()
            pbb = pb.ap[:].bitcast(BF16)
            for h in range(4):
                S.op(PE, "transpose", out=pbb[0:64, 128 * h:128 * h + 128], in_=nb_.ap[:, 64 * h:64 * h + 64],
                     identity=IDN.ap[:], r=[nb_, IDN], w=[pb])
            S.op(DVE, "tensor_copy", out=sg.ap[:, 0:4, 128 * j:128 * j + 128],
                 in_=pbb[0:64, 0:512].rearrange("p (h q) -> p h q", h=4), r=[pb], w=[sg])

        pend = []

        for n in range(NT):
            hTn = hT.get()
            xs_ = {}; sts = {}; hs = {}
            for pair in ((0, 1), (2, 3)):
                for j in pair:
                    xb_ = xbuf.get()
                    r0 = 512 * n + 128 * j
                    S.dma(SP, xb_.ap[:], x[r0:r0 + 128, :], w=[xb_])
                    xs_[j] = xb_
                for j in pair:
                    h_ = hb.get(); st = sm.get()
                    S.op(ACT, "activation", out=h_.ap[:], in_=xs_[j].ap[:], func=AF.Square, accum_out=st.ap[:, 0:1],
                         r=[xs_[j]], w=[h_, st])
                    sts[j] = st; hs[j] = h_
                for j in pair:
                    st = sts[j]
                    S.op(DVE, "tensor_scalar", out=st.ap[:, 1:2], in0=st.ap[:, 0:1], scalar1=1.0 / 1024, scalar2=EPS,
                         op0=ALU.mult, op1=ALU.add, r=[st], w=[st])
                    S.op(POOL, "tensor_tensor", out=st.ap[:, 2:3], in0=st.ap[:, 1:2], in1=CM05.ap[:, 0:1], op=ALU.pow,
                         r=[st, CM05], w=[st])
                for j in pair:
                    S.op(DVE, "scalar_tensor_tensor", out=hs[j].ap[:], in0=xs_[j].ap[:], scalar=sts[j].ap[:, 2:3], in1=gam.ap[:],
                         op0=ALU.mult, op1=ALU.mult, r=[xs_[j], sts[j], gam], w=[hs[j]])
            for j in range(4):
                pb = mbank()
                pbb = pb.ap[:].bitcast(BF16)
                for kc in range(8):
                    S.op(PE, "transpose", out=pbb[:, 128 * kc:128 * kc + 128], in_=hs[j].ap[:, 128 * kc:128 * kc + 128],
                         identity=IDN.ap[:], r=[hs[j], IDN], w=[pb])
                S.op(ACT, "activation", out=hTn.ap[:, :, 128 * j:128 * j + 128], in_=pbb.rearrange("p (k t) -> p k t", k=8),
                     func=AF.Copy, r=[pb], w=[hTn])
            rqt = RQT.get(); rkt = RKT.get()
            m_ = n % 4
            if m_ == 0 and n > 0:
                S.op(POOL, "tensor_copy", out=KCVC.ap[:, 0:16], in_=KCVC.ap[:, 2048:2064], r=[KCVC], w=[KCVC])
            for ci, dst in enumerate([None, rqt, rkt]):
                pb = mbank()
                for kc in range(8):
                    S.op(PE, "matmul", out=pb.ap[:, :], lhsT=wfm.ap[:, kc, 128 * ci:128 * ci + 128], rhs=hTn.ap[:, kc, :],
                         start=(kc == 0), stop=(kc == 7), r=[wfm, hTn], w=[pb])
                if ci == 0:
                    S.op(ACT, "activation", out=KCVC.ap[:, 16 + 512 * m_:16 + 512 * m_ + 512], in_=pb.ap[:, :], func=AF.Copy,
                         r=[pb], w=[KCVC])
                else:
                    S.op(DVE, "tensor_copy", out=dst.ap[:], in_=pb.ap[:, :], r=[pb], w=[dst])
            rv = RV.get(); g2 = G2.get(); kz = KZ.get(); gt = GT.get()
            qks = []
            for j in range(4):
                blk = 4 * n + j
                tok = slice(128 * j, 128 * j + 128)
                pa = mbank()
                for kc in range(8):
                    S.op(PE, "matmul", out=pa.ap[:, :], lhsT=hTn.ap[:, kc, tok], rhs=wtm.ap[:, kc, 0:512], start=(kc == 0),
                         stop=(kc == 7), r=[hTn, wtm], w=[pa])
                sq = sqb.get(); st = sm.get(); raw = t384.get()
                S.op(ACT, "activation", out=sq.ap[:], in_=pa.ap[:, 0:384], func=AF.Square, r=[pa], w=[sq])
                S.op(DVE, "tensor_copy", out=raw.ap[:], in_=pa.ap[:, 0:384], r=[pa], w=[raw])
                S.op(ACT, "activation", out=VSW.ap[:, blk, :, 0:64], in_=pa.ap[:, 384:512].rearrange("p (a d) -> p a d", a=2),
                     func=AF.Copy, r=[pa], w=[VSW])
                pbk = mbank()
                for kc in range(8):
                    S.op(PE, "matmul", out=pbk.ap[:, :], lhsT=hTn.ap[:, kc, tok], rhs=wtm.ap[:, kc, 512:1024], start=(kc == 0),
                         stop=(kc == 7), r=[hTn, wtm], w=[pbk])
                S.op(DVE, "tensor_copy", out=rv.ap[:, j, :], in_=pbk.ap[:, 0:256], r=[pbk], w=[rv])
                S.op(ACT, "activation", out=g2.ap[:, j, :], in_=pbk.ap[:, 256:512], func=AF.Silu, r=[pbk], w=[g2])
                pc = mbank()
                for kc in range(8):
                    S.op(PE, "matmul", out=pc.ap[:, 0:140], lhsT=hTn.ap[:, kc, tok], rhs=wtm.ap[:, kc, 1024:1164], start=(kc == 0),
                         stop=(kc == 7), r=[hTn, wtm], w=[pc])
                S.op(DVE, "tensor_tensor", out=kz.ap[:, j, :], in0=pc.ap[:, 0:128], in1=ZETA.ap[:], op=ALU.mult,
                     r=[pc, ZETA], w=[kz])
                S.op(ACT, "activation", out=gt.ap[:, j, :], in_=pc.ap[:, 128:140], func=AF.Sigmoid, r=[pc], w=[gt])
                S.op(DVE, "tensor_reduce", out=st.ap[:, 0:6], in_=sq.ap[:].rearrange("p (a d) -> p a d", a=6), axis=AX.X,
                     op=ALU.add, r=[sq], w=[st])
                S.op(DVE, "tensor_scalar", out=st.ap[:, 6:12], in0=st.ap[:, 0:6], scalar1=1.0 / 64, scalar2=EPS, op0=ALU.mult,
                     op1=ALU.add, r=[st], w=[st])
                st2 = sm.get()
                S.op(POOL, "tensor_tensor", out=st2.ap[:, 0:6], in0=st.ap[:, 6:12], in1=CM05.ap[:, 0:6], op=ALU.pow,
                     r=[st, CM05], w=[st2])
                S.op(POOL, "tensor_tensor", out=g2.ap[:, j, :], in0=g2.ap[:, j, :], in1=gret.ap[:], op=ALU.mult,
                     r=[g2, gret], w=[g2])
                S.op(DVE, "tensor_tensor", out=raw.ap[:].rearrange("p (a d) -> p a d", a=6),
                     in0=raw.ap[:].rearrange("p (a d) -> p a d", a=6),
                     in1=st2.ap[:, 0:6].unsqueeze(2).broadcast_to([128, 6, 64]), op=ALU.mult, r=[raw, st2], w=[raw])
                qk = qkn.get()
                S.op(POOL, "tensor_tensor", out=qk.ap[:], in0=raw.ap[:], in1=gqk.ap[:], op=ALU.mult, r=[raw, gqk], w=[qk])
                qks.append(qk)
            for j in range(4):
                blk = 4 * n + j
                qk = qks[j]
                pt_ = mbank()
                ptb = pt_.ap[:].bitcast(BF16)
                for a_ in range(6):
                    S.op(PE, "transpose", out=ptb[0:64, 128 * a_:128 * a_ + 128], in_=qk.ap[:, 64 * a_:64 * a_ + 64],
                         identity=IDN.ap[:], r=[qk, IDN], w=[pt_])
                S.op(ACT, "activation", out=QT[j].ap[0:64, :], in_=ptb[0:64, 0:512], func=AF.Copy, r=[pt_], w=[QT[j]])
                S.op(DVE, "tensor_copy", out=KS.ap[0:64, 128 * blk:128 * blk + 128], in_=ptb[0:64, 512:640], r=[pt_], w=[KS])
                S.op(DVE, "tensor_copy", out=KW.ap[0:64, 128 * blk:128 * blk + 128], in_=ptb[0:64, 640:768], r=[pt_], w=[KW])
                S.op(DVE, "tensor_scalar", out=QT[j].ap[64:65, :], in0=QXB.ap[64:65, :], scalar1=float(blk), scalar2=None,
                     op0=ALU.mult, r=[QXB], w=[QT[j]])
            ct = n // 4
            pcm = mbank()
            S.op(PE, "matmul", out=pcm.ap[:, 0:128], lhsT=ONES1.ap[0:1, :], rhs=BIASR.ap[0:1, :],
                                                 start=True, stop=False, r=[ONES1, BIASR], w=[pcm])
            for l in range(32):
                S.op(PE, "matmul", out=pcm.ap[:, 0:128], lhsT=KCVC.ap[:, l:l + 2033:16],
                                                          rhs=wbd.ap[:, l, :], start=False, stop=(l == 31),
                     r=[KCVC, wbd], w=[pcm])
            S.op(ACT, "activation", out=CV.ap[:, ct, 0:64], in_=pcm.ap[:, 64:128], func=AF.Copy,
                 r=[pcm], w=[CV])
            kt_ = kct.get(); st = sm.get()
            S.op(ACT, "activation", out=kt_.ap[:], in_=pcm.ap[:, 0:64], func=AF.Square,
                                                                      accum_out=st.ap[:, 0:1], r=[pcm], w=[kt_, st])
            S.op(DVE, "tensor_scalar", out=st.ap[:, 1:2], in0=st.ap[:, 0:1], scalar1=1.0 / 64, scalar2=EPS,
                                                       op0=ALU.mult, op1=ALU.add, r=[st], w=[st])
            S.op(POOL, "tensor_tensor", out=st.ap[:, 2:3], in0=st.ap[:, 1:2], in1=CM05.ap[:, 0:1],
                                                        op=ALU.pow, r=[st, CM05], w=[st])
            kn = kcn.get()
            S.op(DVE, "scalar_tensor_tensor", out=kn.ap[:], in0=pcm.ap[:, 0:64],
                                                                              scalar=st.ap[:, 2:3], in1=gkc.ap[:],
                                                                              op0=ALU.mult, op1=ALU.mult,
                 r=[pcm, st, gkc], w=[kn])
            pk = mbank()
            pkb = pk.ap[:].bitcast(BF16)
            S.op(PE, "transpose", out=pkb[0:64, 0:128], in_=kn.ap[:, 0:64], identity=IDN.ap[:],
                 r=[kn, IDN], w=[pk])
            S.op(DVE, "tensor_copy", out=KC.ap[0:64, 128 * ct:128 * ct + 128], in_=pkb[0:64, 0:128],
                 r=[pk], w=[KC])

            sg = stg.get()
            for j in range(4):
                i = 4 * n + j
                q0 = 128 * i
                qt = QT[j]
                jobs = []
                nct = (128 * i + 112) // 2048 + 1
                O_c = [OB[0], OB[1]]
                O_w = OB[2]
                O_s = OB[3]
                for c in range(nct):
                    jobs.append(("c", c, c == nct - 1))
                for kt in range(max(0, i - 4), i + 1):
                    jobs.append(("w", kt, kt == i or kt == i - 4))
                for kt in range(0, i + 1):
                    jobs.append(("s", kt, kt == i))
                first = {"c": True, "w": True, "s": True}
                nm_holder = {}

                def emit_qk(job):
                    br, kt, special = job
                    sbk = sbank()
                    if br == "c":
                        S.op(PE, "matmul", out=sbk.ap[:, :], lhsT=KC.ap[0:68, 128 * kt:128 * kt + 128], rhs=qt.ap[0:68, :],
                                                    start=True, stop=not special, r=[KC, qt], w=[sbk])
                        if special:
                            o = q0 - 2048 * kt
                            var = 0 if kt == 0 else 1
                            for h in range(4):
                                S.op(PE, "matmul", out=sbk.ap[:, 128 * h:128 * h + 128], lhsT=IDN.ap[:],
                                                                 rhs=MC.ap[:, var, o:o + 128], start=False, stop=(h == 3),
                                     r=[IDN, MC], w=[sbk])
                    elif br == "w":
                        S.op(PE, "matmul", out=sbk.ap[:, :], lhsT=KW.ap[0:68, 128 * kt:128 * kt + 128], rhs=qt.ap[0:68, :],
                                                    start=True, stop=not special, r=[KW, qt], w=[sbk])
                        if special:
                            msk = MCA if kt == i else MWI
                            S.op(PE, "matmul", out=sbk.ap[:, :], lhsT=IDN.ap[:], rhs=msk.ap[:], start=False, stop=True,
                                 r=[IDN, msk], w=[sbk])
                    else:
                        nm = nm_holder["nm"]
                        S.op(PE, "matmul", out=sbk.ap[:, :], lhsT=KS.ap[0:68, 128 * kt:128 * kt + 128], rhs=qt.ap[0:68, :],
                                                    start=True, stop=False, r=[KS, qt], w=[sbk])
                        S.op(PE, "matmul", out=sbk.ap[:, :], lhsT=EALL.ap[:, 128 * kt:128 * kt + 128], rhs=nm.ap[:],
                                                    start=False, stop=not special, r=[EALL, nm], w=[sbk])
                        if special:
                            S.op(PE, "matmul", out=sbk.ap[:, :], lhsT=IDN.ap[:], rhs=MCA.ap[:], start=False, stop=True,
                                 r=[IDN, MCA], w=[sbk])
                    return sbk

                def emit_exp_pv(job, sbk):
                    br, kt, special = job
                    p = PT.get()
                    S.op(ACT, "activation", out=p.ap[:], in_=sbk.ap[:, :], func=AF.Exp, r=[sbk], w=[p])
                    fst = first[br]
                    first[br] = False
                    if br == "c":
                        for h in range(4):
                            ob = O_c[h // 2]
                            c0 = 193 * (h % 2)
                            S.op(PE, "matmul", out=
                                ob.ap[:, c0:c0 + 193], lhsT=p.ap[:, 128 * h:128 * h + 128], rhs=CV.ap[:, kt, :],
                                start=(fst and h % 2 == 0), stop=False, skip_group_check=True, r=[p, CV], w=[ob])
                    else:
                        ob = O_w if br == "w" else O_s
                        a = 1 if br == "w" else 0
                        for h in range(4):
                            S.op(PE, "matmul", out=ob.ap[:, 65 * h:65 * h + 65], lhsT=p.ap[:, 128 * h:128 * h + 128],
                                                             rhs=VSW.ap[:, kt, a, :], start=(fst and h == 0), stop=False,
                                                             skip_group_check=True, r=[p, VSW], w=[ob])

                ac = acc.get()
                gtj = gt.ap[:, j, :].rearrange("p (r b) -> p r b", b=3)

                def epilogue(br):
                    f = fb.get()
                    bi = {"c": 0, "s": 1, "w": 2}[br]
                    if br == "c":
                        srcs = [(O_c[0], 0), (O_c[0], 193), (O_c[1], 0), (O_c[1], 193)]
                        for h, (ob, c0) in enumerate(srcs):
                            S.op(DVE, "tensor_scalar",
                                out=f.ap[:, h:h + 1], in0=ob.ap[:, c0 + 64:c0 + 65], scalar1=1e-30, scalar2=None, op0=ALU.add,
                                r=[ob], w=[f])
                    else:
                        ob = O_w if br == "w" else O_s
                        S.op(DVE, "tensor_scalar",
                            out=f.ap[:, 0:4], in0=ob.ap[:, 0:260].rearrange("p (h c) -> p h c", c=65)[:, :, 64],
                            scalar1=1e-30, scalar2=None, op0=ALU.add, r=[ob], w=[f])
                    S.op(DVE, "reciprocal", out=f.ap[:, 4:8], in_=f.ap[:, 0:4], r=[f], w=[f])
                    S.op(DVE, "tensor_tensor", out=f.ap[:, 8:12], in0=f.ap[:, 4:8], in1=gtj[:, :, bi], op=ALU.mult,
                         r=[f, gt], w=[f])
                    dst = ac if br == "c" else tmp256.get()
                    if br == "c":
                        def part_b():
                            for h, (ob, c0) in enumerate(srcs):
                                S.op(DVE, "tensor_scalar", out=dst.ap[:, 64 * h:64 * h + 64], in0=ob.ap[:, c0:c0 + 64],
                                     scalar1=f.ap[:, 8 + h:9 + h], scalar2=None, op0=ALU.mult, r=[ob, f], w=[dst])
                        nm_holder["part_b"] = part_b
                    else:
                        S.op(DVE, "tensor_tensor",
                            out=dst.ap[:].rearrange("p (h d) -> p h d", h=4),
                            in0=ob.ap[:, 0:260].rearrange("p (h c) -> p h c", c=65)[:, :, 0:64],
                            in1=f.ap[:, 8:12].unsqueeze(2).broadcast_to([128, 4, 64]), op=ALU.mult, r=[ob, f], w=[dst])
                        S.op(POOL, "tensor_tensor", out=ac.ap[:], in0=ac.ap[:], in1=dst.ap[:], op=ALU.add,
                             r=[ac, dst], w=[ac])
                    return f

                def selection(f):
                    am_ = AMB.get()
                    S.dma(SP, am_.ap[:], D["am"][i, :, :], w=[am_])
                    im = impb.get()
                    srcs = [(O_c[0], 0), (O_c[0], 193), (O_c[1], 0), (O_c[1], 193)]
                    for h, (ob, c0) in enumerate(srcs):
                        prev = am_ if h == 0 else im
                        S.op(DVE, "scalar_tensor_tensor",
                            out=im.ap[:], in0=ob.ap[:, c0 + 65:c0 + 193], scalar=f.ap[:, 4 + h:5 + h], in1=prev.ap[:],
                            op0=ALU.mult, op1=ALU.add, r=[ob, f, prev], w=[im])
                    mm = m8.get(); w_ = wk.get()
                    S.op(DVE, "max", out=mm.ap[:, 0:8], in_=im.ap[:], r=[im], w=[mm])
                    S.op(DVE, "match_replace", out=w_.ap[:], in_to_replace=mm.ap[:, 0:8], in_values=im.ap[:],
                                                        imm_value=-3e38, r=[im, mm], w=[w_])
                    S.op(DVE, "max", out=mm.ap[:, 8:16], in_=w_.ap[:], r=[w_], w=[mm])
                    nq = nmq.get()
                    S.op(DVE, "tensor_scalar", out=nq.ap[:], in0=im.ap[:], scalar1=mm.ap[:, 15:16], scalar2=NEGM,
                                                        op0=ALU.is_lt, op1=ALU.mult, r=[im, mm], w=[nq])
                    nm_holder["nq"] = nq

                def selection2():
                    nq = nm_holder["nq"]
                    pb = mbank()
                    pbb = pb.ap[:].bitcast(BF16)
                    S.op(PE, "transpose", out=pbb[:, 0:128], in_=nq.ap[:], identity=IDN.ap[:], r=[nq, IDN], w=[pb])
                    nm = NMT.get()
                    S.op(ACT, "activation", out=nm.ap[:].rearrange("p (h q) -> p h q", h=4),
                         in_=pbb[:, 0:128].unsqueeze(1).broadcast_to([128, 4, 128]), func=AF.Copy, r=[pb], w=[nm])
                    nm_holder["nm"] = nm

                emitted = {}
                rgen = ret_gen(j, rqt, rkt, rv, g2, kz, sg)

                def ensure(kk):
                    if kk >= len(jobs) or kk in emitted:
                        return True
                    if jobs[kk][0] == "s" and "nm" not in nm_holder:
                        return False
                    emitted[kk] = emit_qk(jobs[kk])
                    return True

                ensure(0)
                for k in range(len(jobs)):
                    job = jobs[k]
                    if ensure(k + 1):
                        ensure(k + 2)
                    emit_exp_pv(job, emitted[k])
                    if job[0] == "s":
                        next(rgen, None)
                    br = job[0]
                    if k + 1 == len(jobs) or jobs[k + 1][0] != br:
                        f = epilogue(br)
                        if br == "c":
                            selection(f)
                            nm_holder["part_b"]()
                    if k + 1 < len(jobs) and (k + 1) not in emitted:
                        if "nm" not in nm_holder:
                            while pend:
                                nsa_out(*pend.pop(0), sg)
                            selection2()
                        ensure(k + 1)
                pend.append((ac, j))
                for _ in rgen:
                    pass
            while pend:
                nsa_out(*pend.pop(0), sg)
            S.dma(POOL, mix_nsa[:, :, 512 * n:512 * n + 512].rearrange("c p t -> p c t"), sg.ap[:, 0:4, :], r=[sg])
            S.dma(POOL, mix_ret[:, :, 512 * n:512 * n + 512].rearrange("c p t -> p c t"), sg.ap[:, 4:8, :], r=[sg])
        S.emit()
        print("mix ops:", {e: len(S.ops[e]) for e in ENGS}, "sems:", S.nsem)


def build_mix(T):
    nc = bass.Bass("TRN2", target_bir_lowering=False)
    D = {}
    mix_decl(nc, D, T, "", "", True)
    x = nc.dram_tensor("x", [T, 1024], F32, kind="ExternalInput").ap()
    mixT = nc.dram_tensor("mixT", [8, 64, T], BF16, kind="ExternalOutput").ap()
    emit_mix(nc, D, T, x, (mixT[0:4], mixT[4:8]), "", "")
    return nc


DFF = 2816
NFC = DFF // 128
FG = 2
NG = NFC // FG


def ffn_decl(nc, D, NE, pf):
    def din(name, shape, dt=F32):
        if name not in D:
            D[name] = nc.dram_tensor(name, list(shape), dt, kind="ExternalInput").ap()
    din(pf + "wout", [1024, 1024]); din(pf + "gam", [128, 1024])
    din(pf + "wg", [NE, 1024, DFF]); din(pf + "wu", [NE, 1024, DFF]); din(pf + "wd", [NE, DFF, 1024])
    din("identf", [128, 128])
    if NE > 1:
        din(pf + "router", [1024, 8]); din(pf + "rb", [128, 8])


def emit_ffn(nc, D0, TOK, NE, x_srcs, mix_srcs, y, pf, sel=None, TS=2048, tag=""):
    moe = NE > 1
    TS = min(TS, TOK)
    NST = TOK // TS
    NSUB = TS // 128
    NTT = TS // 512
    D = dict(D0)
    for k_ in ["wout", "gam", "wg", "wu", "wd", "router", "rb"]:
        if pf + k_ in D0:
            D[k_] = D0[pf + k_]
    blend = len(x_srcs) == 2

    with ExitStack() as es:
        S = Sched(nc, es, tag)
        sb = S.sb
        gam = sb("gam_s", [128, 1024], F32)
        IDF = sb("IDF", [128, 128], F32)
        CM05 = sb("CM05", [128, 8], F32)
        wout = sb("wout_s", [128, 8, 1024], BF16)
        yacc = sb("yacc", [128, NSUB, 1024], F32)
        h2T = sb("h2T", [128, 8, TS], BF16)
        cmb = sb("cmb", [128, NSUB, 8], F32)
        if moe:
            rt = sb("rt_s", [128, 8, 8], F32)
            rb = sb("rb_s", [128, 8], F32)
        banks = [S.ps(f"pb{i}") for i in range(8)]

        class Rot:
            def __init__(self, name, shape, dt, n):
                self.b = [sb(f"{name}{i}", shape, dt) for i in range(n)]
                self.i = 0

            def get(self):
                self.i += 1
                return self.b[self.i % len(self.b)]

        xbuf = Rot("xb", [128, 1024], F32, 2)
        if blend:
            xbuf2 = Rot("xb2", [128, 1024], F32, 1)
            mxb2 = Rot("mxb2", [128, 8, 128], BF16, 1)
            SEL = sb("SEL", [128, 2], F32)
            S.dma(SP, SEL.ap[:], D0[sel][:, :], w=[SEL])
        mxb = Rot("mxb", [128, 8, 128], BF16, 2)
        h2f = Rot("h2f", [128, 1024], F32, 1)
        h2tf = Rot("h2tf", [128, 8, 128], F32, 1)
        sm = Rot("sm", [128, 16], F32, 4)
        lgb = Rot("lgb", [128, 8], F32, 2)
        m8 = Rot("m8", [128, 8], F32, 2)
        c1b = Rot("c1b", [128, 8], F32, 2)
        WG = Rot("WG", [128, 8, FG * 128], BF16, 3)
        WU = Rot("WU", [128, 8, FG * 128], BF16, 3)
        WD = Rot("WD", [128, FG, 1024], BF16, 3)
        AT = Rot("AT", [128, FG, TS], BF16, 2)
        sgb = Rot("sgb", [128, 512], F32, 2)

        S.dma(SP, gam.ap[:], D["gam"][:, :], w=[gam])
        S.dma(SP, IDF.ap[:], D["identf"][:, :], w=[IDF])
        S.dma(POOL, wout.ap[:], D["wout"].rearrange("(c p) n -> p c n", p=128), w=[wout])
        S.op(POOL, "memset", ap=CM05.ap[:], constant=-0.5, w=[CM05])
        S.op(POOL, "memset", ap=cmb.ap[:], constant=1.0, w=[cmb])
        if moe:
            S.dma(SP, rt.ap[:], D["router"].rearrange("(kc p) n -> p kc n", p=128), w=[rt])
            S.dma(SP, rb.ap[:], D["rb"][:, :], w=[rb])

        for stile in range(NST):
            t0 = stile * TS
            for s in range(NSUB):
                r0 = t0 + 128 * s
                xb_ = xbuf.get(); mx = mxb.get()
                S.dma(SP, xb_.ap[:], x_srcs[0][r0:r0 + 128, :], w=[xb_])
                S.dma(SP, mx.ap[:], mix_srcs[0][:, :, r0:r0 + 128].rearrange("c p t -> p c t"), w=[mx])
                if blend:
                    xb2 = xbuf2.get(); mx2 = mxb2.get()
                    S.dma(SP, xb2.ap[:], x_srcs[1][r0:r0 + 128, :], w=[xb2])
                    S.dma(SP, mx2.ap[:], mix_srcs[1][:, :, r0:r0 + 128].rearrange("c p t -> p c t"), w=[mx2])
                    S.op(POOL, "tensor_scalar", out=xb_.ap[:], in0=xb_.ap[:], scalar1=SEL.ap[:, 0:1], scalar2=None,
                         op0=ALU.mult, r=[xb_, SEL], w=[xb_])
                    S.op(DVE, "scalar_tensor_tensor", out=xb_.ap[:], in0=xb2.ap[:], scalar=SEL.ap[:, 1:2], in1=xb_.ap[:],
                         op0=ALU.mult, op1=ALU.add, r=[xb2, SEL, xb_], w=[xb_])
                    S.op(POOL, "tensor_scalar", out=mx.ap[:], in0=mx.ap[:], scalar1=SEL.ap[:, 0:1], scalar2=None,
                         op0=ALU.mult, r=[mx, SEL], w=[mx])
                    S.op(DVE, "scalar_tensor_tensor", out=mx.ap[:], in0=mx2.ap[:], scalar=SEL.ap[:, 1:2], in1=mx.ap[:],
                         op0=ALU.mult, op1=ALU.add, r=[mx2, SEL, mx], w=[mx])
                for half in range(2):
                    pb = banks[half]
                    for c in range(8):
                        S.op(PE, "matmul", out=pb.ap[:, :], lhsT=mx.ap[:, c, :], rhs=wout.ap[:, c, 512 * half:512 * half + 512],
                             start=(c == 0), stop=(c == 7), r=[mx, wout], w=[pb])
                    S.op(DVE, "tensor_tensor", out=yacc.ap[:, s, 512 * half:512 * half + 512], in0=pb.ap[:, :],
                         in1=xb_.ap[:, 512 * half:512 * half + 512], op=ALU.add, r=[pb, xb_], w=[yacc])
                hf = h2f.get(); st = sm.get()
                S.op(ACT, "activation", out=hf.ap[:], in_=yacc.ap[:, s, :], func=AF.Square, accum_out=st.ap[:, 0:1],
                     r=[yacc], w=[hf, st])
                S.op(DVE, "tensor_scalar", out=st.ap[:, 1:2], in0=st.ap[:, 0:1], scalar1=1.0 / 1024, scalar2=EPS,
                     op0=ALU.mult, op1=ALU.add, r=[st], w=[st])
                S.op(POOL, "tensor_tensor", out=st.ap[:, 2:3], in0=st.ap[:, 1:2], in1=CM05.ap[:, 0:1], op=ALU.pow,
                     r=[st, CM05], w=[st])
                S.op(DVE, "scalar_tensor_tensor", out=hf.ap[:], in0=yacc.ap[:, s, :], scalar=st.ap[:, 2:3], in1=gam.ap[:],
                     op0=ALU.mult, op1=ALU.mult, r=[yacc, st, gam], w=[hf])
                for hh in range(2):
                    pb = banks[2 + hh]
                    for k4 in range(4):
                        kc = 4 * hh + k4
                        S.op(PE, "transpose", out=pb.ap[:, 128 * k4:128 * k4 + 128], in_=hf.ap[:, 128 * kc:128 * kc + 128],
                             identity=IDF.ap[:], r=[hf, IDF], w=[pb])
                    S.op(ACT, "activation", out=h2T.ap[:, 4 * hh:4 * hh + 4, 128 * s:128 * s + 128],
                         in_=pb.ap[:, :].rearrange("p (k t) -> p k t", k=4), func=AF.Copy, r=[pb], w=[h2T])
                    if moe:
                        if hh == 0:
                            htf = h2tf.get()
                        S.op(DVE, "tensor_copy", out=htf.ap[:, 4 * hh:4 * hh + 4, :],
                             in_=pb.ap[:, :].rearrange("p (k t) -> p k t", k=4), r=[pb], w=[htf])
                if moe:
                    pr = banks[4]
                    for kc in range(8):
                        S.op(PE, "matmul", out=pr.ap[:, 0:8], lhsT=htf.ap[:, kc, :], rhs=rt.ap[:, kc, :], start=(kc == 0),
                             stop=(kc == 7), r=[htf, rt], w=[pr])
                    lg = lgb.get(); mm = m8.get(); c1 = c1b.get(); st2 = sm.get()
                    S.op(DVE, "tensor_tensor", out=lg.ap[:], in0=pr.ap[:, 0:8], in1=rb.ap[:], op=ALU.add, r=[pr, rb], w=[lg])
                    S.op(DVE, "max", out=mm.ap[:], in_=lg.ap[:], r=[lg], w=[mm])
                    S.op(DVE, "tensor_tensor", out=st2.ap[:, 0:1], in0=mm.ap[:, 0:1], in1=mm.ap[:, 1:2], op=ALU.subtract,
                         r=[mm], w=[st2])
                    S.op(ACT, "activation", out=st2.ap[:, 1:2], in_=st2.ap[:, 0:1], func=AF.Sigmoid, r=[st2], w=[st2])
                    S.op(DVE, "tensor_scalar", out=st2.ap[:, 2:3], in0=st2.ap[:, 1:2], scalar1=-1.0, scalar2=1.0,
                         op0=ALU.mult, op1=ALU.add, r=[st2], w=[st2])
                    S.op(DVE, "tensor_scalar", out=c1.ap[:], in0=lg.ap[:], scalar1=mm.ap[:, 0:1], scalar2=st2.ap[:, 1:2],
                         op0=ALU.is_equal, op1=ALU.mult, r=[lg, mm, st2], w=[c1])
                    S.op(DVE, "tensor_scalar", out=cmb.ap[:, s, :], in0=lg.ap[:], scalar1=mm.ap[:, 1:2], scalar2=st2.ap[:, 2:3],
                         op0=ALU.is_equal, op1=ALU.mult, r=[lg, mm, st2], w=[cmb])
                    S.op(DVE, "tensor_tensor", out=cmb.ap[:, s, :], in0=cmb.ap[:, s, :], in1=c1.ap[:], op=ALU.add,
                         r=[cmb, c1], w=[cmb])
            groups = [(e, fg) for e in range(NE) for fg in range(NG)]

            def load(e, fg):
                wg_ = WG.get(); wu_ = WU.get(); wd_ = WD.get()
                c0 = fg * FG * 128
                S.dma(POOL, wg_.ap[:], D["wg"][e].rearrange("(kc p) n -> p kc n", p=128)[:, :, c0:c0 + FG * 128], w=[wg_])
                S.dma(POOL, wu_.ap[:], D["wu"][e].rearrange("(kc p) n -> p kc n", p=128)[:, :, c0:c0 + FG * 128], w=[wu_])
                S.dma(POOL, wd_.ap[:], D["wd"][e].rearrange("(fc p) n -> p fc n", p=128)[:, fg * FG:fg * FG + FG, :], w=[wd_])
                return wg_, wu_, wd_

            def gu(w3):
                wg_, wu_, wd_ = w3
                at = AT.get()
                for tt in range(NTT):
                    for fc in range(FG):
                        pg = banks[(tt * FG + fc) % 2]
                        pu = banks[2 + (tt * FG + fc) % 2]
                        for kc in range(8):
                            S.op(PE, "matmul", out=pg.ap[:, :], lhsT=wg_.ap[:, kc, 128 * fc:128 * fc + 128],
                                 rhs=h2T.ap[:, kc, 512 * tt:512 * tt + 512], start=(kc == 0), stop=(kc == 7), r=[wg_, h2T], w=[pg])
                        for kc in range(8):
                            S.op(PE, "matmul", out=pu.ap[:, :], lhsT=wu_.ap[:, kc, 128 * fc:128 * fc + 128],
                                 rhs=h2T.ap[:, kc, 512 * tt:512 * tt + 512], start=(kc == 0), stop=(kc == 7), r=[wu_, h2T], w=[pu])
                        sg = sgb.get()
                        S.op(ACT, "activation", out=sg.ap[:], in_=pg.ap[:, :], func=AF.Silu, r=[pg], w=[sg])
                        S.op(DVE, "tensor_tensor", out=at.ap[:, fc, 512 * tt:512 * tt + 512], in0=pu.ap[:, :], in1=sg.ap[:],
                             op=ALU.mult, r=[pu, sg], w=[at])
                return at

            dcnt = [0]

            def down(e, w3, at):
                wd_ = w3[2]
                for s in range(NSUB):
                    for half in range(2):
                        dcnt[0] += 1
                        pd = banks[4 + dcnt[0] % 4]
                        for fc in range(FG):
                            S.op(PE, "matmul", out=pd.ap[:, :], lhsT=at.ap[:, fc, 128 * s:128 * s + 128],
                                 rhs=wd_.ap[:, fc, 512 * half:512 * half + 512], start=(fc == 0), stop=(fc == FG - 1),
                                 r=[at, wd_], w=[pd])
                        S.op(DVE, "scalar_tensor_tensor", out=yacc.ap[:, s, 512 * half:512 * half + 512], in0=pd.ap[:, :],
                             scalar=cmb.ap[:, s, e:e + 1], in1=yacc.ap[:, s, 512 * half:512 * half + 512], op0=ALU.mult,
                             op1=ALU.add, r=[pd, cmb, yacc], w=[yacc])

            w_cur = load(*groups[0])
            w_nxt = load(*groups[1]) if len(groups) > 1 else None
            at_prev = None
            prev = None
            for gi, (e, fg) in enumerate(groups):
                at = gu(w_cur)
                if prev is not None:
                    down(*prev)
                prev = (e, w_cur, at)
                w_cur = w_nxt
                if gi + 2 < len(groups):
                    w_nxt = load(*groups[gi + 2])
            down(*prev)
            for s in range(NSUB):
                r0 = t0 + 128 * s
                S.dma(SP, y[r0:r0 + 128, :], yacc.ap[:, s, :], r=[yacc])
        S.emit()
        print("ffn ops:", {e: len(S.ops[e]) for e in ENGS}, "sems:", S.nsem)


def build_ffn(TOK, NE, TS=2048):
    nc = bass.Bass("TRN2", target_bir_lowering=False)
    D = {}
    ffn_decl(nc, D, NE, "")
    x = nc.dram_tensor("x", [TOK, 1024], F32, kind="ExternalInput").ap()
    mixf = nc.dram_tensor("mixf", [8, 128, TOK], BF16, kind="ExternalInput").ap()
    y = nc.dram_tensor("y", [TOK, 1024], F32, kind="ExternalOutput").ap()
    emit_ffn(nc, D, TOK, NE, [x], [mixf], y, "", TS=TS)
    return nc


def build_fused(T):
    TOK = T // 2
    nc = bass.Bass("TRN2", target_bir_lowering=False)
    D = {}
    x = nc.dram_tensor("x", [T, 1024], F32, kind="ExternalInput").ap()
    D["sel"] = nc.dram_tensor("sel", [128, 2], F32, kind="ExternalInput").ap()
    first = True
    for l in range(2):
        for g in range(2):
            mix_decl(nc, D, T, f"m{l}{g}_", f"t{g}_", first)
            first = False
    ffn_decl(nc, D, 1, "f0_")
    ffn_decl(nc, D, 8, "f1_")
    y = nc.dram_tensor("y", [TOK, 1024], F32, kind="ExternalOutput").ap()
    mixs = nc.dram_tensor("mixs", [16, 64, T], BF16, kind="Internal").ap()
    x1s = nc.dram_tensor("x1s", [T, 1024], F32, kind="Internal").ap()
    mix8 = mixs.rearrange("(c two) p t -> c (two p) t", two=2)
    for g in range(2):
        emit_mix(nc, D, T, x, (mixs[4 * g:4 * g + 4], mixs[8 + 4 * g:8 + 4 * g + 4]), f"m0{g}_", f"t{g}_", tag=f"a{g}")
    emit_ffn(nc, D, T, 1, [x], [mix8], x1s, "f0_", tag="b")
    for g in range(2):
        emit_mix(nc, D, T, x1s, (mixs[4 * g:4 * g + 4], mixs[8 + 4 * g:8 + 4 * g + 4]), f"m1{g}_", f"t{g}_", tag=f"c{g}")
    emit_ffn(nc, D, TOK, 8, [x1s[0:TOK], x1s[TOK:T]], [mix8[:, :, 0:TOK], mix8[:, :, TOK:T]], y, "f1_", sel="sel", tag="d")
    return nc


def fused_inputs(T, b, hf, xb, p, tabs):
    d = {"x": xb, "sel": np.ascontiguousarray(np.broadcast_to(np.array([1.0 - hf, float(hf)], np.float32)[None, :], (128, 2)))}
    shared = ["kx", "kxc", "mcaus", "mwin", "mc", "eall", "ov", "am", "bd", "ident"]
    for k_ in shared:
        d[k_] = tabs[0][k_]
    for g in range(2):
        for k_ in TG_NAMES:
            d[f"t{g}_{k_}"] = tabs[g][k_]
        for l in range(2):
            for k_, v in mix_weights(l, g, p).items():
                d[f"m{l}{g}_{k_}"] = v
    rep = lambda v, n: np.ascontiguousarray(np.broadcast_to(v[None, :], (128, n)))
    d["identf"] = np.eye(128, dtype=np.float32)
    d["f0_wout"] = p["w_out"][0]; d["f0_gam"] = rep(p["norm_ffn_g"][0], 1024)
    d["f0_wg"] = p["ffn_w_gate"]; d["f0_wu"] = p["ffn_w_up"]; d["f0_wd"] = p["ffn_w_down"]
    d["f1_wout"] = p["w_out"][1]; d["f1_gam"] = rep(p["norm_ffn_g"][1], 1024)
    d["f1_wg"] = p["moe_w_gate"][0]; d["f1_wu"] = p["moe_w_up"][0]; d["f1_wd"] = p["moe_w_down"][0]
    d["f1_router"] = p["moe_router"][0]; d["f1_rb"] = rep(p["moe_router_b"][0], 8)
    return d


_CACHE = {}


def kernel(x, norm_mix_g, w_in, q_norm_g, k_norm_g, cmp_pos, w_cmp, ret_norm_g, w_out,
           norm_ffn_g, ffn_w_gate, ffn_w_up, ffn_w_down,
           moe_router, moe_router_b, moe_w_gate, moe_w_up, moe_w_down):
    f32 = lambda a: np.ascontiguousarray(np.asarray(a, dtype=np.float32))
    p = {"norm_mix_g": f32(norm_mix_g), "w_in": f32(w_in), "q_norm_g": f32(q_norm_g), "k_norm_g": f32(k_norm_g),
         "cmp_pos": f32(cmp_pos), "w_cmp": f32(w_cmp), "ret_norm_g": f32(ret_norm_g), "w_out": f32(w_out),
         "norm_ffn_g": f32(norm_ffn_g), "ffn_w_gate": f32(ffn_w_gate), "ffn_w_up": f32(ffn_w_up),
         "ffn_w_down": f32(ffn_w_down), "moe_router": f32(moe_router), "moe_router_b": f32(moe_router_b),
         "moe_w_gate": f32(moe_w_gate), "moe_w_up": f32(moe_w_up), "moe_w_down": f32(moe_w_down)}
    xc = f32(x)
    B, T, _ = xc.shape
    TOK = T // 2
    if T not in _CACHE:
        _CACHE[T] = build_fused(T)
    nc = _CACHE[T]
    tabs = [mix_tables(T, g) for g in range(2)]
    ims = [fused_inputs(T, c // 2, c % 2, xc[c // 2], p, tabs) for c in range(8)]
    res = run_bass_kernel_spmd(nc, ims, core_ids=list(range(8))).results
    out = np.empty_like(xc)
    for c in range(8):
        b, hf = c // 2, c % 2
        out[b, hf * TOK:(hf + 1) * TOK] = np.asarray(res[c]["y"])
    return out
```

```python
import numpy as np
import ml_dtypes
from contextlib import ExitStack
import concourse.bass as bass
import concourse.mybir as mybir
from concourse.bass_utils import run_bass_kernel_spmd

F32 = mybir.dt.float32
BF16 = mybir.dt.bfloat16
AF = mybir.ActivationFunctionType
ALU = mybir.AluOpType
AX = mybir.AxisListType
NPBF = ml_dtypes.bfloat16

PE, ACT, DVE, POOL, SP = "tensor", "scalar", "vector", "gpsimd", "sync"
ENGS = [PE, ACT, DVE, POOL, SP]
SEM_LIMIT = 30000


class Buf:
    __slots__ = ("ap", "w", "r", "rd", "excl", "name")

    def __init__(self, ap, excl=False, name=""):
        self.ap = ap
        self.w = None
        self.r = {}
        self.rd = []
        self.excl = excl
        self.name = name

    def __getitem__(self, k):
        return self.ap[k]


class Op:
    __slots__ = ("eng", "fn", "deps", "inc", "tok", "dma", "slotdep")

    def __init__(self, eng, fn, dma):
        self.eng = eng
        self.fn = fn
        self.deps = []
        self.inc = False
        self.tok = None
        self.dma = dma
        self.slotdep = None


class Sched:
    def __init__(self, nc, es, tag=""):
        self.tag = tag
        self.nc = nc
        self.es = es
        self.ops = {e: [] for e in ENGS}
        self.n_dma_sems = 6

    def sb(self, name, shape, dt, excl=False):
        t = self.es.enter_context(self.nc.sbuf_tensor(self.tag + name, list(shape), dt))
        return Buf(t, excl, name)

    def ps(self, name, shape=(128, 512), dt=F32):
        t = self.es.enter_context(self.nc.psum_tensor(self.tag + name, list(shape), dt))
        return Buf(t, True, name)

    def op(self, eng, fn, r=(), w=(), dma=False, **kw):
        if isinstance(fn, str):
            name = fn
            fn = lambda e, name=name, kw=kw: getattr(e, name)(**kw)
        o = Op(eng, fn, dma)
        deps = o.deps

        def add(d):
            if d is None:
                return
            if d.eng == eng and not d.dma and eng == PE:
                return
            if d not in deps:
                deps.append(d)

        for b in r:
            add(b.w)
            if b.excl:
                for e2, d in b.r.items():
                    if e2 != eng:
                        add(d)
        for b in w:
            add(b.w)
            for e2, d in b.r.items():
                if e2 != eng or dma or d.dma:
                    add(d)
            for d in b.rd:
                add(d)
        for b in r:
            if dma:
                b.rd.append(o)
            else:
                b.r[eng] = o
        for b in w:
            b.w = o
            b.r = {}
            b.rd = []
        self.ops[eng].append(o)
        return o

    def dma(self, eng, out, in_, r=(), w=(), **kw):
        return self.op(eng, lambda e, out=out, in_=in_, kw=kw: e.dma_start(out=out, in_=in_, **kw), r=r, w=w, dma=True)

    def emit(self):
        nc, es = self.nc, self.es
        for e in ENGS:
            for o in self.ops[e]:
                for d in o.deps:
                    d.inc = True
                if o.dma:
                    o.inc = True
        sems = {}
        nsem = [0]

        def newsem(tag):
            nsem[0] += 1
            return es.enter_context(nc.semaphore(f"{self.tag}s_{tag}_{nsem[0]}"))

        for e in ENGS:
            cur = None
            cnt = 0
            dma_sems = []
            dma_cnt = []
            dma_last = []
            k = 0
            for o in self.ops[e]:
                if not o.inc:
                    continue
                if o.dma:
                    i = k % self.n_dma_sems
                    k += 1
                    if len(dma_sems) <= i:
                        dma_sems.append(newsem(e + "d"))
                        dma_cnt.append(0)
                        dma_last.append(None)
                    if dma_cnt[i] + 16 > SEM_LIMIT:
                        dma_sems[i] = newsem(e + "d")
                        dma_cnt[i] = 0
                    o.slotdep = dma_last[i]
                    dma_cnt[i] += 16
                    o.tok = (dma_sems[i], dma_cnt[i], 16)
                    dma_last[i] = o
                else:
                    if cur is None or cnt + 1 > SEM_LIMIT:
                        cur = newsem(e)
                        cnt = 0
                    cnt += 1
                    o.tok = (cur, cnt, 1)
        self.nsem = nsem[0]
        blk = es.enter_context(nc.Block())

        def make(ename):
            def body(eng):
                waited = {}
                for o in self.ops[ename]:
                    dl = list(o.deps)
                    if o.slotdep is not None:
                        dl.append(o.slotdep)
                    for d in dl:
                        s, v, _ = d.tok
                        key = id(s)
                        if waited.get(key, 0) >= v:
                            continue
                        waited[key] = v
                        eng.wait_ge(s, v)
                    inst = o.fn(eng)
                    if o.inc:
                        s, v, step = o.tok
                        inst.then_inc(s, step)
                for o in self.ops[ename]:
                    if o.dma:
                        s, v, _ = o.tok
                        if waited.get(id(s), 0) < v:
                            waited[id(s)] = v
                            eng.wait_ge(s, v)
            return body

        for ename in ENGS:
            if self.ops[ename]:
                getattr(blk, ename)(make(ename))


EPS = 1e-6
NEGM = -30000.0


def mix_tables(T, g):
    NB = T // 128
    tb = {}
    slopes = np.array([2.0 ** -(4 * g + r + 1) for r in range(4)], np.float64)
    pos = np.arange(T)
    kx = np.stack([np.ones(T), np.ones(T), 128.0 * (pos // 128), (pos % 128).astype(np.float64)])
    tb["kx"] = kx.astype(NPBF)
    NS = 512
    s = np.arange(NS)
    cpos = 16 * s + 15
    kxc = np.stack([np.ones(NS), np.ones(NS), 128.0 * (cpos // 128), (cpos % 128).astype(np.float64)])
    tb["kxc"] = kxc.astype(NPBF)
    ql = np.arange(128)
    qx = np.zeros((4, 4, 128))
    for h in range(4):
        qx[0, h] = -slopes[h] * 128.0
        qx[1, h] = -slopes[h] * ql
        qx[2, h] = slopes[h]
        qx[3, h] = slopes[h]
    tb["qx"] = qx.reshape(4, 512).astype(NPBF)
    k = np.arange(128)[:, None]
    q = np.arange(128)[None, :]
    mca = np.where(k <= q, 0.0, NEGM)
    mwi = np.where(k > q, 0.0, NEGM)
    tb["mcaus"] = np.tile(mca, (1, 4)).astype(NPBF)
    tb["mwin"] = np.tile(mwi, (1, 4)).astype(NPBF)
    u = np.arange(2048 + 128)[None, :]
    sl = np.arange(128)[:, None]
    mc = np.where(16 * sl + 15 <= u, 0.0, NEGM)
    mc0 = mc.copy()
    mc0[0, :] = NEGM
    tb["mc"] = np.stack([mc0, mc], 1).astype(NPBF)
    blk = np.arange(128)[:, None]
    key = np.arange(T)[None, :]
    tb["eall"] = (key // 64 == blk).astype(np.float32).astype(NPBF)
    ov = np.zeros((128, 4, 129), np.float32)
    for ct in range(4):
        for s_l in range(128):
            c = 128 * ct + s_l - 1
            if c < 0:
                continue
            cs, ce = 16 * c, 16 * c + 32
            for b_ in range(cs // 64, min(127, (ce - 1) // 64) + 1):
                o = min(ce, 64 * b_ + 64) - max(cs, 64 * b_)
                if o > 0:
                    ov[s_l, ct, b_] = o / 32.0
        ov[:, ct, 128] = 1.0
    tb["ov"] = np.concatenate([ov[:, :, 128:129], ov[:, :, 0:128]], 2).astype(NPBF)
    am = np.zeros((NB, 128, 128), np.float32)
    for i in range(NB):
        t = 128 * i + np.arange(128)
        cur = t // 64
        b_ = np.arange(128)[None, :]
        forced = (b_ == 0) | (b_ == cur[:, None]) | (b_ == cur[:, None] - 1)
        valid = b_ * 64 <= t[:, None]
        am[i] = np.where(forced, 1e9, np.where(valid, 0.0, -1e9))
    tb["am"] = am
    hh = 4 * g + np.arange(4)
    log_g = np.log1p(-np.exp2(-5.0 - hh.astype(np.float64)))
    idx = np.arange(128, dtype=np.float64)
    sc = 32 ** -0.5
    dec = np.zeros((128, 4, 128))
    for h in range(4):
        d = idx[None, :] - idx[:, None]
        dec[:, h, :] = np.where(d >= 0, np.exp(np.maximum(d, 0) * log_g[h]), 0.0) * sc
    tb["decT"] = dec.astype(np.float32)
    xi = np.zeros((128, 128))
    zeta = np.zeros((128, 128))
    gc = np.zeros((128, 1))
    bd = np.zeros((128, 4, 128))
    for h in range(4):
        xi[32 * h:32 * h + 32, :] = np.exp((idx + 1) * log_g[h])[None, :] * sc
        zeta[:, 32 * h:32 * h + 32] = np.exp((127 - idx) * log_g[h])[:, None]
        gc[32 * h:32 * h + 32] = np.exp(128 * log_g[h])
        bd[32 * h:32 * h + 32, h, :] = 1.0
    tb["xi"] = xi.astype(np.float32)
    tb["zeta"] = zeta.astype(np.float32)
    tb["gc"] = gc.astype(np.float32)
    tb["bd"] = bd.astype(np.float32).astype(NPBF)
    tb["ident"] = np.eye(128, dtype=np.float32).astype(NPBF)
    return tb


def mix_weights(l, g, p):
    w_in = p["w_in"][l]
    offs = np.cumsum([0, 512, 128, 128, 128, 128, 128, 128, 24, 256, 256, 512, 512])
    q0, kc0, vc0, ks0, vs0, kw0, vw0, gt0, rq0, rk0, rv0, rg0 = offs[:12]
    cq = w_in[:, q0 + 256 * g: q0 + 256 * g + 256]
    sl = lambda o, w: w_in[:, o + w * g: o + w * g + w]
    wtm = np.concatenate([cq, sl(ks0, 64), sl(kw0, 64), sl(vs0, 64), sl(vw0, 64), sl(rv0, 256), sl(rg0, 256),
                          sl(rk0, 128), sl(gt0, 12)], 1)
    wfm = np.concatenate([sl(kc0, 64), sl(vc0, 64), sl(rq0, 128), sl(rk0, 128)], 1)
    wc = p["w_cmp"][l]
    wbd = np.zeros((128, 32, 128), np.float32)
    wbd[0:64, :, 0:64] = wc[0].reshape(32, 64, 64).transpose(1, 0, 2)
    wbd[64:128, :, 64:128] = wc[1].reshape(32, 64, 64).transpose(1, 0, 2)
    wcf = np.concatenate([wc[0], wc[1]], 1).reshape(16, 128, 128).transpose(1, 0, 2)
    cp = p["cmp_pos"][l].reshape(2, 2048)
    posf = cp.reshape(2, 16, 128).transpose(2, 0, 1)
    rep = lambda v: np.ascontiguousarray(np.broadcast_to(v[None, :], (128, v.shape[0])))
    gqk = np.concatenate([np.tile(p["q_norm_g"][l], 4), p["k_norm_g"][l][1], p["k_norm_g"][l][2]])
    return {
        "gam": rep(p["norm_mix_g"][l]), "wtm": np.ascontiguousarray(wtm), "wfm": np.ascontiguousarray(wfm),
        "wbd": wbd, "wcf": np.ascontiguousarray(wcf), "posf": np.ascontiguousarray(posf),
        "gqk": rep(gqk), "gkc": rep(p["k_norm_g"][l][0]),
        "gret": rep(p["ret_norm_g"][l][256 * g:256 * g + 256]),
    }


def mix_decl(nc, D, T, pw, pt, shared):
    NB = T // 128

    def din(name, shape, dt=F32):
        if name not in D:
            D[name] = nc.dram_tensor(name, list(shape), dt, kind="ExternalInput").ap()

    for nm_, shp in [("gam", [128, 1024]), ("wtm", [1024, 1164]), ("wfm", [1024, 384]), ("wbd", [128, 32, 128]),
                     ("wcf", [128, 16, 128]), ("posf", [128, 2, 16]), ("gqk", [128, 384]), ("gkc", [128, 64]),
                     ("gret", [128, 256])]:
        din(pw + nm_, shp)
    din(pt + "qx", [4, 512], BF16); din(pt + "decT", [128, 4, 128]); din(pt + "xi", [128, 128])
    din(pt + "zeta", [128, 128]); din(pt + "gc", [128, 1])
    if shared:
        din("kx", [4, T], BF16); din("kxc", [4, 512], BF16)
        din("mcaus", [128, 512], BF16); din("mwin", [128, 512], BF16); din("mc", [128, 2, 2176], BF16)
        din("eall", [128, T], BF16); din("ov", [128, 4, 129], BF16); din("am", [NB, 128, 128])
        din("bd", [128, 4, 128], BF16); din("ident", [128, 128], BF16)


WNAMES = ["gam", "wtm", "wfm", "wbd", "wcf", "posf", "gqk", "gkc", "gret"]
TG_NAMES = ["qx", "decT", "xi", "zeta", "gc"]


def emit_mix(nc, D0, T, x, mix_out, pw, pt, tag=""):
    NT = T // 512
    NB = T // 128
    D = dict(D0)
    for k_ in WNAMES:
        D[k_] = D0[pw + k_]
    for k_ in TG_NAMES:
        D[k_] = D0[pt + k_]
    mix_nsa, mix_ret = mix_out

    with ExitStack() as es:
        S = Sched(nc, es, tag)
        sb = S.sb
        gam = sb("gam_s", [128, 1024], F32)
        wtm = sb("wtm_s", [128, 8, 1164], BF16)
        wfm = sb("wfm_s", [128, 8, 384], BF16)
        wbd = sb("wbd_s", [128, 32, 128], BF16)
        wcf = sb("wcf_s", [128, 16, 128], BF16)
        posf = sb("posf_s", [128, 2, 16], BF16)
        gqk = sb("gqk_s", [128, 384], F32)
        gkc = sb("gkc_s", [128, 64], F32)
        gret = sb("gret_s", [128, 256], F32)
        KS = sb("KS", [68, T], BF16)
        KW = sb("KW", [68, T], BF16)
        KC = sb("KC", [68, 512], BF16)
        VSW = sb("VSW", [128, NB, 2, 65], BF16)
        CV = sb("CV", [128, 4, 193], BF16)
        KCVC = sb("KCVC", [128, 16 + 2048], BF16)
        EALL = sb("EALL", [128, T], BF16)
        QXB = sb("QXB", [68, 512], BF16)
        MCA = sb("MCA", [128, 512], BF16); MWI = sb("MWI", [128, 512], BF16)
        MC = sb("MC", [128, 2, 2176], BF16)
        DEC = sb("DEC", [128, 4, 128], F32); XI = sb("XI", [128, 128], F32); ZETA = sb("ZETA", [128, 128], F32)
        GC = sb("GC", [128, 1], F32); BD = sb("BD", [128, 4, 128], BF16)
        IDN = sb("IDN", [128, 128], BF16)
        ONES1 = sb("ONES1", [1, 128], BF16)
        BIASR = sb("BIASR", [1, 128], BF16)
        CM05 = sb("CM05", [128, 8], F32)
        STATE = sb("STATE", [128, 64], F32)
        SBD = sb("SBD", [128, 4, 64], BF16)
        banks = [S.ps(f"pb{i}") for i in range(8)]
        OB, PB4, RB = banks[4:8], banks[0:3], banks[3]
        cnt = {"s": 0, "m": 0}

        def mbank():
            cnt["m"] += 1
            return PB4[cnt["m"] % 3]

        def sbank():
            return mbank()

        class Rot:
            def __init__(self, name, shape, dt, n):
                self.b = [sb(f"{name}{i}", shape, dt) for i in range(n)]
                self.i = 0

            def get(self):
                self.i += 1
                return self.b[self.i % len(self.b)]

        xbuf = Rot("xb", [128, 1024], F32, 2)
        hb = Rot("hb", [128, 1024], BF16, 4)
        hT = Rot("hT", [128, 8, 512], BF16, 1)
        sm = Rot("sm", [128, 16], F32, 24)
        sqb = Rot("sqb", [128, 384], F32, 2)
        qkn = Rot("qkn", [128, 384], BF16, 4)
        t384 = Rot("t384", [128, 384], F32, 2)
        QT = [sb(f"QT{i}", [68, 512], BF16) for i in range(4)]
        RQT = Rot("RQT", [128, 512], BF16, 1); RKT = Rot("RKT", [128, 512], BF16, 1)
        RV = Rot("RV", [128, 4, 256], BF16, 1)
        G2 = Rot("G2", [128, 4, 256], F32, 1)
        KZ = Rot("KZ", [128, 4, 128], BF16, 1)
        GT = Rot("GT", [128, 4, 12], F32, 1)
        PT = Rot("PT", [128, 512], BF16, 3)
        NMT = Rot("NMT", [128, 512], BF16, 2)
        AMB = Rot("AMB", [128, 128], F32, 2)
        impb = Rot("impb", [128, 128], F32, 2)
        wk = Rot("wk", [128, 128], F32, 1)
        nmq = Rot("nmq", [128, 128], BF16, 1)
        m8 = Rot("m8", [128, 16], F32, 2)
        acc = Rot("acc", [128, 256], F32, 2)
        tmp256 = Rot("tmp256", [128, 256], F32, 2)
        fb = Rot("fb", [128, 16], F32, 4)
        nsab = Rot("nsab", [128, 256], BF16, 2)
        RQBD = Rot("RQBD", [128, 4, 128], BF16, 2)
        RQX = Rot("RQX", [128, 128], BF16, 2)
        IND = Rot("IND", [128, 4, 128], BF16, 2)
        sq256 = Rot("sq256", [128, 256], F32, 1)
        retb = Rot("retb", [128, 256], BF16, 2)
        red64 = Rot("red64", [128, 64], F32, 1)
        t4 = Rot("t4", [128, 4, 64], F32, 1)
        stg = Rot("stg", [64, 8, 512], BF16, 1)
        kcn = Rot("kcn", [128, 64], BF16, 1)
        kct = Rot("kct", [128, 64], F32, 1)

        def ld(eng, dst, src, **kw):
            S.dma(eng, dst.ap[:] if isinstance(dst, Buf) else dst, src, w=[dst] if isinstance(dst, Buf) else (), **kw)

        ld(SP, gam, D["gam"][:, :])
        S.dma(POOL, wtm.ap[:], D["wtm"].rearrange("(kc p) n -> p kc n", p=128), w=[wtm])
        S.dma(POOL, wfm.ap[:], D["wfm"].rearrange("(kc p) n -> p kc n", p=128), w=[wfm])
        S.dma(POOL, wbd.ap[:], D["wbd"][:, :, :], w=[wbd])
        S.dma(POOL, wcf.ap[:], D["wcf"][:, :, :], w=[wcf])
        S.dma(POOL, posf.ap[:], D["posf"][:, :, :], w=[posf])
        ld(SP, gqk, D["gqk"][:, :]); ld(SP, gkc, D["gkc"][:, :]); ld(SP, gret, D["gret"][:, :])
        S.dma(SP, KS.ap[64:68, :], D["kx"][:, :], w=[KS])
        S.dma(SP, KW.ap[64:68, :], D["kx"][:, :], w=[KW])
        S.dma(SP, KC.ap[64:68, :], D["kxc"][:, :], w=[KC])
        S.dma(SP, QXB.ap[64:68, :], D["qx"][:, :], w=[QXB])
        for q_ in QT:
            S.dma(SP, q_.ap[64:68, :], D["qx"][:, :], w=[q_])
        ld(SP, MCA, D["mcaus"][:, :]); ld(SP, MWI, D["mwin"][:, :]); ld(SP, MC, D["mc"][:, :, :])
        ld(SP, EALL, D["eall"][:, :])
        ld(SP, DEC, D["decT"][:, :, :]); ld(SP, XI, D["xi"][:, :]); ld(SP, ZETA, D["zeta"][:, :])
        ld(SP, GC, D["gc"][:, :]); ld(SP, BD, D["bd"][:, :, :]); ld(SP, IDN, D["ident"][:, :])
        S.op(POOL, "memset", ap=VSW.ap[:], constant=1.0, w=[VSW])
        S.op(POOL, "memset", ap=CV.ap[:], constant=0.0, w=[CV])
        S.dma(SP, CV.ap[:, :, 64:193], D["ov"][:, :, :], w=[CV])
        S.op(POOL, "memset", ap=KCVC.ap[:], constant=0.0, w=[KCVC])
        S.op(POOL, "memset", ap=KC.ap[0:64, :], constant=0.0, w=[KC])
        S.op(POOL, "memset", ap=ONES1.ap[:], constant=1.0, w=[ONES1])
        S.op(POOL, "memset", ap=CM05.ap[:], constant=-0.5, w=[CM05])
        S.op(POOL, "memset", ap=STATE.ap[:], constant=0.0, w=[STATE])
        S.op(POOL, "memset", ap=SBD.ap[:], constant=0.0, w=[SBD])
        S.op(DVE, "tensor_scalar", out=gqk.ap[:, 0:256], in0=gqk.ap[:, 0:256], scalar1=0.125, scalar2=None,
                                            op0=ALU.mult, r=[gqk], w=[gqk])
        b_ = mbank()
        for kv in range(2):
            for c in range(16):
                S.op(PE, "matmul", out=b_.ap[0:1, 64 * kv:64 * kv + 64], lhsT=posf.ap[:, kv, c:c + 1],
                                                         rhs=wcf.ap[:, c, 64 * kv:64 * kv + 64],
                                                         start=(c == 0 and kv == 0), stop=(c == 15), skip_group_check=True,
                     r=[posf, wcf], w=[b_])
        S.op(ACT, "activation", out=BIASR.ap[:], in_=b_.ap[0:1, 0:128], func=AF.Copy, r=[b_], w=[BIASR])

        def rstd_pow(dst, src, n, scale):
            pass

        def ret_gen(j, rqt, rkt, rv, g2, kz, sg):
            tok = slice(128 * j, 128 * j + 128)
            rqbd = RQBD.get(); rqx = RQX.get()
            S.op(POOL, "tensor_tensor", out=rqbd.ap[:], in0=rqt.ap[:, tok].unsqueeze(1).broadcast_to([128, 4, 128]),
                 in1=BD.ap[:], op=ALU.mult, r=[rqt, BD], w=[rqbd])
            S.op(POOL, "tensor_tensor", out=rqx.ap[:], in0=rqt.ap[:, tok], in1=XI.ap[:], op=ALU.mult, r=[rqt, XI], w=[rqx])
            yield
            pin = RB
            S.op(PE, "matmul", out=pin.ap[:, :], lhsT=rkt.ap[:, tok], rhs=rqbd.ap[:].rearrange("p h i -> p (h i)"),
                 start=True, stop=True, r=[rkt, rqbd], w=[pin])
            yield
            ind = IND.get()
            S.op(DVE, "tensor_tensor", out=ind.ap[:].rearrange("p h i -> p (h i)"), in0=pin.ap[:, :],
                 in1=DEC.ap[:].rearrange("p h i -> p (h i)"), op=ALU.mult, r=[pin, DEC], w=[ind])
            yield
            yield
            po = RB
            S.op(PE, "matmul", out=po.ap[:, 0:256], lhsT=rqx.ap[:], rhs=SBD.ap[:].rearrange("p h e -> p (h e)"),
                 start=True, stop=False, r=[rqx, SBD], w=[po])
            for h in range(4):
                S.op(PE, "matmul", out=po.ap[:, 64 * h:64 * h + 64], lhsT=ind.ap[:, h, :], rhs=rv.ap[:, j, 64 * h:64 * h + 64],
                     start=False, stop=(h == 3), r=[ind, rv], w=[po])
            pkv = RB
            S.op(PE, "matmul", out=pkv.ap[:, 256:512], lhsT=kz.ap[:, j, :], rhs=rv.ap[:, j, :], start=True, stop=True,
                 r=[kz, rv], w=[pkv])
            yield
            sq = sq256.get(); st = sm.get()
            S.op(ACT, "activation", out=sq.ap[:], in_=po.ap[:, 0:256], func=AF.Square, r=[po], w=[sq])
            t4_ = t4.get(); r64 = red64.get()
            S.op(DVE, "tensor_tensor", out=t4_.ap[:], in0=pkv.ap[:, 256:512].rearrange("p (h e) -> p h e", h=4),
                 in1=BD.ap[:, :, 0:64], op=ALU.mult, r=[pkv, BD], w=[t4_])
            S.op(DVE, "tensor_reduce", out=r64.ap[:], in_=t4_.ap[:].rearrange("p h e -> p e h"), axis=AX.X, op=ALU.add,
                 r=[t4_], w=[r64])
            S.op(DVE, "scalar_tensor_tensor", out=STATE.ap[:], in0=STATE.ap[:], scalar=GC.ap[:, 0:1], in1=r64.ap[:],
                 op0=ALU.mult, op1=ALU.add, r=[STATE, GC, r64], w=[STATE])
            S.op(POOL, "tensor_tensor", out=SBD.ap[:], in0=STATE.ap[:].unsqueeze(1).broadcast_to([128, 4, 64]),
                 in1=BD.ap[:, :, 0:64], op=ALU.mult, r=[STATE, BD], w=[SBD])
            S.op(DVE, "tensor_reduce", out=st.ap[:, 0:4], in_=sq.ap[:].rearrange("p (h d) -> p h d", h=4), axis=AX.X,
                 op=ALU.add, r=[sq], w=[st])
            S.op(DVE, "tensor_scalar", out=st.ap[:, 4:8], in0=st.ap[:, 0:4], scalar1=1.0 / 64, scalar2=EPS, op0=ALU.mult,
                 op1=ALU.add, r=[st], w=[st])
            S.op(POOL, "tensor_tensor", out=st.ap[:, 8:12], in0=st.ap[:, 4:8], in1=CM05.ap[:, 0:4], op=ALU.pow,
                 r=[st, CM05], w=[st])
            for _ in range(4):
                yield
            tm_ = tmp256.get()
            S.op(DVE, "tensor_tensor", out=tm_.ap[:].rearrange("p (h d) -> p h d", h=4),
                 in0=po.ap[:, 0:256].rearrange("p (h d) -> p h d", h=4),
                 in1=st.ap[:, 8:12].unsqueeze(2).broadcast_to([128, 4, 64]), op=ALU.mult, r=[po, st], w=[tm_])
            rb = retb.get()
            S.op(POOL, "tensor_tensor", out=rb.ap[:], in0=tm_.ap[:], in1=g2.ap[:, j, :], op=ALU.mult, r=[tm_, g2], w=[rb])
            for _ in range(3):
                yield
            pb2 = RB
            pbb2 = pb2.ap[:].bitcast(BF16)
            for h in range(4):
                S.op(PE, "transpose", out=pbb2[0:64, 128 * h:128 * h + 128], in_=rb.ap[:, 64 * h:64 * h + 64],
                     identity=IDN.ap[:], r=[rb, IDN], w=[pb2])
            yield
            S.op(DVE, "tensor_copy", out=sg.ap[:, 4:8, 128 * j:128 * j + 128],
                 in_=pbb2[0:64, 0:512].rearrange("p (h q) -> p h q", h=4), r=[pb2], w=[sg])

        def nsa_out(ac, j, sg):
            nb_ = nsab.get()
            S.op(ACT, "activation", out=nb_.ap[:], in_=ac.ap[:], func=AF.Copy, r=[ac], w=[nb_])
            pb = mbank()
            pbb = pb.ap[:].bitcast(BF16)
            for h in range(4):
                S.op(PE, "transpose", out=pbb[0:64, 128 * h:128 * h + 128], in_=nb_.ap[:, 64 * h:64 * h + 64],
                     identity=IDN.ap[:], r=[nb_, IDN], w=[pb])
            S.op(DVE, "tensor_copy", out=sg.ap[:, 0:4, 128 * j:128 * j + 128],
                 in_=pbb[0:64, 0:512].rearrange("p (h q) -> p h q", h=4), r=[pb], w=[sg])

        pend = []

        def stage_a(n):
            xs_ = {}; sts = {}; hs = {}
            for pair in ((0, 1), (2, 3)):
                for j in pair:
                    xb_ = xbuf.get()
                    r0 = 512 * n + 128 * j
                    S.dma(SP, xb_.ap[:], x[r0:r0 + 128, :], w=[xb_])
                    xs_[j] = xb_
                for j in pair:
                    h_ = hb.get(); st = sm.get()
                    S.op(ACT, "activation", out=h_.ap[:], in_=xs_[j].ap[:], func=AF.Square, accum_out=st.ap[:, 0:1],
                         r=[xs_[j]], w=[h_, st])
                    sts[j] = st; hs[j] = h_
                for j in pair:
                    st = sts[j]
                    S.op(DVE, "tensor_scalar", out=st.ap[:, 1:2], in0=st.ap[:, 0:1], scalar1=1.0 / 1024, scalar2=EPS,
                         op0=ALU.mult, op1=ALU.add, r=[st], w=[st])
                    S.op(POOL, "tensor_tensor", out=st.ap[:, 2:3], in0=st.ap[:, 1:2], in1=CM05.ap[:, 0:1], op=ALU.pow,
                         r=[st, CM05], w=[st])
                for j in pair:
                    S.op(DVE, "scalar_tensor_tensor", out=hs[j].ap[:], in0=xs_[j].ap[:], scalar=sts[j].ap[:, 2:3], in1=gam.ap[:],
                         op0=ALU.mult, op1=ALU.mult, r=[xs_[j], sts[j], gam], w=[hs[j]])
            return hs

        hs_next = stage_a(0)

        for n in range(NT):
            hTn = hT.get()
            hs = hs_next
            for j in range(4):
                pb = mbank()
                pbb = pb.ap[:].bitcast(BF16)
                for kc in range(8):
                    S.op(PE, "transpose", out=pbb[:, 128 * kc:128 * kc + 128], in_=hs[j].ap[:, 128 * kc:128 * kc + 128],
                         identity=IDN.ap[:], r=[hs[j], IDN], w=[pb])
                S.op(ACT, "activation", out=hTn.ap[:, :, 128 * j:128 * j + 128], in_=pbb.rearrange("p (k t) -> p k t", k=8),
                     func=AF.Copy, r=[pb], w=[hTn])
            rqt = RQT.get(); rkt = RKT.get()
            m_ = n % 4
            if m_ == 0 and n > 0:
                S.op(POOL, "tensor_copy", out=KCVC.ap[:, 0:16], in_=KCVC.ap[:, 2048:2064], r=[KCVC], w=[KCVC])
            for ci, dst in enumerate([None, rqt, rkt]):
                pb = mbank()
                for kc in range(8):
                    S.op(PE, "matmul", out=pb.ap[:, :], lhsT=wfm.ap[:, kc, 128 * ci:128 * ci + 128], rhs=hTn.ap[:, kc, :],
                         start=(kc == 0), stop=(kc == 7), r=[wfm, hTn], w=[pb])
                if ci == 0:
                    S.op(ACT, "activation", out=KCVC.ap[:, 16 + 512 * m_:16 + 512 * m_ + 512], in_=pb.ap[:, :], func=AF.Copy,
                         r=[pb], w=[KCVC])
                else:
                    S.op(DVE, "tensor_copy", out=dst.ap[:], in_=pb.ap[:, :], r=[pb], w=[dst])
            rv = RV.get(); g2 = G2.get(); kz = KZ.get(); gt = GT.get()
            qks = []
            for j in range(4):
                blk = 4 * n + j
                tok = slice(128 * j, 128 * j + 128)
                pa = mbank()
                for kc in range(8):
                    S.op(PE, "matmul", out=pa.ap[:, :], lhsT=hTn.ap[:, kc, tok], rhs=wtm.ap[:, kc, 0:512], start=(kc == 0),
                         stop=(kc == 7), r=[hTn, wtm], w=[pa])
                sq = sqb.get(); st = sm.get(); raw = t384.get()
                S.op(ACT, "activation", out=sq.ap[:], in_=pa.ap[:, 0:384], func=AF.Square, r=[pa], w=[sq])
                S.op(DVE, "tensor_copy", out=raw.ap[:], in_=pa.ap[:, 0:384], r=[pa], w=[raw])
                S.op(ACT, "activation", out=VSW.ap[:, blk, :, 0:64], in_=pa.ap[:, 384:512].rearrange("p (a d) -> p a d", a=2),
                     func=AF.Copy, r=[pa], w=[VSW])
                pbk = mbank()
                for kc in range(8):
                    S.op(PE, "matmul", out=pbk.ap[:, :], lhsT=hTn.ap[:, kc, tok], rhs=wtm.ap[:, kc, 512:1024], start=(kc == 0),
                         stop=(kc == 7), r=[hTn, wtm], w=[pbk])
                S.op(DVE, "tensor_copy", out=rv.ap[:, j, :], in_=pbk.ap[:, 0:256], r=[pbk], w=[rv])
                S.op(ACT, "activation", out=g2.ap[:, j, :], in_=pbk.ap[:, 256:512], func=AF.Silu, r=[pbk], w=[g2])
                pc = mbank()
                for kc in range(8):
                    S.op(PE, "matmul", out=pc.ap[:, 0:140], lhsT=hTn.ap[:, kc, tok], rhs=wtm.ap[:, kc, 1024:1164], start=(kc == 0),
                         stop=(kc == 7), r=[hTn, wtm], w=[pc])
                S.op(DVE, "tensor_tensor", out=kz.ap[:, j, :], in0=pc.ap[:, 0:128], in1=ZETA.ap[:], op=ALU.mult,
                     r=[pc, ZETA], w=[kz])
                S.op(ACT, "activation", out=gt.ap[:, j, :], in_=pc.ap[:, 128:140], func=AF.Sigmoid, r=[pc], w=[gt])
                S.op(DVE, "tensor_reduce", out=st.ap[:, 0:6], in_=sq.ap[:].rearrange("p (a d) -> p a d", a=6), axis=AX.X,
                     op=ALU.add, r=[sq], w=[st])
                S.op(DVE, "tensor_scalar", out=st.ap[:, 6:12], in0=st.ap[:, 0:6], scalar1=1.0 / 64, scalar2=EPS, op0=ALU.mult,
                     op1=ALU.add, r=[st], w=[st])
                st2 = sm.get()
                S.op(POOL, "tensor_tensor", out=st2.ap[:, 0:6], in0=st.ap[:, 6:12], in1=CM05.ap[:, 0:6], op=ALU.pow,
                     r=[st, CM05], w=[st2])
                S.op(POOL, "tensor_tensor", out=g2.ap[:, j, :], in0=g2.ap[:, j, :], in1=gret.ap[:], op=ALU.mult,
                     r=[g2, gret], w=[g2])
                S.op(DVE, "tensor_tensor", out=raw.ap[:].rearrange("p (a d) -> p a d", a=6),
                     in0=raw.ap[:].rearrange("p (a d) -> p a d", a=6),
                     in1=st2.ap[:, 0:6].unsqueeze(2).broadcast_to([128, 6, 64]), op=ALU.mult, r=[raw, st2], w=[raw])
                qk = qkn.get()
                S.op(POOL, "tensor_tensor", out=qk.ap[:], in0=raw.ap[:], in1=gqk.ap[:], op=ALU.mult, r=[raw, gqk], w=[qk])
                qks.append(qk)
            for j in range(4):
                blk = 4 * n + j
                qk = qks[j]
                pt_ = mbank()
                ptb = pt_.ap[:].bitcast(BF16)
                for a_ in range(6):
                    S.op(PE, "transpose", out=ptb[0:64, 128 * a_:128 * a_ + 128], in_=qk.ap[:, 64 * a_:64 * a_ + 64],
                         identity=IDN.ap[:], r=[qk, IDN], w=[pt_])
                S.op(ACT, "activation", out=QT[j].ap[0:64, :], in_=ptb[0:64, 0:512], func=AF.Copy, r=[pt_], w=[QT[j]])
                S.op(DVE, "tensor_copy", out=KS.ap[0:64, 128 * blk:128 * blk + 128], in_=ptb[0:64, 512:640], r=[pt_], w=[KS])
                S.op(DVE, "tensor_copy", out=KW.ap[0:64, 128 * blk:128 * blk + 128], in_=ptb[0:64, 640:768], r=[pt_], w=[KW])
                S.op(DVE, "tensor_scalar", out=QT[j].ap[64:65, :], in0=QXB.ap[64:65, :], scalar1=float(blk), scalar2=None,
                     op0=ALU.mult, r=[QXB], w=[QT[j]])
            ct = n // 4
            pcm = mbank()
            S.op(PE, "matmul", out=pcm.ap[:, 0:128], lhsT=ONES1.ap[0:1, :], rhs=BIASR.ap[0:1, :],
                                                 start=True, stop=False, r=[ONES1, BIASR], w=[pcm])
            for l in range(32):
                S.op(PE, "matmul", out=pcm.ap[:, 0:128], lhsT=KCVC.ap[:, l:l + 2033:16],
                                                          rhs=wbd.ap[:, l, :], start=False, stop=(l == 31),
                     r=[KCVC, wbd], w=[pcm])
            S.op(ACT, "activation", out=CV.ap[:, ct, 0:64], in_=pcm.ap[:, 64:128], func=AF.Copy,
                 r=[pcm], w=[CV])
            kt_ = kct.get(); st = sm.get()
            S.op(ACT, "activation", out=kt_.ap[:], in_=pcm.ap[:, 0:64], func=AF.Square,
                                                                      accum_out=st.ap[:, 0:1], r=[pcm], w=[kt_, st])
            S.op(DVE, "tensor_scalar", out=st.ap[:, 1:2], in0=st.ap[:, 0:1], scalar1=1.0 / 64, scalar2=EPS,
                                                       op0=ALU.mult, op1=ALU.add, r=[st], w=[st])
            S.op(POOL, "tensor_tensor", out=st.ap[:, 2:3], in0=st.ap[:, 1:2], in1=CM05.ap[:, 0:1],
                                                        op=ALU.pow, r=[st, CM05], w=[st])
            kn = kcn.get()
            S.op(DVE, "scalar_tensor_tensor", out=kn.ap[:], in0=pcm.ap[:, 0:64],
                                                                              scalar=st.ap[:, 2:3], in1=gkc.ap[:],
                                                                              op0=ALU.mult, op1=ALU.mult,
                 r=[pcm, st, gkc], w=[kn])
            pk = mbank()
            pkb = pk.ap[:].bitcast(BF16)
            S.op(PE, "transpose", out=pkb[0:64, 0:128], in_=kn.ap[:, 0:64], identity=IDN.ap[:],
                 r=[kn, IDN], w=[pk])
            S.op(DVE, "tensor_copy", out=KC.ap[0:64, 128 * ct:128 * ct + 128], in_=pkb[0:64, 0:128],
                 r=[pk], w=[KC])

            sg = stg.get()
            for j in range(4):
                if j == 3 and n + 1 < NT:
                    hs_next = stage_a(n + 1)
                i = 4 * n + j
                q0 = 128 * i
                qt = QT[j]
                jobs = []
                nct = (128 * i + 112) // 2048 + 1
                O_c = [OB[0], OB[1]]
                O_w = OB[2]
                O_s = OB[3]
                for c in range(nct):
                    jobs.append(("c", c, c == nct - 1))
                for kt in range(max(0, i - 4), i + 1):
                    jobs.append(("w", kt, kt == i or kt == i - 4))
                for kt in range(0, i + 1):
                    jobs.append(("s", kt, kt == i))
                first = {"c": True, "w": True, "s": True}
                nm_holder = {}

                def emit_qk(job):
                    br, kt, special = job
                    sbk = sbank()
                    if br == "c":
                        S.op(PE, "matmul", out=sbk.ap[:, :], lhsT=KC.ap[0:68, 128 * kt:128 * kt + 128], rhs=qt.ap[0:68, :],
                                                    start=True, stop=not special, r=[KC, qt], w=[sbk])
                        if special:
                            o = q0 - 2048 * kt
                            var = 0 if kt == 0 else 1
                            for h in range(4):
                                S.op(PE, "matmul", out=sbk.ap[:, 128 * h:128 * h + 128], lhsT=IDN.ap[:],
                                                                 rhs=MC.ap[:, var, o:o + 128], start=False, stop=(h == 3),
                                     r=[IDN, MC], w=[sbk])
                    elif br == "w":
                        S.op(PE, "matmul", out=sbk.ap[:, :], lhsT=KW.ap[0:68, 128 * kt:128 * kt + 128], rhs=qt.ap[0:68, :],
                                                    start=True, stop=not special, r=[KW, qt], w=[sbk])
                        if special:
                            msk = MCA if kt == i else MWI
                            S.op(PE, "matmul", out=sbk.ap[:, :], lhsT=IDN.ap[:], rhs=msk.ap[:], start=False, stop=True,
                                 r=[IDN, msk], w=[sbk])
                    else:
                        nm = nm_holder["nm"]
                        S.op(PE, "matmul", out=sbk.ap[:, :], lhsT=KS.ap[0:68, 128 * kt:128 * kt + 128], rhs=qt.ap[0:68, :],
                                                    start=True, stop=False, r=[KS, qt], w=[sbk])
                        S.op(PE, "matmul", out=sbk.ap[:, :], lhsT=EALL.ap[:, 128 * kt:128 * kt + 128], rhs=nm.ap[:],
                                                    start=False, stop=not special, r=[EALL, nm], w=[sbk])
                        if special:
                            S.op(PE, "matmul", out=sbk.ap[:, :], lhsT=IDN.ap[:], rhs=MCA.ap[:], start=False, stop=True,
                                 r=[IDN, MCA], w=[sbk])
                    return sbk

                def emit_exp_pv(job, sbk):
                    br, kt, special = job
                    p = PT.get()
                    S.op(ACT, "activation", out=p.ap[:], in_=sbk.ap[:, :], func=AF.Exp, r=[sbk], w=[p])
                    fst = first[br]
                    first[br] = False
                    if br == "c":
                        for h in range(4):
                            ob = O_c[h // 2]
                            c0 = 193 * (h % 2)
                            S.op(PE, "matmul", out=
                                ob.ap[:, c0:c0 + 193], lhsT=p.ap[:, 128 * h:128 * h + 128], rhs=CV.ap[:, kt, :],
                                start=(fst and h % 2 == 0), stop=False, skip_group_check=True, r=[p, CV], w=[ob])
                    else:
                        ob = O_w if br == "w" else O_s
                        a = 1 if br == "w" else 0
                        for h in range(4):
                            S.op(PE, "matmul", out=ob.ap[:, 65 * h:65 * h + 65], lhsT=p.ap[:, 128 * h:128 * h + 128],
                                                             rhs=VSW.ap[:, kt, a, :], start=(fst and h == 0), stop=False,
                                                             skip_group_check=True, r=[p, VSW], w=[ob])

                ac = acc.get()
                gtj = gt.ap[:, j, :].rearrange("p (r b) -> p r b", b=3)

                def epilogue(br):
                    f = fb.get()
                    bi = {"c": 0, "s": 1, "w": 2}[br]
                    if br == "c":
                        srcs = [(O_c[0], 0), (O_c[0], 193), (O_c[1], 0), (O_c[1], 193)]
                        for h, (ob, c0) in enumerate(srcs):
                            S.op(DVE, "tensor_scalar",
                                out=f.ap[:, h:h + 1], in0=ob.ap[:, c0 + 64:c0 + 65], scalar1=1e-30, scalar2=None, op0=ALU.add,
                                r=[ob], w=[f])
                    else:
                        ob = O_w if br == "w" else O_s
                        S.op(DVE, "tensor_scalar",
                            out=f.ap[:, 0:4], in0=ob.ap[:, 0:260].rearrange("p (h c) -> p h c", c=65)[:, :, 64],
                            scalar1=1e-30, scalar2=None, op0=ALU.add, r=[ob], w=[f])
                    S.op(DVE, "reciprocal", out=f.ap[:, 4:8], in_=f.ap[:, 0:4], r=[f], w=[f])
                    S.op(DVE, "tensor_tensor", out=f.ap[:, 8:12], in0=f.ap[:, 4:8], in1=gtj[:, :, bi], op=ALU.mult,
                         r=[f, gt], w=[f])
                    dst = ac if br == "c" else tmp256.get()
                    if br == "c":
                        def part_b():
                            for h, (ob, c0) in enumerate(srcs):
                                S.op(DVE, "tensor_scalar", out=dst.ap[:, 64 * h:64 * h + 64], in0=ob.ap[:, c0:c0 + 64],
                                     scalar1=f.ap[:, 8 + h:9 + h], scalar2=None, op0=ALU.mult, r=[ob, f], w=[dst])
                        nm_holder["part_b"] = part_b
                    else:
                        S.op(DVE, "tensor_tensor",
                            out=dst.ap[:].rearrange("p (h d) -> p h d", h=4),
                            in0=ob.ap[:, 0:260].rearrange("p (h c) -> p h c", c=65)[:, :, 0:64],
                            in1=f.ap[:, 8:12].unsqueeze(2).broadcast_to([128, 4, 64]), op=ALU.mult, r=[ob, f], w=[dst])
                        S.op(POOL, "tensor_tensor", out=ac.ap[:], in0=ac.ap[:], in1=dst.ap[:], op=ALU.add,
                             r=[ac, dst], w=[ac])
                    return f

                def selection(f):
                    am_ = AMB.get()
                    S.dma(SP, am_.ap[:], D["am"][i, :, :], w=[am_])
                    im = impb.get()
                    srcs = [(O_c[0], 0), (O_c[0], 193), (O_c[1], 0), (O_c[1], 193)]
                    for h, (ob, c0) in enumerate(srcs):
                        prev = am_ if h == 0 else im
                        S.op(DVE, "scalar_tensor_tensor",
                            out=im.ap[:], in0=ob.ap[:, c0 + 65:c0 + 193], scalar=f.ap[:, 4 + h:5 + h], in1=prev.ap[:],
                            op0=ALU.mult, op1=ALU.add, r=[ob, f, prev], w=[im])
                    mm = m8.get(); w_ = wk.get()
                    S.op(DVE, "max", out=mm.ap[:, 0:8], in_=im.ap[:], r=[im], w=[mm])
                    S.op(DVE, "match_replace", out=w_.ap[:], in_to_replace=mm.ap[:, 0:8], in_values=im.ap[:],
                                                        imm_value=-3e38, r=[im, mm], w=[w_])
                    S.op(DVE, "max", out=mm.ap[:, 8:16], in_=w_.ap[:], r=[w_], w=[mm])
                    nq = nmq.get()
                    S.op(DVE, "tensor_scalar", out=nq.ap[:], in0=im.ap[:], scalar1=mm.ap[:, 15:16], scalar2=NEGM,
                                                        op0=ALU.is_lt, op1=ALU.mult, r=[im, mm], w=[nq])
                    nm_holder["nq"] = nq

                def selection2():
                    nq = nm_holder["nq"]
                    pb = mbank()
                    pbb = pb.ap[:].bitcast(BF16)
                    S.op(PE, "transpose", out=pbb[:, 0:128], in_=nq.ap[:], identity=IDN.ap[:], r=[nq, IDN], w=[pb])
                    nm = NMT.get()
                    S.op(ACT, "activation", out=nm.ap[:].rearrange("p (h q) -> p h q", h=4),
                         in_=pbb[:, 0:128].unsqueeze(1).broadcast_to([128, 4, 128]), func=AF.Copy, r=[pb], w=[nm])
                    nm_holder["nm"] = nm

                emitted = {}
                rgen = ret_gen(j, rqt, rkt, rv, g2, kz, sg)

                def ensure(kk):
                    if kk >= len(jobs) or kk in emitted:
                        return True
                    if jobs[kk][0] == "s" and "nm" not in nm_holder:
                        return False
                    emitted[kk] = emit_qk(jobs[kk])
                    return True

                ensure(0)
                for k in range(len(jobs)):
                    job = jobs[k]
                    if ensure(k + 1):
                        ensure(k + 2)
                    emit_exp_pv(job, emitted[k])
                    if job[0] == "s":
                        next(rgen, None)
                    br = job[0]
                    if k + 1 == len(jobs) or jobs[k + 1][0] != br:
                        f = epilogue(br)
                        if br == "c":
                            selection(f)
                            nm_holder["part_b"]()
                    if k + 1 < len(jobs) and (k + 1) not in emitted:
                        if "nm" not in nm_holder:
                            while pend:
                                nsa_out(*pend.pop(0), sg)
                            selection2()
                        ensure(k + 1)
                pend.append((ac, j))
                for _ in rgen:
                    pass
            while pend:
                nsa_out(*pend.pop(0), sg)
            S.dma(POOL, mix_nsa[:, :, 512 * n:512 * n + 512].rearrange("c p t -> p c t"), sg.ap[:, 0:4, :], r=[sg])
            S.dma(POOL, mix_ret[:, :, 512 * n:512 * n + 512].rearrange("c p t -> p c t"), sg.ap[:, 4:8, :], r=[sg])
        S.emit()
        print("mix ops:", {e: len(S.ops[e]) for e in ENGS}, "sems:", S.nsem)


def build_mix(T):
    nc = bass.Bass("TRN2", target_bir_lowering=False)
    D = {}
    mix_decl(nc, D, T, "", "", True)
    x = nc.dram_tensor("x", [T, 1024], F32, kind="ExternalInput").ap()
    mixT = nc.dram_tensor("mixT", [8, 64, T], BF16, kind="ExternalOutput").ap()
    emit_mix(nc, D, T, x, (mixT[0:4], mixT[4:8]), "", "")
    return nc


DFF = 2816
NFC = DFF // 128
FG = 2
NG = NFC // FG


def ffn_decl(nc, D, NE, pf):
    def din(name, shape, dt=F32):
        if name not in D:
            D[name] = nc.dram_tensor(name, list(shape), dt, kind="ExternalInput").ap()
    din(pf + "wout", [1024, 1024]); din(pf + "gam", [128, 1024])
    din(pf + "wg", [NE, 1024, DFF]); din(pf + "wu", [NE, 1024, DFF]); din(pf + "wd", [NE, DFF, 1024])
    din("identf", [128, 128])
    if NE > 1:
        din(pf + "router", [1024, 8]); din(pf + "rb", [128, 8])


def emit_ffn(nc, D0, TOK, NE, x_srcs, mix_srcs, y, pf, sel=None, TS=2048, tag=""):
    moe = NE > 1
    TS = min(TS, TOK)
    NST = TOK // TS
    NSUB = TS // 128
    NTT = TS // 512
    D = dict(D0)
    for k_ in ["wout", "gam", "wg", "wu", "wd", "router", "rb"]:
        if pf + k_ in D0:
            D[k_] = D0[pf + k_]
    blend = len(x_srcs) == 2

    with ExitStack() as es:
        S = Sched(nc, es, tag)
        sb = S.sb
        gam = sb("gam_s", [128, 1024], F32)
        IDF = sb("IDF", [128, 128], F32)
        CM05 = sb("CM05", [128, 8], F32)
        wout = sb("wout_s", [128, 8, 1024], BF16)
        yacc = sb("yacc", [128, NSUB, 1024], F32)
        h2T = sb("h2T", [128, 8, TS], BF16)
        cmb = sb("cmb", [128, NSUB, 8], F32)
        if moe:
            rt = sb("rt_s", [128, 8, 8], F32)
            rb = sb("rb_s", [128, 8], F32)
        banks = [S.ps(f"pb{i}") for i in range(8)]

        class Rot:
            def __init__(self, name, shape, dt, n):
                self.b = [sb(f"{name}{i}", shape, dt) for i in range(n)]
                self.i = 0

            def get(self):
                self.i += 1
                return self.b[self.i % len(self.b)]

        xbuf = Rot("xb", [128, 1024], F32, 2)
        if blend:
            xbuf2 = Rot("xb2", [128, 1024], F32, 1)
            mxb2 = Rot("mxb2", [128, 8, 128], BF16, 1)
            SEL = sb("SEL", [128, 2], F32)
            S.dma(SP, SEL.ap[:], D0[sel][:, :], w=[SEL])
        mxb = Rot("mxb", [128, 8, 128], BF16, 2)
        h2f = Rot("h2f", [128, 1024], F32, 1)
        h2tf = Rot("h2tf", [128, 8, 128], F32, 1)
        sm = Rot("sm", [128, 16], F32, 4)
        lgb = Rot("lgb", [128, 8], F32, 2)
        m8 = Rot("m8", [128, 8], F32, 2)
        c1b = Rot("c1b", [128, 8], F32, 2)
        WG = Rot("WG", [128, 8, FG * 128], BF16, 3)
        WU = Rot("WU", [128, 8, FG * 128], BF16, 3)
        WD = Rot("WD", [128, FG, 1024], BF16, 3)
        AT = Rot("AT", [128, FG, TS], BF16, 2)
        sgb = Rot("sgb", [128, 512], F32, 2)

        S.dma(SP, gam.ap[:], D["gam"][:, :], w=[gam])
        S.dma(SP, IDF.ap[:], D["identf"][:, :], w=[IDF])
        S.dma(POOL, wout.ap[:], D["wout"].rearrange("(c p) n -> p c n", p=128), w=[wout])
        S.op(POOL, "memset", ap=CM05.ap[:], constant=-0.5, w=[CM05])
        S.op(POOL, "memset", ap=cmb.ap[:], constant=1.0, w=[cmb])
        if moe:
            S.dma(SP, rt.ap[:], D["router"].rearrange("(kc p) n -> p kc n", p=128), w=[rt])
            S.dma(SP, rb.ap[:], D["rb"][:, :], w=[rb])

        for stile in range(NST):
            t0 = stile * TS
            for s in range(NSUB):
                r0 = t0 + 128 * s
                xb_ = xbuf.get(); mx = mxb.get()
                S.dma(SP, xb_.ap[:], x_srcs[0][r0:r0 + 128, :], w=[xb_])
                S.dma(SP, mx.ap[:], mix_srcs[0][:, :, r0:r0 + 128].rearrange("c p t -> p c t"), w=[mx])
                if blend:
                    xb2 = xbuf2.get(); mx2 = mxb2.get()
                    S.dma(SP, xb2.ap[:], x_srcs[1][r0:r0 + 128, :], w=[xb2])
                    S.dma(SP, mx2.ap[:], mix_srcs[1][:, :, r0:r0 + 128].rearrange("c p t -> p c t"), w=[mx2])
                    S.op(POOL, "tensor_scalar", out=xb_.ap[:], in0=xb_.ap[:], scalar1=SEL.ap[:, 0:1], scalar2=None,
                         op0=ALU.mult, r=[xb_, SEL], w=[xb_])
                    S.op(DVE, "scalar_tensor_tensor", out=xb_.ap[:], in0=xb2.ap[:], scalar=SEL.ap[:, 1:2], in1=xb_.ap[:],
                         op0=ALU.mult, op1=ALU.add, r=[xb2, SEL, xb_], w=[xb_])
                    S.op(POOL, "tensor_scalar", out=mx.ap[:], in0=mx.ap[:], scalar1=SEL.ap[:, 0:1], scalar2=None,
                         op0=ALU.mult, r=[mx, SEL], w=[mx])
                    S.op(DVE, "scalar_tensor_tensor", out=mx.ap[:], in0=mx2.ap[:], scalar=SEL.ap[:, 1:2], in1=mx.ap[:],
                         op0=ALU.mult, op1=ALU.add, r=[mx2, SEL, mx], w=[mx])
                for half in range(2):
                    pb = banks[half]
                    for c in range(8):
                        S.op(PE, "matmul", out=pb.ap[:, :], lhsT=mx.ap[:, c, :], rhs=wout.ap[:, c, 512 * half:512 * half + 512],
                             start=(c == 0), stop=(c == 7), r=[mx, wout], w=[pb])
                    S.op(DVE, "tensor_tensor", out=yacc.ap[:, s, 512 * half:512 * half + 512], in0=pb.ap[:, :],
                         in1=xb_.ap[:, 512 * half:512 * half + 512], op=ALU.add, r=[pb, xb_], w=[yacc])
                hf = h2f.get(); st = sm.get()
                S.op(ACT, "activation", out=hf.ap[:], in_=yacc.ap[:, s, :], func=AF.Square, accum_out=st.ap[:, 0:1],
                     r=[yacc], w=[hf, st])
                S.op(DVE, "tensor_scalar", out=st.ap[:, 1:2], in0=st.ap[:, 0:1], scalar1=1.0 / 1024, scalar2=EPS,
                     op0=ALU.mult, op1=ALU.add, r=[st], w=[st])
                S.op(POOL, "tensor_tensor", out=st.ap[:, 2:3], in0=st.ap[:, 1:2], in1=CM05.ap[:, 0:1], op=ALU.pow,
                     r=[st, CM05], w=[st])
                S.op(DVE, "scalar_tensor_tensor", out=hf.ap[:], in0=yacc.ap[:, s, :], scalar=st.ap[:, 2:3], in1=gam.ap[:],
                     op0=ALU.mult, op1=ALU.mult, r=[yacc, st, gam], w=[hf])
                for hh in range(2):
                    pb = banks[2 + hh]
                    for k4 in range(4):
                        kc = 4 * hh + k4
                        S.op(PE, "transpose", out=pb.ap[:, 128 * k4:128 * k4 + 128], in_=hf.ap[:, 128 * kc:128 * kc + 128],
                             identity=IDF.ap[:], r=[hf, IDF], w=[pb])
                    S.op(ACT, "activation", out=h2T.ap[:, 4 * hh:4 * hh + 4, 128 * s:128 * s + 128],
                         in_=pb.ap[:, :].rearrange("p (k t) -> p k t", k=4), func=AF.Copy, r=[pb], w=[h2T])
                    if moe:
                        if hh == 0:
                            htf = h2tf.get()
                        S.op(DVE, "tensor_copy", out=htf.ap[:, 4 * hh:4 * hh + 4, :],
                             in_=pb.ap[:, :].rearrange("p (k t) -> p k t", k=4), r=[pb], w=[htf])
                if moe:
                    pr = banks[4]
                    for kc in range(8):
                        S.op(PE, "matmul", out=pr.ap[:, 0:8], lhsT=htf.ap[:, kc, :], rhs=rt.ap[:, kc, :], start=(kc == 0),
                             stop=(kc == 7), r=[htf, rt], w=[pr])
                    lg = lgb.get(); mm = m8.get(); c1 = c1b.get(); st2 = sm.get()
                    S.op(DVE, "tensor_tensor", out=lg.ap[:], in0=pr.ap[:, 0:8], in1=rb.ap[:], op=ALU.add, r=[pr, rb], w=[lg])
                    S.op(DVE, "max", out=mm.ap[:], in_=lg.ap[:], r=[lg], w=[mm])
                    S.op(DVE, "tensor_tensor", out=st2.ap[:, 0:1], in0=mm.ap[:, 0:1], in1=mm.ap[:, 1:2], op=ALU.subtract,
                         r=[mm], w=[st2])
                    S.op(ACT, "activation", out=st2.ap[:, 1:2], in_=st2.ap[:, 0:1], func=AF.Sigmoid, r=[st2], w=[st2])
                    S.op(DVE, "tensor_scalar", out=st2.ap[:, 2:3], in0=st2.ap[:, 1:2], scalar1=-1.0, scalar2=1.0,
                         op0=ALU.mult, op1=ALU.add, r=[st2], w=[st2])
                    S.op(DVE, "tensor_scalar", out=c1.ap[:], in0=lg.ap[:], scalar1=mm.ap[:, 0:1], scalar2=st2.ap[:, 1:2],
                         op0=ALU.is_equal, op1=ALU.mult, r=[lg, mm, st2], w=[c1])
                    S.op(DVE, "tensor_scalar", out=cmb.ap[:, s, :], in0=lg.ap[:], scalar1=mm.ap[:, 1:2], scalar2=st2.ap[:, 2:3],
                         op0=ALU.is_equal, op1=ALU.mult, r=[lg, mm, st2], w=[cmb])
                    S.op(DVE, "tensor_tensor", out=cmb.ap[:, s, :], in0=cmb.ap[:, s, :], in1=c1.ap[:], op=ALU.add,
                         r=[cmb, c1], w=[cmb])
            groups = [(e, fg) for e in range(NE) for fg in range(NG)]

            def load(e, fg):
                wg_ = WG.get(); wu_ = WU.get(); wd_ = WD.get()
                c0 = fg * FG * 128
                S.dma(POOL, wg_.ap[:], D["wg"][e].rearrange("(kc p) n -> p kc n", p=128)[:, :, c0:c0 + FG * 128], w=[wg_])
                S.dma(POOL, wu_.ap[:], D["wu"][e].rearrange("(kc p) n -> p kc n", p=128)[:, :, c0:c0 + FG * 128], w=[wu_])
                S.dma(POOL, wd_.ap[:], D["wd"][e].rearrange("(fc p) n -> p fc n", p=128)[:, fg * FG:fg * FG + FG, :], w=[wd_])
                return wg_, wu_, wd_

            def gu(w3):
                wg_, wu_, wd_ = w3
                at = AT.get()
                for tt in range(NTT):
                    for fc in range(FG):
                        pg = banks[(tt * FG + fc) % 2]
                        pu = banks[2 + (tt * FG + fc) % 2]
                        for kc in range(8):
                            S.op(PE, "matmul", out=pg.ap[:, :], lhsT=wg_.ap[:, kc, 128 * fc:128 * fc + 128],
                                 rhs=h2T.ap[:, kc, 512 * tt:512 * tt + 512], start=(kc == 0), stop=(kc == 7), r=[wg_, h2T], w=[pg])
                        for kc in range(8):
                            S.op(PE, "matmul", out=pu.ap[:, :], lhsT=wu_.ap[:, kc, 128 * fc:128 * fc + 128],
                                 rhs=h2T.ap[:, kc, 512 * tt:512 * tt + 512], start=(kc == 0), stop=(kc == 7), r=[wu_, h2T], w=[pu])
                        sg = sgb.get()
                        S.op(ACT, "activation", out=sg.ap[:], in_=pg.ap[:, :], func=AF.Silu, r=[pg], w=[sg])
                        S.op(DVE, "tensor_tensor", out=at.ap[:, fc, 512 * tt:512 * tt + 512], in0=pu.ap[:, :], in1=sg.ap[:],
                             op=ALU.mult, r=[pu, sg], w=[at])
                return at

            dcnt = [0]

            def down(e, w3, at):
                wd_ = w3[2]
                for s in range(NSUB):
                    for half in range(2):
                        dcnt[0] += 1
                        pd = banks[4 + dcnt[0] % 4]
                        for fc in range(FG):
                            S.op(PE, "matmul", out=pd.ap[:, :], lhsT=at.ap[:, fc, 128 * s:128 * s + 128],
                                 rhs=wd_.ap[:, fc, 512 * half:512 * half + 512], start=(fc == 0), stop=(fc == FG - 1),
                                 r=[at, wd_], w=[pd])
                        S.op(DVE, "scalar_tensor_tensor", out=yacc.ap[:, s, 512 * half:512 * half + 512], in0=pd.ap[:, :],
                             scalar=cmb.ap[:, s, e:e + 1], in1=yacc.ap[:, s, 512 * half:512 * half + 512], op0=ALU.mult,
                             op1=ALU.add, r=[pd, cmb, yacc], w=[yacc])

            w_cur = load(*groups[0])
            w_nxt = load(*groups[1]) if len(groups) > 1 else None
            at_prev = None
            prev = None
            for gi, (e, fg) in enumerate(groups):
                at = gu(w_cur)
                if prev is not None:
                    down(*prev)
                prev = (e, w_cur, at)
                w_cur = w_nxt
                if gi + 2 < len(groups):
                    w_nxt = load(*groups[gi + 2])
            down(*prev)
            for s in range(NSUB):
                r0 = t0 + 128 * s
                S.dma(SP, y[r0:r0 + 128, :], yacc.ap[:, s, :], r=[yacc])
        S.emit()
        print("ffn ops:", {e: len(S.ops[e]) for e in ENGS}, "sems:", S.nsem)


def build_ffn(TOK, NE, TS=2048):
    nc = bass.Bass("TRN2", target_bir_lowering=False)
    D = {}
    ffn_decl(nc, D, NE, "")
    x = nc.dram_tensor("x", [TOK, 1024], F32, kind="ExternalInput").ap()
    mixf = nc.dram_tensor("mixf", [8, 128, TOK], BF16, kind="ExternalInput").ap()
    y = nc.dram_tensor("y", [TOK, 1024], F32, kind="ExternalOutput").ap()
    emit_ffn(nc, D, TOK, NE, [x], [mixf], y, "", TS=TS)
    return nc


def build_fused(T):
    TOK = T // 2
    nc = bass.Bass("TRN2", target_bir_lowering=False)
    D = {}
    x = nc.dram_tensor("x", [T, 1024], F32, kind="ExternalInput").ap()
    D["sel"] = nc.dram_tensor("sel", [128, 2], F32, kind="ExternalInput").ap()
    first = True
    for l in range(2):
        for g in range(2):
            mix_decl(nc, D, T, f"m{l}{g}_", f"t{g}_", first)
            first = False
    ffn_decl(nc, D, 1, "f0_")
    ffn_decl(nc, D, 8, "f1_")
    y = nc.dram_tensor("y", [TOK, 1024], F32, kind="ExternalOutput").ap()
    mixs = nc.dram_tensor("mixs", [16, 64, T], BF16, kind="Internal").ap()
    x1s = nc.dram_tensor("x1s", [T, 1024], F32, kind="Internal").ap()
    mix8 = mixs.rearrange("(c two) p t -> c (two p) t", two=2)
    for g in range(2):
        emit_mix(nc, D, T, x, (mixs[4 * g:4 * g + 4], mixs[8 + 4 * g:8 + 4 * g + 4]), f"m0{g}_", f"t{g}_", tag=f"a{g}")
    emit_ffn(nc, D, T, 1, [x], [mix8], x1s, "f0_", tag="b")
    for g in range(2):
        emit_mix(nc, D, T, x1s, (mixs[4 * g:4 * g + 4], mixs[8 + 4 * g:8 + 4 * g + 4]), f"m1{g}_", f"t{g}_", tag=f"c{g}")
    emit_ffn(nc, D, TOK, 8, [x1s[0:TOK], x1s[TOK:T]], [mix8[:, :, 0:TOK], mix8[:, :, TOK:T]], y, "f1_", sel="sel", tag="d")
    return nc


def fused_inputs(T, b, hf, xb, p, tabs):
    d = {"x": xb, "sel": np.ascontiguousarray(np.broadcast_to(np.array([1.0 - hf, float(hf)], np.float32)[None, :], (128, 2)))}
    shared = ["kx", "kxc", "mcaus", "mwin", "mc", "eall", "ov", "am", "bd", "ident"]
    for k_ in shared:
        d[k_] = tabs[0][k_]
    for g in range(2):
        for k_ in TG_NAMES:
            d[f"t{g}_{k_}"] = tabs[g][k_]
        for l in range(2):
            for k_, v in mix_weights(l, g, p).items():
                d[f"m{l}{g}_{k_}"] = v
    rep = lambda v, n: np.ascontiguousarray(np.broadcast_to(v[None, :], (128, n)))
    d["identf"] = np.eye(128, dtype=np.float32)
    d["f0_wout"] = p["w_out"][0]; d["f0_gam"] = rep(p["norm_ffn_g"][0], 1024)
    d["f0_wg"] = p["ffn_w_gate"]; d["f0_wu"] = p["ffn_w_up"]; d["f0_wd"] = p["ffn_w_down"]
    d["f1_wout"] = p["w_out"][1]; d["f1_gam"] = rep(p["norm_ffn_g"][1], 1024)
    d["f1_wg"] = p["moe_w_gate"][0]; d["f1_wu"] = p["moe_w_up"][0]; d["f1_wd"] = p["moe_w_down"][0]
    d["f1_router"] = p["moe_router"][0]; d["f1_rb"] = rep(p["moe_router_b"][0], 8)
    return d


_CACHE = {}


def kernel(x, norm_mix_g, w_in, q_norm_g, k_norm_g, cmp_pos, w_cmp, ret_norm_g, w_out,
           norm_ffn_g, ffn_w_gate, ffn_w_up, ffn_w_down,
           moe_router, moe_router_b, moe_w_gate, moe_w_up, moe_w_down):
    f32 = lambda a: np.ascontiguousarray(np.asarray(a, dtype=np.float32))
    p = {"norm_mix_g": f32(norm_mix_g), "w_in": f32(w_in), "q_norm_g": f32(q_norm_g), "k_norm_g": f32(k_norm_g),
         "cmp_pos": f32(cmp_pos), "w_cmp": f32(w_cmp), "ret_norm_g": f32(ret_norm_g), "w_out": f32(w_out),
         "norm_ffn_g": f32(norm_ffn_g), "ffn_w_gate": f32(ffn_w_gate), "ffn_w_up": f32(ffn_w_up),
         "ffn_w_down": f32(ffn_w_down), "moe_router": f32(moe_router), "moe_router_b": f32(moe_router_b),
         "moe_w_gate": f32(moe_w_gate), "moe_w_up": f32(moe_w_up), "moe_w_down": f32(moe_w_down)}
    xc = f32(x)
    B, T, _ = xc.shape
    TOK = T // 2
    if T not in _CACHE:
        _CACHE[T] = build_fused(T)
    nc = _CACHE[T]
    tabs = [mix_tables(T, g) for g in range(2)]
    ims = [fused_inputs(T, c // 2, c % 2, xc[c // 2], p, tabs) for c in range(8)]
    res = run_bass_kernel_spmd(nc, ims, core_ids=list(range(8))).results
    out = np.empty_like(xc)
    for c in range(8):
        b, hf = c // 2, c % 2
        out[b, hf * TOK:(hf + 1) * TOK] = np.asarray(res[c]["y"])
    return out
```
